# Optimizing a Trainium2 kernel written in Bass

```python
import math
import jax, jax.numpy as jnp
from jax import lax
import numpy as np

D_MODEL = 4096
BATCH = 1
SEQ = 8192
DEPTH = 1

RET_HEADS = 8
RET_HEAD_DIM = 256
RET_WIDTH = RET_HEADS * RET_HEAD_DIM
LRU_WIDTH = D_MODEL - RET_WIDTH
LRU_BLOCKS = 8
LRU_BLOCK_DIM = LRU_WIDTH // LRU_BLOCKS
CONV_WIDTH = 4
RG_C = 8.0
RET_CHUNK = 128
ROPE_BASE = 10000.0
IN_COLS = 4 * RET_WIDTH + 2 * LRU_WIDTH
MEM_LEN = 256
X_HEADS = 4
X_HEAD_DIM = D_MODEL // X_HEADS
N_GROUPS = 4
EXPERTS_PER_GROUP = 8
N_EXPERTS = N_GROUPS * EXPERTS_PER_GROUP
TOP_K_IN_GROUP = 2
D_EXPERT = D_MODEL // 4
MOE_BLOCK = 128
NORM_EPS = 1e-6
GN_EPS = 1e-5

kernel_name = 'hymba_style_retention_rglru_memxattn_hmoe'


def rms_norm(x, g):
    xf = x.astype(jnp.float32)
    y = xf * lax.rsqrt(jnp.mean(xf * xf, axis=-1, keepdims=True) + NORM_EPS)
    return (y * g.astype(jnp.float32)).astype(x.dtype)


def rope_tables(positions):
    inv_freq = ROPE_BASE ** (-jnp.arange(0, RET_HEAD_DIM, 2, dtype=jnp.float32) / RET_HEAD_DIM)
    ang = positions.astype(jnp.float32)[..., None] * inv_freq
    return jnp.cos(ang)[:, :, None, :], jnp.sin(ang)[:, :, None, :]


def apply_rope(t, cos, sin):
    t1, t2 = jnp.split(t, 2, axis=-1)
    return jnp.concatenate([t1 * cos - t2 * sin, t1 * sin + t2 * cos], axis=-1)


def chunkwise_retention(q, k, v):
    B, S, H, dk = q.shape
    dv = v.shape[-1]
    nc = S // RET_CHUNK
    C = RET_CHUNK

    def to_chunks(t):
        return t.reshape(B, nc, C, H, t.shape[-1]).transpose(1, 0, 3, 2, 4)

    qc, kc, vc = to_chunks(q), to_chunks(k), to_chunks(v)
    lg = jnp.log1p(-jnp.exp2(-5.0 - jnp.arange(H, dtype=jnp.float32)))
    idx = jnp.arange(C, dtype=jnp.float32)
    diff = idx[:, None] - idx[None, :]
    decay = jnp.where(diff >= 0, jnp.exp(lg[:, None, None] * jnp.maximum(diff, 0.0)), 0.0)
    xi = jnp.exp(lg[:, None] * (idx + 1.0))
    zeta = jnp.exp(lg[:, None] * (C - 1.0 - idx))
    chunk_decay = jnp.exp(lg * C)

    def step(R, qkv):
        qi, ki, vi = qkv
        inner = jnp.einsum('bhnd,bhmd->bhnm', qi, ki) * decay
        o = (jnp.einsum('bhnm,bhme->bhne', inner, vi)
             + jnp.einsum('bhnd,bhde->bhne', qi, R) * xi[None, :, :, None])
        R = (R * chunk_decay[None, :, None, None]
             + jnp.einsum('bhmd,bhme->bhde', ki * zeta[None, :, :, None], vi))
        return R, o

    R0 = jnp.zeros((B, H, dk, dv), jnp.float32)
    _, o = lax.scan(step, R0, (qc, kc, vc))
    return o.transpose(1, 0, 3, 2, 4).reshape(B, S, H, dv)


def retention_group(q, k, v, g, cos, sin, gn_g):
    B, S, _ = q.shape
    shp = (B, S, RET_HEADS, RET_HEAD_DIM)
    qf = apply_rope(q.astype(jnp.float32).reshape(shp), cos, sin)
    kf = apply_rope(k.astype(jnp.float32).reshape(shp), cos, sin) * (RET_HEAD_DIM ** -0.5)
    vf = v.astype(jnp.float32).reshape(shp)
    o = chunkwise_retention(qf, kf, vf)
    mu = jnp.mean(o, axis=-1, keepdims=True)
    var = jnp.mean(jnp.square(o - mu), axis=-1, keepdims=True)
    o = (o - mu) * lax.rsqrt(var + GN_EPS) * gn_g.astype(jnp.float32).reshape(RET_HEADS, RET_HEAD_DIM)
    o = o.reshape(B, S, RET_WIDTH) * jax.nn.silu(g.astype(jnp.float32))
    return o.astype(q.dtype)


def rg_lru_group(xb, gb, conv_w, conv_b, w_a, b_a, w_i, b_i, lam, out_g):
    B, S, C = xb.shape
    xc = lax.conv_general_dilated(
        xb, conv_w.astype(xb.dtype)[:, None, :], window_strides=(1,),
        padding=[(CONV_WIDTH - 1, 0)], dimension_numbers=('NWC', 'WIO', 'NWC'),
        feature_group_count=C) + conv_b.astype(xb.dtype)
    xg = xc.astype(jnp.float32).reshape(B, S, LRU_BLOCKS, LRU_BLOCK_DIM)
    r = jax.nn.sigmoid(jnp.einsum('bsnc,ncd->bsnd', xg, w_a.astype(jnp.float32))
                       + b_a.astype(jnp.float32).reshape(LRU_BLOCKS, LRU_BLOCK_DIM))
    i = jax.nn.sigmoid(jnp.einsum('bsnc,ncd->bsnd', xg, w_i.astype(jnp.float32))
                       + b_i.astype(jnp.float32).reshape(LRU_BLOCKS, LRU_BLOCK_DIM))
    log_a = (-RG_C * r.reshape(B, S, C)) * jax.nn.softplus(-lam.astype(jnp.float32))
    a = jnp.exp(log_a)
    bterm = jnp.sqrt(-jnp.expm1(2.0 * log_a)) * (i * xg).reshape(B, S, C)

    def combine(left, right):
        a1, b1 = left
        a2, b2 = right
        return a1 * a2, a2 * b1 + b2

    _, h = lax.associative_scan(combine, (a, bterm), axis=1)
    y = h * jax.nn.gelu(gb.astype(jnp.float32), approximate=True)
    return rms_norm(y, out_g).astype(xb.dtype)


def memory_cross_attention(h, memn, wq, wk, wv, wo):
    B, S, D = h.shape
    M = memn.shape[1]
    q = (h @ wq).reshape(B, S, X_HEADS, X_HEAD_DIM)
    k = (memn @ wk).reshape(B, M, X_HEADS, X_HEAD_DIM)
    v = (memn @ wv).reshape(B, M, X_HEADS, X_HEAD_DIM)
    s = jnp.einsum('bshd,bmhd->bhsm', q, k).astype(jnp.float32) * (X_HEAD_DIM ** -0.5)
    p = jax.nn.softmax(s, axis=-1).astype(v.dtype)
    o = jnp.einsum('bhsm,bmhd->bshd', p, v).reshape(B, S, D)
    return o @ wo


def hierarchical_moe(h, wg_r, bg_r, we_r, be_r, w_gate, w_up, w_down):
    B, S, D = h.shape
    T = B * S
    N = T * TOP_K_IN_GROUP
    ht = h.reshape(T, D)
    g_prob = jax.nn.softmax((ht @ wg_r).astype(jnp.float32) + bg_r.astype(jnp.float32), axis=-1)
    g_val, g_idx = lax.top_k(g_prob, 1)
    e_logits = ((ht @ we_r).astype(jnp.float32) + be_r.astype(jnp.float32)).reshape(T, N_GROUPS, EXPERTS_PER_GROUP)
    e_sel = jnp.take_along_axis(e_logits, g_idx[:, :, None], axis=1)[:, 0]
    top_logit, top_local = lax.top_k(e_sel, TOP_K_IN_GROUP)
    top_w = jax.nn.softmax(top_logit, axis=-1) * g_val
    expert_id = (g_idx * EXPERTS_PER_GROUP + top_local).reshape(N)
    weight = top_w.reshape(N)
    token_id = jnp.repeat(jnp.arange(T, dtype=jnp.int32), TOP_K_IN_GROUP)
    order = jnp.argsort(expert_id)
    se, stok, sw = expert_id[order], token_id[order], weight[order]
    counts = jnp.bincount(expert_id, length=N_EXPERTS)
    starts = jnp.cumsum(counts) - counts
    padded = (counts + MOE_BLOCK - 1) // MOE_BLOCK * MOE_BLOCK
    pends = jnp.cumsum(padded)
    pstarts = pends - padded
    dest = pstarts[se] + jnp.arange(N, dtype=jnp.int32) - starts[se]
    n_blocks = (N + MOE_BLOCK - 1) // MOE_BLOCK + N_EXPERTS
    R = n_blocks * MOE_BLOCK
    row_tok = jnp.zeros((R,), jnp.int32).at[dest].set(stok)
    row_w = jnp.zeros((R,), jnp.float32).at[dest].set(sw)
    block_start = jnp.arange(n_blocks, dtype=jnp.int32) * MOE_BLOCK
    block_expert = jnp.minimum(jnp.searchsorted(pends, block_start, side='right'), N_EXPERTS - 1)

    def run_block(args):
        tok, wrow, e = args
        xb = ht[tok]
        y = (jax.nn.silu(xb @ w_gate[e]) * (xb @ w_up[e])) @ w_down[e]
        return y * wrow[:, None].astype(y.dtype)

    y = lax.map(run_block, (row_tok.reshape(n_blocks, MOE_BLOCK),
                            row_w.reshape(n_blocks, MOE_BLOCK), block_expert))
    out = jnp.zeros((T, D), h.dtype).at[row_tok].add(y.reshape(R, D).astype(h.dtype))
    return out.reshape(B, S, D)


def setup_inputs(seed: int = 0) -> dict:
    key = jax.random.key(seed)
    ks = jax.random.split(key, 32)
    f32 = jnp.float32
    L = DEPTH

    def nrm(k, shape, fan_in):
        return jax.random.normal(k, shape, f32) * (fan_in ** -0.5)

    def gain(k, shape):
        return 1.0 + 0.02 * jax.random.normal(k, shape, f32)

    def bias(k, shape):
        return 0.01 * jax.random.normal(k, shape, f32)

    a0 = jax.random.uniform(ks[12], (L, LRU_WIDTH), f32, 0.9, 0.999)
    s0 = a0 ** (1.0 / RG_C)
    lru_lambda = jnp.log(s0) - jnp.log1p(-s0)
    return {
        'x': jax.random.normal(ks[0], (BATCH, SEQ, D_MODEL), f32),
        'mem': jax.random.normal(ks[1], (BATCH, MEM_LEN, D_MODEL), f32),
        'positions': jnp.broadcast_to(jnp.arange(SEQ, dtype=jnp.int32), (BATCH, SEQ)),
        'mix_norm_g': gain(ks[2], (L, D_MODEL)),
        'w_in': nrm(ks[3], (L, D_MODEL, IN_COLS), D_MODEL),
        'ret_norm_g': gain(ks[4], (L, RET_WIDTH)),
        'lru_conv_w': nrm(ks[5], (L, CONV_WIDTH, LRU_WIDTH), CONV_WIDTH),
        'lru_conv_b': bias(ks[6], (L, LRU_WIDTH)),
        'lru_w_a': nrm(ks[7], (L, LRU_BLOCKS, LRU_BLOCK_DIM, LRU_BLOCK_DIM), LRU_BLOCK_DIM),
        'lru_b_a': bias(ks[8], (L, LRU_WIDTH)),
        'lru_w_i': nrm(ks[9], (L, LRU_BLOCKS, LRU_BLOCK_DIM, LRU_BLOCK_DIM), LRU_BLOCK_DIM),
        'lru_b_i': bias(ks[10], (L, LRU_WIDTH)),
        'lru_lambda': lru_lambda,
        'lru_norm_g': gain(ks[11], (L, LRU_WIDTH)),
        'w_out': nrm(ks[13], (L, D_MODEL, D_MODEL), D_MODEL),
        'xattn_norm_g': gain(ks[14], (L, D_MODEL)),
        'mem_norm_g': gain(ks[15], (L, D_MODEL)),
        'xattn_wq': nrm(ks[16], (L, D_MODEL, D_MODEL), D_MODEL),
        'xattn_wk': nrm(ks[17], (L, D_MODEL, D_MODEL), D_MODEL),
        'xattn_wv': nrm(ks[18], (L, D_MODEL, D_MODEL), D_MODEL),
        'xattn_wo': nrm(ks[19], (L, D_MODEL, D_MODEL), D_MODEL),
        'moe_norm_g': gain(ks[20], (L, D_MODEL)),
        'router_group_w': nrm(ks[21], (L, D_MODEL, N_GROUPS), D_MODEL),
        'router_group_b': bias(ks[22], (L, N_GROUPS)),
        'router_expert_w': nrm(ks[23], (L, D_MODEL, N_EXPERTS), D_MODEL),
        'router_expert_b': bias(ks[24], (L, N_EXPERTS)),
        'expert_w_gate': nrm(ks[25], (L, N_EXPERTS, D_MODEL, D_EXPERT), D_MODEL),
        'expert_w_up': nrm(ks[26], (L, N_EXPERTS, D_MODEL, D_EXPERT), D_MODEL),
        'expert_w_down': nrm(ks[27], (L, N_EXPERTS, D_EXPERT, D_MODEL), D_EXPERT),
        'final_norm_g': gain(ks[28], (D_MODEL,)),
    }


def reference(x, mem, positions, mix_norm_g, w_in, ret_norm_g, lru_conv_w, lru_conv_b,
              lru_w_a, lru_b_a, lru_w_i, lru_b_i, lru_lambda, lru_norm_g, w_out,
              xattn_norm_g, mem_norm_g, xattn_wq, xattn_wk, xattn_wv, xattn_wo,
              moe_norm_g, router_group_w, router_group_b, router_expert_w, router_expert_b,
              expert_w_gate, expert_w_up, expert_w_down, final_norm_g):
    cos, sin = rope_tables(positions)
    splits = [RET_WIDTH, 2 * RET_WIDTH, 3 * RET_WIDTH, 4 * RET_WIDTH, 4 * RET_WIDTH + LRU_WIDTH]
    for l in range(DEPTH):
        h = rms_norm(x, mix_norm_g[l])
        proj = h @ w_in[l]
        q, k, v, g, xb, gb = jnp.split(proj, splits, axis=-1)
        ret = retention_group(q, k, v, g, cos, sin, ret_norm_g[l])
        lru = rg_lru_group(xb, gb, lru_conv_w[l], lru_conv_b[l], lru_w_a[l], lru_b_a[l],
                           lru_w_i[l], lru_b_i[l], lru_lambda[l], lru_norm_g[l])
        x = x + jnp.concatenate([ret, lru], axis=-1) @ w_out[l]
        h = rms_norm(x, xattn_norm_g[l])
        memn = rms_norm(mem, mem_norm_g[l])
        x = x + memory_cross_attention(h, memn, xattn_wq[l], xattn_wk[l], xattn_wv[l], xattn_wo[l])
        h = rms_norm(x, moe_norm_g[l])
        x = x + hierarchical_moe(h, router_group_w[l], router_group_b[l], router_expert_w[l],
                                 router_expert_b[l], expert_w_gate[l], expert_w_up[l], expert_w_down[l])
    return rms_norm(x, final_norm_g)
```

```python
import contextlib
from contextlib import ExitStack
import numpy as np
import concourse.bass as bass
import concourse.mybir as mybir
from concourse.bass_utils import run_bass_kernel_spmd

F32 = mybir.dt.float32
BF16 = mybir.dt.bfloat16
I32 = mybir.dt.int32
ALU = mybir.AluOpType
AF = mybir.ActivationFunctionType
AX = mybir.AxisListType

NCORES = 8
D = 4096
KC = 32
SLAB = 1024
NT = 8
NSLAB = 8
NEXP = 32
DE = 1024

C_INVF = 0
C_DEC = 128
C_XI = C_DEC + 8 * 128
C_ZETA = C_XI + 8
C_CD = C_ZETA + 8
C_WGT = C_CD + 8
C_ID = C_WGT + 8 * 56
NCST = C_ID + 128
P_CW = 0
P_CB = 64
P_BA = 80
P_BI = 96
P_LAM = 112
P_LG = 128
NPP = 144


class Buf:
    __slots__ = ("name", "writer", "readers")

    def __init__(self, name=""):
        self.name = name
        self.writer = None
        self.readers = []


class _Eng:
    def __init__(self, name, eng, sem):
        self.name = name
        self.eng = eng
        self.sem = sem
        self.cnt = 0
        self.seen = {}


class Sched:
    def __init__(self, nc, stack, n_dma_sems=32):
        self.nc = nc
        self.E = {}
        for name, eng in (("pe", nc.tensor), ("act", nc.scalar), ("dve", nc.vector),
                          ("pool", nc.gpsimd), ("sp", nc.sync)):
            sem = stack.enter_context(nc.semaphore("s_" + name))
            self.E[name] = _Eng(name, eng, sem)
        self.dsems = []
        for i in range(n_dma_sems):
            sem = stack.enter_context(nc.semaphore("s_dma%d" % i))
            self.dsems.append([sem, 0])
        self.drr = 0

    def buf(self, name=""):
        return Buf(name)

    def bufs(self, n, name=""):
        return [Buf(name + str(i)) for i in range(n)]

    def _wait(self, E, deps):
        need = {}
        for (sem, val, ename) in deps:
            if ename == E.name and ename == "pe":
                continue
            k = id(sem)
            if E.seen.get(k, 0) >= val:
                continue
            if k not in need or need[k][1] < val:
                need[k] = (sem, val)
        for k, (sem, val) in need.items():
            E.eng.wait_ge(sem, val)
            E.seen[k] = val

    @staticmethod
    def _deps(reads, writes):
        deps = []
        for b in reads:
            if b.writer is not None:
                deps.append(b.writer)
        for b in writes:
            if b.writer is not None:
                deps.append(b.writer)
            deps.extend(b.readers)
        return deps

    @staticmethod
    def _commit(tok, reads, writes):
        for b in writes:
            b.writer = tok
            b.readers = []
        for b in reads:
            b.readers.append(tok)

    def op(self, engname, fn, reads=(), writes=()):
        E = self.E[engname]
        self._wait(E, self._deps(reads, writes))
        ins = fn(E.eng)
        E.cnt += 1
        ins.then_inc(E.sem, 1)
        self._commit((E.sem, E.cnt, engname), reads, writes)
        return ins

    def dma(self, qname, out, in_, reads=(), writes=(), **kw):
        E = self.E[qname]
        slot = self.dsems[self.drr]
        self.drr = (self.drr + 1) % len(self.dsems)
        deps = self._deps(reads, writes)
        if slot[1] > 0:
            deps.append((slot[0], slot[1], "dma"))
        self._wait(E, deps)
        ins = E.eng.dma_start(out=out, in_=in_, **kw)
        slot[1] += 16
        ins.then_inc(slot[0], 16)
        self._commit((slot[0], slot[1], "dma"), reads, writes)
        return ins

    def barrier(self):
        toks = [(E.sem, E.cnt, n) for n, E in self.E.items() if E.cnt > 0]
        toks += [(d[0], d[1], "dma") for d in self.dsems if d[1] > 0]
        for E in self.E.values():
            self._wait(E, [t for t in toks if t[2] != E.name])

    def finish(self, bufs):
        deps = []
        for b in bufs:
            if b.writer is not None:
                deps.append(b.writer)
            deps.extend(b.readers)
        self._wait(self.E["sp"], deps)


def make_consts():
    lg = np.log1p(-np.exp2(-5.0 - np.arange(8, dtype=np.float64)))
    cst = np.zeros((128, NCST), np.float64)
    invf = np.float32(10000.0) ** (-(np.arange(0, 256, 2, dtype=np.float32)) / np.float32(256.0))
    cst[:, C_INVF:C_INVF + 128] = invf[None, :].astype(np.float64)
    idx = np.arange(128, dtype=np.float64)
    for h in range(8):
        diff = idx[None, :] - idx[:, None]
        dec = np.where(diff >= 0, np.exp(lg[h] * np.maximum(diff, 0.0)), 0.0) / 16.0
        cst[:, C_DEC + h * 128:C_DEC + (h + 1) * 128] = dec
        cst[:, C_XI + h] = np.exp(lg[h] * (idx + 1.0))
        cst[:, C_ZETA + h] = np.exp(lg[h] * (127.0 - idx)) / 16.0
        cst[:, C_CD + h] = np.exp(lg[h] * 128.0)
        for j in range(56):
            cst[:, C_WGT + h * 56 + j] = np.exp(lg[h] * (7167.0 - (128.0 * j + idx))) / 16.0
    cst[:, C_ID:C_ID + 128] = np.eye(128)
    return cst.astype(np.float32)


def build_program(stop_after=None, skipA=False):
    nc = bass.Bass("TRN2", target_bir_lowering=False)

    def din(name, shape, dt=F32):
        return nc.dram_tensor(name, list(shape), dt, kind="ExternalInput").ap()

    xs = din("xs", [NSLAB * SLAB, D])
    w_outs = None
    pos_pm = din("pos_pm", [128, 64], I32)
    vmask_d = din("vmask", [128, 8])
    mem_d = din("mem", [256, D])
    cst_d = din("cst", [128, NCST])
    pp_d = din("pp", [128, NPP])
    mix_g = din("mix_g", [1, D]); gn_g = din("gn_g", [1, 2048]); xat_g = din("xat_g", [1, D])
    mem_g = din("mem_g", [1, D]); moe_g = din("moe_g", [1, D]); fin_g = din("fin_g", [1, D])
    rb_d = din("rb", [1, 36])
    wr_d = din("wr", [128, KC * 36])
    w_in = din("w_in", [D, 12288] if not skipA else [1, 1])
    w_out = din("w_out", [D, D]); wq = din("wq", [D, D]); wk = din("wk", [D, D])
    wv = din("wv", [D, D]); wo = din("wo", [D, D])
    w_a = din("w_a", [8, 256, 256]); w_i = din("w_i", [8, 256, 256])
    big = stop_after is None
    nexp = NEXP if big else 2
    wg = din("wg", [nexp, D, DE]); wu = din("wu", [nexp, D, DE]); wd = din("wd", [nexp, DE, D])
    out_d = nc.dram_tensor("out", [SLAB, D], F32, kind="ExternalOutput").ap()
    dbg = stop_after is not None
    kind_s = "ExternalOutput" if dbg else "Internal"
    x1_d = nc.dram_tensor("x1_d", [SLAB, D], F32, kind=kind_s).ap()
    x2_d = nc.dram_tensor("x2_d", [SLAB, D], F32, kind=kind_s).ap()
    x3_d = nc.dram_tensor("x3_d", [SLAB, D], F32, kind="Internal").ap()
    mixT_d = nc.dram_tensor("mixT_d", [D, SLAB], BF16, kind="Internal").ap()
    yT_d = nc.dram_tensor("yT_d", [2048, SLAB], F32, kind="Internal").ap()

    with ExitStack() as st:
        S = Sched(nc, st)

        def sb(name, shape, dt, stack=st):
            return stack.enter_context(nc.sbuf_tensor("sb_" + name, list(shape), dt))

        def ps(name, shape, dt):
            return st.enter_context(nc.psum_tensor("ps_" + name, list(shape), dt))

        cst = sb("cst", [128, NCST], F32); b_cst = S.buf()
        pp = sb("pp", [128, NPP], F32); b_pp = S.buf()
        vmask = sb("vmaskt", [128, 8], F32); b_vm = S.buf()
        posi = sb("posi", [128, 64], I32); posf = sb("posf", [128, 64], F32); b_pos = S.buf()
        identb = sb("identb", [128, 128], BF16); b_idb = S.buf()
        onesf = sb("onesf", [128, 128], F32); b_ones = S.buf()
        AT = sb("AT", [128, KC, SLAB], BF16); b_AT = S.buf()
        NQ = 3
        ring = sb("ring", [128, NQ * 8192], BF16); b_ring = S.bufs(NQ)
        ring_i = [0]
        pmm = ps("pmm", [128, 4, 512], F32); b_pmm = S.bufs(4); pmm_i = [0]
        ptr = ps("ptr", [128, 1024], BF16); b_ptr = S.buf()
        ptf = ps("ptf", [128, 512], F32); b_ptf = S.buf()
        pms = ps("pms", [128, 2, 512], F32); b_pms = S.bufs(4)
        sdesc = sb("sdesc", [128, 16], F32); b_sd = S.buf()

        S.dma("sp", cst[:], cst_d, writes=[b_cst])
        S.dma("sp", pp[:], pp_d, writes=[b_pp])
        S.dma("sp", vmask[:], vmask_d, writes=[b_vm])
        S.dma("sp", posi[:], pos_pm, writes=[b_pos])
        S.op("dve", lambda e: e.tensor_copy(out=posf[:], in_=posi[:]), reads=[b_pos], writes=[b_pos])
        S.op("dve", lambda e: e.tensor_copy(out=identb[:], in_=cst[:, C_ID:C_ID + 128]), reads=[b_cst], writes=[b_idb])
        S.op("dve", lambda e: e.memset(onesf[:], 1.0), writes=[b_ones])
        identf = cst[:, C_ID:C_ID + 128]

        def next_pmm():
            i = pmm_i[0]
            pmm_i[0] = (i + 1) % 4
            return i

        def load_wslab(W2d, kc, ncols):
            assert kc * ncols == 8192
            q = ring_i[0]
            ring_i[0] = (q + 1) % NQ
            view = ring[:, q * 8192:(q + 1) * 8192].rearrange("p (k n) -> p k n", k=kc)
            S.dma("pool", view, W2d.rearrange("(k p) n -> p k n", p=128), writes=[b_ring[q]])
            return view, b_ring[q]

        def run_jobs(jobs, depth=2):
            handles = {}
            n = len(jobs)
            for i in range(min(depth, n)):
                handles[i] = jobs[i][0]()
            for i in range(n):
                jobs[i][1](handles.pop(i))
                if i + depth < n:
                    handles[i + depth] = jobs[i + depth][0]()

        def mm_tok(w, bw, A, bA, ntiles, kc, evac, ncols=256):
            for t in range(ntiles):
                pb = next_pmm()
                pv = pmm[:, pb, 0:ncols]
                for k in range(kc):
                    S.op("pe", lambda e, k=k, t=t, pv=pv: e.matmul(pv, lhsT=A[:, k, t * 128:(t + 1) * 128], rhs=w[:, k, 0:ncols],
                                                                  start=(k == 0), stop=(k == kc - 1)),
                         reads=[bA, bw], writes=[b_pmm[pb]])
                evac(t, pv, b_pmm[pb])

        def mm_feat(w, bw, A, bA, ntok, kc, evac, ncols=256):
            for cc in range(ncols // 128):
                for hf in range(max(1, ntok // 512)):
                    n = min(512, ntok)
                    pb = next_pmm()
                    pv = pmm[:, pb, 0:n]
                    for k in range(kc):
                        S.op("pe", lambda e, k=k, cc=cc, hf=hf, pv=pv, n=n: e.matmul(
                            pv, lhsT=w[:, k, cc * 128:(cc + 1) * 128], rhs=A[:, k, hf * 512:hf * 512 + n],
                            start=(k == 0), stop=(k == kc - 1)),
                             reads=[bA, bw], writes=[b_pmm[pb]])
                    evac(cc, hf, pv, b_pmm[pb])

        def rms_rstd(ss_ap, n, eps, out_ap, b):
            S.op("dve", lambda e: e.tensor_scalar(out=out_ap, in0=ss_ap, scalar1=1.0 / n, scalar2=eps,
                                                  op0=ALU.mult, op1=ALU.add), reads=[b], writes=[b])
            S.op("act", lambda e: e.activation(out=out_ap, in_=out_ap, func=AF.Sqrt), reads=[b], writes=[b])
            S.op("dve", lambda e: e.reciprocal(out=out_ap, in_=out_ap), reads=[b], writes=[b])

        tr_cnt = [0]

        def norm_transpose(ph, src_rows, ntiles, gvec, AT_dst, b_dst, col0, f32_out=None):
            with ExitStack() as ls:
                gbc = sb(ph + "gbc", [128, D], F32, ls); b_g = S.buf()
                xt = sb(ph + "xt", [128, D], F32, ls); b_xt = S.buf()
                hb = sb(ph + "hb", [128, D // 2], BF16, ls); b_hb = S.buf()
                ss = sb(ph + "ss", [128, 4], F32, ls); b_ss = S.buf()
                S.dma("sp", gbc[:], gvec.partition_broadcast(128), writes=[b_g])
                HD = D // 2
                for t in range(ntiles):
                    S.dma("sp", xt[:], src_rows[t * 128:(t + 1) * 128, :], writes=[b_xt])
                    for hh in range(2):
                        S.op("act", lambda e, hh=hh: e.activation(out=hb[:], in_=xt[:, hh * HD:(hh + 1) * HD], func=AF.Square), reads=[b_xt], writes=[b_hb])
                        S.op("dve", lambda e, hh=hh: e.reduce_sum(out=ss[:, 2 + hh:3 + hh], in_=hb[:], axis=AX.X), reads=[b_hb], writes=[b_ss])
                    S.op("dve", lambda e: e.tensor_tensor(out=ss[:, 0:1], in0=ss[:, 2:3], in1=ss[:, 3:4], op=ALU.add), reads=[b_ss], writes=[b_ss])
                    rms_rstd(ss[:, 0:1], float(D), 1e-6, ss[:, 1:2], b_ss)
                    for hh in range(2):
                        S.op("dve", lambda e, hh=hh: e.scalar_tensor_tensor(out=hb[:], in0=xt[:, hh * HD:(hh + 1) * HD], scalar=ss[:, 1:2],
                                                                            in1=gbc[:, hh * HD:(hh + 1) * HD], op0=ALU.mult, op1=ALU.mult),
                             reads=[b_xt, b_ss, b_g], writes=[b_hb])
                        for g8 in range(2):
                            for j in range(8):
                                k = g8 * 8 + j
                                S.op("pe", lambda e, k=k, j=j: e.transpose(out=ptr[:, j * 128:(j + 1) * 128],
                                                                           in_=hb[:, k * 128:(k + 1) * 128], identity=identb[:]),
                                     reads=[b_hb, b_idb], writes=[b_ptr])
                            eng = "act" if (tr_cnt[0] % 2 == 0) else "dve"
                            tr_cnt[0] += 1
                            k0 = hh * 16 + g8 * 8
                            dst = AT_dst[:, k0:k0 + 8, col0 + t * 128:col0 + (t + 1) * 128]
                            src = ptr[:, :].rearrange("p (k n) -> p k n", k=8)
                            if eng == "act":
                                S.op("act", lambda e, dst=dst, src=src: e.activation(out=dst, in_=src, func=AF.Copy),
                                     reads=[b_ptr], writes=[b_dst])
                            else:
                                S.op("dve", lambda e, dst=dst, src=src: e.tensor_copy(out=dst, in_=src),
                                     reads=[b_ptr], writes=[b_dst])
                S.barrier()

        with ExitStack() as pa:
          if skipA:
            b_mixT = S.bufs(32, "mixT")
          else:
              Racc = sb("Racc", [128, 16, 256], F32, pa); b_R = S.bufs(16)
              for i in range(16):
                  S.op("dve", lambda e, i=i: e.memset(Racc[:, i, :], 0.0), writes=[b_R[i]])
              lstate = sb("lstate", [128, 16], F32, pa); b_ls = S.bufs(16)
              halo = sb("halo", [128, 16, 4], F32, pa); b_halo = S.bufs(16)
              S.op("dve", lambda e: e.memset(lstate[:], 0.0), writes=b_ls)
              S.op("dve", lambda e: e.memset(halo[:], 0.0), writes=b_halo)
              wab = wib = b_wab = None
              lsc = sb("lsc", [128, 48], F32, pa); b_lsc = S.buf()
              S.op("act", lambda e: e.activation(out=lsc[:, 0:16], in_=pp[:, P_LAM:P_LAM + 16], func=AF.Exp, scale=-1.0),
                   reads=[b_pp], writes=[b_lsc])
              S.op("act", lambda e: e.activation(out=lsc[:, 0:16], in_=lsc[:, 0:16], func=AF.Ln, bias=1.0),
                   reads=[b_lsc], writes=[b_lsc])
              S.op("dve", lambda e: e.tensor_scalar(out=lsc[:, 16:32], in0=lsc[:, 0:16], scalar1=-8.0, scalar2=None, op0=ALU.mult),
                   reads=[b_lsc], writes=[b_lsc])
              S.op("dve", lambda e: e.tensor_scalar(out=lsc[:, 32:48], in0=lsc[:, 0:16], scalar1=-16.0, scalar2=None, op0=ALU.mult),
                   reads=[b_lsc], writes=[b_lsc])
              cosT = sinT = b_cs = rtmp = b_rt = rki = kt = b_kt = vt = b_vt = rpt = b_rpt = None
              ret_n = [0]

              def alloc_ret(stack):
                  nonlocal cosT, sinT, b_cs, rtmp, b_rt, rki, kt, b_kt, vt, b_vt, rpt, b_rpt
                  n = "%d" % ret_n[0]; ret_n[0] += 1
                  cosT = sb("cosT" + n, [128, NT, 128], F32, stack); sinT = sb("sinT" + n, [128, NT, 128], F32, stack); b_cs = S.buf()
                  rtmp = sb("rtmp" + n, [128, 3, 128], F32, stack); b_rt = S.buf()
                  rki = sb("rki" + n, [128, 128], I32, stack)
                  kt = sb("kt" + n, [128, NT, 256], BF16, stack); b_kt = S.buf()
                  vt = sb("vt" + n, [128, NT, 256], BF16, stack); b_vt = S.buf()
                  rpt = sb("rpt" + n, [128, 2, 256], F32, stack); b_rpt = S.buf()
              LB = {}
              lru_n = [0]

              def alloc_lru(stack):
                  nonlocal wab, wib, b_wab
                  n = lru_n[0]; lru_n[0] += 1
                  wab = sb("wab%d" % n, [128, 8, 2, 256], BF16, stack); wib = sb("wib%d" % n, [128, 8, 2, 256], BF16, stack); b_wab = S.buf()
                  S.dma("pool", wab[:], w_a.rearrange("b (c p) d -> p b c d", p=128), writes=[b_wab])
                  S.dma("pool", wib[:], w_i.rearrange("b (c p) d -> p b c d", p=128), writes=[b_wab])
                  LB["xb"] = sb("xb%d" % n, [128, 2, 1028], F32, stack); LB["b_xb"] = S.bufs(2)
                  LB["xc"] = sb("xc%d" % n, [128, 2, 1024], F32, stack); LB["b_xc"] = S.bufs(2)
                  LB["xcb"] = sb("xcb%d" % n, [128, 2, 1024], BF16, stack); LB["b_xcb"] = S.bufs(2)
                  for nm in ("lr", "li", "l2", "lh"):
                      LB[nm] = sb(nm + "%d" % n, [128, 1024], F32, stack); LB["b_" + nm] = S.buf()

              def rope_tables(j):
                  for t in range(NT):
                      pcol = posf[:, j * NT + t:j * NT + t + 1]
                      ang = rtmp[:, 0, :]; y = rtmp[:, 1, :]; kf = rtmp[:, 2, :]
                      S.op("dve", lambda e, pcol=pcol: e.tensor_scalar(out=ang, in0=cst[:, C_INVF:C_INVF + 128], scalar1=pcol,
                                                                       scalar2=None, op0=ALU.mult), reads=[b_cst, b_pos], writes=[b_rt])
                      S.op("dve", lambda e: e.tensor_scalar(out=y, in0=ang, scalar1=float(1.0 / (2 * np.pi)), scalar2=0.5,
                                                            op0=ALU.mult, op1=ALU.add), reads=[b_rt], writes=[b_rt])
                      S.op("dve", lambda e: e.tensor_copy(out=rki[:], in_=y), reads=[b_rt], writes=[b_rt])
                      S.op("dve", lambda e: e.tensor_copy(out=kf, in_=rki[:]), reads=[b_rt], writes=[b_rt])
                      S.op("dve", lambda e: e.scalar_tensor_tensor(out=y, in0=kf, scalar=-6.28125, in1=ang, op0=ALU.mult, op1=ALU.add),
                           reads=[b_rt], writes=[b_rt])
                      S.op("dve", lambda e: e.scalar_tensor_tensor(out=y, in0=kf, scalar=-0.0019353071795864769, in1=y,
                                                                   op0=ALU.mult, op1=ALU.add), reads=[b_rt], writes=[b_rt])
                      S.op("dve", lambda e: e.tensor_scalar(out=kf, in0=y, scalar1=-float(np.pi), scalar2=float(2 * np.pi),
                                                            op0=ALU.is_lt, op1=ALU.mult), reads=[b_rt], writes=[b_rt])
                      S.op("dve", lambda e: e.tensor_tensor(out=y, in0=y, in1=kf, op=ALU.add), reads=[b_rt], writes=[b_rt])
                      S.op("dve", lambda e: e.tensor_scalar(out=kf, in0=y, scalar1=float(np.pi), scalar2=-float(2 * np.pi),
                                                            op0=ALU.is_gt, op1=ALU.mult), reads=[b_rt], writes=[b_rt])
                      S.op("dve", lambda e: e.tensor_tensor(out=y, in0=y, in1=kf, op=ALU.add), reads=[b_rt], writes=[b_rt])
                      S.op("act", lambda e, t=t: e.activation(out=sinT[:, t, :], in_=y, func=AF.Sin), reads=[b_rt], writes=[b_cs])
                      S.op("act", lambda e: e.activation(out=kf, in_=y, func=AF.Abs), reads=[b_rt], writes=[b_rt])
                      S.op("act", lambda e, t=t: e.activation(out=cosT[:, t, :], in_=kf, func=AF.Sin, scale=-1.0, bias=float(np.pi / 2)),
                           reads=[b_rt], writes=[b_cs])

              def rope_evac(dst, bdst):
                  def f(t, pv, bp):
                      t1 = pv[:, 0:128]; t2 = pv[:, 128:256]
                      a = rpt[:, 0, 0:128]; b = rpt[:, 0, 128:256]; c = rpt[:, 1, 0:128]; d = rpt[:, 1, 128:256]
                      S.op("dve", lambda e: e.tensor_tensor(out=a, in0=t1, in1=cosT[:, t, :], op=ALU.mult), reads=[bp, b_cs], writes=[b_rpt])
                      S.op("dve", lambda e: e.tensor_tensor(out=b, in0=t2, in1=sinT[:, t, :], op=ALU.mult), reads=[bp, b_cs], writes=[b_rpt])
                      S.op("dve", lambda e: e.tensor_tensor(out=c, in0=t1, in1=sinT[:, t, :], op=ALU.mult), reads=[bp, b_cs], writes=[b_rpt])
                      S.op("dve", lambda e: e.tensor_tensor(out=d, in0=t2, in1=cosT[:, t, :], op=ALU.mult), reads=[bp, b_cs], writes=[b_rpt])
                      S.op("dve", lambda e: e.tensor_tensor(out=dst[:, t, 0:128], in0=a, in1=b, op=ALU.subtract), reads=[b_rpt], writes=[bdst])
                      S.op("dve", lambda e: e.tensor_tensor(out=dst[:, t, 128:256], in0=c, in1=d, op=ALU.add), reads=[b_rpt], writes=[bdst])
                  return f

              def copy_evac(dst, bdst, eng="act"):
                  def f(t, pv, bp):
                      if eng == "act":
                          S.op("act", lambda e: e.activation(out=dst[:, t, :], in_=pv, func=AF.Copy), reads=[bp], writes=[bdst])
                      else:
                          S.op("dve", lambda e: e.tensor_copy(out=dst[:, t, :], in_=pv), reads=[bp], writes=[bdst])
                  return f

              def lru_block(j, blk, own, gate_handles=None):
                  xb, xc, xcb, lr, li, l2, lh = (LB[n_] for n_ in ("xb", "xc", "xcb", "lr", "li", "l2", "lh"))
                  b_xb, b_xc, b_xcb, b_lr, b_li, b_l2, b_lh = (LB["b_" + n_] for n_ in ("xb", "xc", "xcb", "lr", "li", "l2", "lh"))
                  for cc in range(2):
                      ch = blk * 2 + cc
                      S.op("dve", lambda e, cc=cc, ch=ch: e.tensor_copy(out=xb[:, cc, 0:4], in_=halo[:, ch, :]),
                           reads=[b_halo[ch]], writes=[b_xb[cc]])
                      cw = lambda jj, ch=ch: pp[:, P_CW + ch * 4 + jj:P_CW + ch * 4 + jj + 1]
                      S.op("dve", lambda e, cc=cc, ch=ch: e.tensor_scalar(out=xc[:, cc, :], in0=xb[:, cc, 4:1028], scalar1=cw(3),
                                                                          scalar2=pp[:, P_CB + ch:P_CB + ch + 1], op0=ALU.mult, op1=ALU.add),
                           reads=[b_xb[cc], b_pp], writes=[b_xc[cc]])
                      for jj in range(3):
                          S.op("dve", lambda e, cc=cc, jj=jj: e.scalar_tensor_tensor(out=xc[:, cc, :], in0=xb[:, cc, 1 + jj:1025 + jj],
                                                                                     scalar=cw(jj), in1=xc[:, cc, :], op0=ALU.mult, op1=ALU.add),
                               reads=[b_xb[cc], b_pp, b_xc[cc]], writes=[b_xc[cc]])
                      S.op("dve", lambda e, cc=cc, ch=ch: e.tensor_copy(out=halo[:, ch, :], in_=xb[:, cc, 1024:1028]),
                           reads=[b_xb[cc]], writes=[b_halo[ch]])
                      S.op("act", lambda e, cc=cc: e.activation(out=xcb[:, cc, :], in_=xc[:, cc, :], func=AF.Copy),
                           reads=[b_xc[cc]], writes=[b_xcb[cc]])
                  for dc in range(2):
                      ch = blk * 2 + dc
                      for (wmat, bias_off, dst, bd) in ((wab, P_BA, lr, b_lr), (wib, P_BI, li, b_li)):
                          for hf in range(2):
                              pb = next_pmm(); pv = pmm[:, pb, :]
                              for c2 in range(2):
                                  S.op("pe", lambda e, c2=c2, hf=hf, pv=pv, wmat=wmat, dc=dc: e.matmul(
                                      pv, lhsT=wmat[:, blk, c2, dc * 128:(dc + 1) * 128], rhs=xcb[:, c2, hf * 512:(hf + 1) * 512],
                                      start=(c2 == 0), stop=(c2 == 1)), reads=[b_wab, b_xcb[0], b_xcb[1]], writes=[b_pmm[pb]])
                              S.op("act", lambda e, hf=hf, pv=pv, dst=dst, bias_off=bias_off, ch=ch: e.activation(
                                  out=dst[:, hf * 512:(hf + 1) * 512], in_=pv, func=AF.Sigmoid,
                                  bias=pp[:, bias_off + ch:bias_off + ch + 1]), reads=[b_pmm[pb], b_pp], writes=[bd])
                      S.op("act", lambda e, ch=ch: e.activation(out=l2[:], in_=lr[:], func=AF.Exp, scale=lsc[:, 32 + ch:33 + ch]),
                           reads=[b_lr, b_lsc], writes=[b_l2])
                      S.op("act", lambda e, ch=ch: e.activation(out=lr[:], in_=lr[:], func=AF.Exp, scale=lsc[:, 16 + ch:17 + ch]),
                           reads=[b_lr, b_lsc], writes=[b_lr])
                      S.op("dve", lambda e: e.tensor_scalar(out=l2[:], in0=l2[:], scalar1=-1.0, scalar2=1.0, op0=ALU.mult, op1=ALU.add),
                           reads=[b_l2], writes=[b_l2])
                      S.op("dve", lambda e: e.tensor_scalar(out=l2[:], in0=l2[:], scalar1=0.0, scalar2=None, op0=ALU.max),
                           reads=[b_l2], writes=[b_l2])
                      S.op("act", lambda e: e.activation(out=l2[:], in_=l2[:], func=AF.Sqrt), reads=[b_l2], writes=[b_l2])
                      S.op("dve", lambda e: e.scalar_tensor_tensor(out=li[:], in0=l2[:], scalar=vmask[:, j:j + 1], in1=li[:],
                                                                   op0=ALU.mult, op1=ALU.mult), reads=[b_l2, b_li, b_vm], writes=[b_li])
                      S.op("dve", lambda e, dc=dc: e.tensor_tensor(out=l2[:], in0=li[:], in1=xc[:, dc, :], op=ALU.mult),
                           reads=[b_li, b_xc[dc]], writes=[b_l2])
                      S.op("dve", lambda e, ch=ch: e.tensor_tensor_scan(out=lh[:], data0=lr[:], data1=l2[:], initial=lstate[:, ch:ch + 1],
                                                                        op0=ALU.mult, op1=ALU.add),
                           reads=[b_lr, b_l2, b_ls[ch]], writes=[b_lh])
                      S.op("dve", lambda e, ch=ch: e.tensor_copy(out=lstate[:, ch:ch + 1], in_=lh[:, 1023:1024]),
                           reads=[b_lh], writes=[b_ls[ch]])
                      if own:
                          own_lru_out(ch, dc)

              own_ctx = {}

              def own_lru_out(ch, dc):
                  xb, xc, xcb, lr, li, l2, lh = (LB[n_] for n_ in ("xb", "xc", "xcb", "lr", "li", "l2", "lh"))
                  b_xb, b_xc, b_xcb, b_lr, b_li, b_l2, b_lh = (LB["b_" + n_] for n_ in ("xb", "xc", "xcb", "lr", "li", "l2", "lh"))
                  gt = own_ctx["gt"]; b_gt = own_ctx["b_gt"]; ssum = own_ctx["ssum"]; b_ssum = own_ctx["b_ssum"]
                  g = gt[:, dc, :]
                  u = li[:]
                  S.op("dve", lambda e: e.tensor_tensor(out=u, in0=g, in1=g, op=ALU.mult), reads=[b_gt[dc]], writes=[b_li])
                  S.op("dve", lambda e: e.tensor_scalar(out=u, in0=u, scalar1=0.044715, scalar2=1.0, op0=ALU.mult, op1=ALU.add),
                       reads=[b_li], writes=[b_li])
                  S.op("dve", lambda e: e.tensor_tensor(out=u, in0=u, in1=g, op=ALU.mult), reads=[b_li, b_gt[dc]], writes=[b_li])
                  S.op("act", lambda e: e.activation(out=u, in_=u, func=AF.Sigmoid, scale=1.5957691216057308), reads=[b_li], writes=[b_li])
                  S.op("dve", lambda e: e.tensor_tensor(out=u, in0=u, in1=g, op=ALU.mult), reads=[b_li, b_gt[dc]], writes=[b_li])
                  S.op("dve", lambda e: e.tensor_tensor(out=lh[:], in0=lh[:], in1=u, op=ALU.mult), reads=[b_li, b_lh], writes=[b_lh])
                  S.dma("sp", yT_d[ch * 128:(ch + 1) * 128, :], lh[:], reads=[b_lh], writes=[own_ctx["b_yT"][ch]])
                  S.op("act", lambda e: e.activation(out=l2[:], in_=lh[:], func=AF.Square), reads=[b_lh], writes=[b_l2])
                  for hf in range(2):
                      S.op("pe", lambda e, hf=hf: e.matmul(ptf[:, :], lhsT=onesf[:], rhs=l2[:, hf * 512:(hf + 1) * 512], start=True, stop=True),
                           reads=[b_ones, b_l2], writes=[b_ptf])
                      S.op("dve", lambda e, hf=hf: e.tensor_tensor(out=ssum[:, hf * 512:(hf + 1) * 512], in0=ssum[:, hf * 512:(hf + 1) * 512],
                                                                   in1=ptf[:, :], op=ALU.add), reads=[b_ptf, b_ssum], writes=[b_ssum])

              def xb_evac(cc_base):
                  def f(cc, hf, pv, bp):
                      xb = LB["xb"]; b_xb = LB["b_xb"]
                      S.op("act", lambda e: e.activation(out=xb[:, cc, 4 + hf * 512:4 + (hf + 1) * 512], in_=pv, func=AF.Copy),
                           reads=[bp], writes=[b_xb[cc]])
                  return f

              for j in range(NSLAB - 1):
                  norm_transpose("pa%d" % j, xs[j * SLAB:(j + 1) * SLAB, :], NT, mix_g, AT, b_AT, 0)
                  rsc_ = ExitStack()
                  alloc_ret(rsc_)
                  rope_tables(j)
                  jobs = []
                  for h in range(8):
                      def mk(h):
                          def ld_k():
                              return load_wslab(w_in[:, 2048 + h * 256:2048 + (h + 1) * 256], KC, 256)

                          def cp_k(hd):
                              mm_tok(hd[0], hd[1], AT, b_AT, NT, KC, rope_evac(kt, b_kt))
                              for t in range(NT):
                                  S.op("dve", lambda e, t=t: e.tensor_scalar(out=kt[:, t, :], in0=kt[:, t, :],
                                                                             scalar1=cst[:, C_WGT + h * 56 + j * NT + t:C_WGT + h * 56 + j * NT + t + 1],
                                                                             scalar2=None, op0=ALU.mult), reads=[b_kt, b_cst], writes=[b_kt])

                          def ld_v():
                              return load_wslab(w_in[:, 4096 + h * 256:4096 + (h + 1) * 256], KC, 256)

                          def cp_v(hd):
                              mm_tok(hd[0], hd[1], AT, b_AT, NT, KC, copy_evac(vt, b_vt))
                              for dc in range(2):
                                  r = dc
                                  pv = pms[:, r, 0:256]
                                  for t in range(NT):
                                      S.op("pe", lambda e, t=t, dc=dc, pv=pv: e.matmul(pv, lhsT=kt[:, t, dc * 128:(dc + 1) * 128], rhs=vt[:, t, :],
                                                                                      start=(t == 0), stop=(t == NT - 1)),
                                           reads=[b_kt, b_vt], writes=[b_pms[r]])
                                  S.op("dve", lambda e, dc=dc, pv=pv: e.tensor_tensor(out=Racc[:, h * 2 + dc, :], in0=Racc[:, h * 2 + dc, :], in1=pv, op=ALU.add),
                                       reads=[b_pms[r]], writes=[b_R[h * 2 + dc]])
                          return [(ld_k, cp_k), (ld_v, cp_v)]
                      jobs += mk(h)
                  run_jobs(jobs)
                  S.barrier()
                  rsc_.close()
                  jobs = []
                  lsc_ = ExitStack()
                  alloc_lru(lsc_)
                  for blk in range(8):
                      def mkl(blk):
                          def ld():
                              return load_wslab(w_in[:, 8192 + blk * 256:8192 + (blk + 1) * 256], KC, 256)

                          def cp(hd):
                              mm_feat(hd[0], hd[1], AT, b_AT, SLAB, KC, xb_evac(0))
                              lru_block(j, blk, False)
                          return [(ld, cp)]
                      jobs += mkl(blk)
                  run_jobs(jobs)
                  S.barrier()
                  lsc_.close()

              j = NSLAB - 1
              norm_transpose("pa7", xs[j * SLAB:(j + 1) * SLAB, :], NT, mix_g, AT, b_AT, 0)
              with ExitStack() as po:
                  alloc_ret(po)
                  rope_tables(j)
                  qt = sb("qt", [128, NT, 256], BF16, po); b_qt = S.buf()
                  sg = sb("sg", [128, NT, 256], F32, po); b_sg = S.buf()
                  qT = sb("qT", [128, 2, SLAB], BF16, po); b_qT = S.buf()
                  kT = sb("kT", [128, 2, SLAB], BF16, po); b_kT = S.buf()
                  Rb = sb("Rb", [128, 2, 256], BF16, po); b_Rb = S.buf()
                  PT = sb("PT", [128, 128], BF16, po); b_PT = S.buf()
                  oc = sb("oc", [128, 256], F32, po); b_oc = S.buf()
                  osb = sb("osb", [128, 256], F32, po); b_osb = S.buf()
                  rtb = sb("rtb", [128, 256], BF16, po); b_rtb = S.buf()
                  rTs = sb("rTs", [128, 2, 128], BF16, po); b_rTs = S.buf()
                  kz = sb("kz", [128, 256], BF16, po); b_kz = S.buf()
                  gng = sb("gng", [128, 2048], F32, po); b_gng = S.buf()
                  st6 = sb("st6", [128, 16], F32, po); b_st6 = S.buf()
                  b_mixT = S.bufs(32, "mixT")
                  S.dma("sp", gng[:], gn_g.partition_broadcast(128), writes=[b_gng])

                  def retention_head(h):
                      for (src, bs, dstT, bdT) in ((qt, b_qt, qT, b_qT), (kt, b_kt, kT, b_kT)):
                          for t in range(NT):
                              for dc in range(2):
                                  S.op("pe", lambda e, t=t, dc=dc, src=src: e.transpose(out=ptr[:, dc * 128:(dc + 1) * 128],
                                                                                      in_=src[:, t, dc * 128:(dc + 1) * 128], identity=identb[:]),
                                       reads=[bs, b_idb], writes=[b_ptr])
                              S.op("act", lambda e, t=t, dstT=dstT: e.activation(out=dstT[:, :, t * 128:(t + 1) * 128],
                                                                                 in_=ptr[:, 0:256].rearrange("p (k n) -> p k n", k=2), func=AF.Copy),
                                   reads=[b_ptr], writes=[bdT])
                      for dc in range(2):
                          S.op("act", lambda e, dc=dc: e.activation(out=Rb[:, dc, :], in_=Racc[:, h * 2 + dc, :], func=AF.Copy),
                               reads=[b_R[h * 2 + dc]], writes=[b_Rb])
                      for i in range(NT):
                          cs = slice(i * 128, (i + 1) * 128)
                          pST = pms[:, 0, 0:128]
                          for dc in range(2):
                              S.op("pe", lambda e, dc=dc: e.matmul(pST, lhsT=kT[:, dc, cs], rhs=qT[:, dc, cs], start=(dc == 0), stop=(dc == 1)),
                                   reads=[b_kT, b_qT], writes=[b_pms[0]])
                          S.op("dve", lambda e: e.tensor_tensor(out=PT[:], in0=pST, in1=cst[:, C_DEC + h * 128:C_DEC + (h + 1) * 128], op=ALU.mult),
                               reads=[b_pms[0], b_cst], writes=[b_PT])
                          pOI = pms[:, 0, 256:512]
                          S.op("pe", lambda e: e.matmul(pOI, lhsT=PT[:], rhs=vt[:, i, :], start=True, stop=True),
                               reads=[b_PT, b_vt], writes=[b_pms[1]])
                          pOC = pms[:, 1, 0:256]
                          for dc in range(2):
                              S.op("pe", lambda e, dc=dc: e.matmul(pOC, lhsT=qT[:, dc, cs], rhs=Rb[:, dc, :], start=(dc == 0), stop=(dc == 1)),
                                   reads=[b_qT, b_Rb], writes=[b_pms[2]])
                          S.op("act", lambda e: e.activation(out=oc[:], in_=pOC, func=AF.Copy, scale=cst[:, C_XI + h:C_XI + h + 1]),
                               reads=[b_pms[2], b_cst], writes=[b_oc])
                          S.op("dve", lambda e: e.tensor_tensor(out=osb[:], in0=pOI, in1=oc[:], op=ALU.add),
                               reads=[b_pms[1], b_oc], writes=[b_osb])
                          S.op("dve", lambda e: e.bn_stats(out=st6[:, 0:6], in_=osb[:]), reads=[b_osb], writes=[b_st6])
                          S.op("dve", lambda e: e.bn_aggr(out=st6[:, 8:10], in_=st6[:, 0:6]), reads=[b_st6], writes=[b_st6])
                          S.op("dve", lambda e: e.tensor_scalar(out=st6[:, 10:11], in0=st6[:, 9:10], scalar1=1e-5, scalar2=None, op0=ALU.add),
                               reads=[b_st6], writes=[b_st6])
                          S.op("act", lambda e: e.activation(out=st6[:, 10:11], in_=st6[:, 10:11], func=AF.Sqrt), reads=[b_st6], writes=[b_st6])
                          S.op("dve", lambda e: e.reciprocal(out=st6[:, 11:12], in_=st6[:, 10:11]), reads=[b_st6], writes=[b_st6])
                          S.op("dve", lambda e: e.tensor_scalar(out=osb[:], in0=osb[:], scalar1=st6[:, 8:9], scalar2=st6[:, 11:12],
                                                                op0=ALU.subtract, op1=ALU.mult), reads=[b_osb, b_st6], writes=[b_osb])
                          S.op("dve", lambda e: e.tensor_tensor(out=osb[:], in0=osb[:], in1=gng[:, h * 256:(h + 1) * 256], op=ALU.mult),
                               reads=[b_osb, b_gng], writes=[b_osb])
                          S.op("dve", lambda e: e.tensor_tensor(out=rtb[:], in0=osb[:], in1=sg[:, i, :], op=ALU.mult),
                               reads=[b_osb, b_sg], writes=[b_rtb])
                          for dc in range(2):
                              S.op("pe", lambda e, dc=dc: e.transpose(out=ptr[:, 512 + dc * 128:512 + (dc + 1) * 128], in_=rtb[:, dc * 128:(dc + 1) * 128],
                                                                      identity=identb[:]), reads=[b_rtb, b_idb], writes=[b_ptr])
                          S.op("act", lambda e: e.activation(out=rTs[:], in_=ptr[:, 512:768].rearrange("p (k n) -> p k n", k=2), func=AF.Copy),
                               reads=[b_ptr], writes=[b_rTs])
                          for dc in range(2):
                              r0 = h * 256 + dc * 128
                              S.dma("sp", mixT_d[r0:r0 + 128, cs], rTs[:, dc, :], reads=[b_rTs], writes=[b_mixT[h * 2 + dc]])
                          if i < NT - 1:
                              S.op("dve", lambda e: e.tensor_scalar(out=kz[:], in0=kt[:, i, :], scalar1=cst[:, C_ZETA + h:C_ZETA + h + 1],
                                                                    scalar2=None, op0=ALU.mult), reads=[b_kt, b_cst], writes=[b_kz])
                              for dc in range(2):
                                  pRU = pms[:, 1, 256:512]
                                  S.op("pe", lambda e, dc=dc: e.matmul(pRU, lhsT=kz[:, dc * 128:(dc + 1) * 128], rhs=vt[:, i, :], start=True, stop=True),
                                       reads=[b_kz, b_vt], writes=[b_pms[3]])
                                  S.op("dve", lambda e, dc=dc: e.scalar_tensor_tensor(out=Racc[:, h * 2 + dc, :], in0=Racc[:, h * 2 + dc, :],
                                                                                      scalar=cst[:, C_CD + h:C_CD + h + 1], in1=pRU,
                                                                                      op0=ALU.mult, op1=ALU.add),
                                       reads=[b_pms[3], b_cst], writes=[b_R[h * 2 + dc]])
                                  S.op("act", lambda e, dc=dc: e.activation(out=Rb[:, dc, :], in_=Racc[:, h * 2 + dc, :], func=AF.Copy),
                                       reads=[b_R[h * 2 + dc]], writes=[b_Rb])

                  jobs = []
                  for h in range(8):
                      def mko(h):
                          def ld(c0):
                              return lambda: load_wslab(w_in[:, c0 + h * 256:c0 + (h + 1) * 256], KC, 256)

                          def cp_q(hd):
                              mm_tok(hd[0], hd[1], AT, b_AT, NT, KC, rope_evac(qt, b_qt))

                          def cp_k(hd):
                              mm_tok(hd[0], hd[1], AT, b_AT, NT, KC, rope_evac(kt, b_kt))

                          def cp_v(hd):
                              mm_tok(hd[0], hd[1], AT, b_AT, NT, KC, copy_evac(vt, b_vt))

                          def cp_g(hd):
                              def ev(t, pv, bp):
                                  S.op("act", lambda e: e.activation(out=sg[:, t, :], in_=pv, func=AF.Silu), reads=[bp], writes=[b_sg])
                              mm_tok(hd[0], hd[1], AT, b_AT, NT, KC, ev)
                              retention_head(h)
                          return [(ld(0), cp_q), (ld(2048), cp_k), (ld(4096), cp_v), (ld(6144), cp_g)]
                      jobs += mko(h)
                  run_jobs(jobs)
                  S.barrier()

              with ExitStack() as pl:
                  alloc_lru(pl)
                  gt = sb("gt", [128, 2, 1024], F32, pl); b_gt = S.bufs(2)
                  ssum = sb("ssum", [128, 1024], F32, pl); b_ssum = S.buf()
                  b_yT = S.bufs(16, "yT")
                  S.op("dve", lambda e: e.memset(ssum[:], 0.0), writes=[b_ssum])
                  own_ctx.update(gt=gt, b_gt=b_gt, ssum=ssum, b_ssum=b_ssum, b_yT=b_yT)
                  jobs = []
                  for blk in range(8):
                      def mkl2(blk):
                          def ld_x():
                              return load_wslab(w_in[:, 8192 + blk * 256:8192 + (blk + 1) * 256], KC, 256)

                          def cp_x(hd):
                              mm_feat(hd[0], hd[1], AT, b_AT, SLAB, KC, xb_evac(0))

                          def ld_g():
                              return load_wslab(w_in[:, 10240 + blk * 256:10240 + (blk + 1) * 256], KC, 256)

                          def cp_g(hd):
                              def ev(cc, hf, pv, bp):
                                  S.op("act", lambda e: e.activation(out=gt[:, cc, hf * 512:(hf + 1) * 512], in_=pv, func=AF.Copy),
                                       reads=[bp], writes=[b_gt[cc]])
                              mm_feat(hd[0], hd[1], AT, b_AT, SLAB, KC, ev)
                              lru_block(NSLAB - 1, blk, True)
                          return [(ld_x, cp_x), (ld_g, cp_g)]
                      jobs += mkl2(blk)
                  run_jobs(jobs)
                  rms_rstd(ssum[:], 2048.0, 1e-6, ssum[:], b_ssum)
                  lh = LB["lh"]; b_lh = LB["b_lh"]; xcb = LB["xcb"]; b_xcb = LB["b_xcb"]
                  for ch in range(16):
                      S.dma("sp", lh[:], yT_d[ch * 128:(ch + 1) * 128, :], reads=[b_yT[ch]], writes=[b_lh])
                      S.op("dve", lambda e, ch=ch: e.scalar_tensor_tensor(out=xcb[:, 0, :], in0=lh[:], scalar=pp[:, P_LG + ch:P_LG + ch + 1],
                                                                          in1=ssum[:], op0=ALU.mult, op1=ALU.mult),
                           reads=[b_lh, b_pp, b_ssum], writes=[b_xcb[0]])
                      S.dma("sp", mixT_d[2048 + ch * 128:2048 + (ch + 1) * 128, :], xcb[:, 0, :], reads=[b_xcb[0]], writes=[b_mixT[16 + ch]])
                  S.barrier()
              S.barrier()

        b_x1 = [[S.buf() for _ in range(16)] for _ in range(NT)]
        b_x2 = [[S.buf() for _ in range(16)] for _ in range(NT)]

        def load_AT_from(dram_T, deps):
            for k in range(KC):
                S.dma("sp", AT[:, k, :], dram_T[k * 128:(k + 1) * 128, :], reads=[deps[k]], writes=[b_AT])

        def resid_linear(ph, W, src_rows, src_bufs, dst_rows=None, dst_bufs=None):
            dst_rows = x1_d if dst_rows is None else dst_rows
            dst_bufs = b_x1 if dst_bufs is None else dst_bufs
            with ExitStack() as ls:
                rs = sb(ph + "rs", [128, 4, 256], F32, ls); b_rs = S.bufs(4)
                ri = [0]
                jobs = []
                for s in range(16):
                    def mk(s):
                        def ld():
                            return load_wslab(W[:, s * 256:(s + 1) * 256], KC, 256)

                        def cp(hd):
                            def ev(t, pv, bp):
                                r = ri[0]; ri[0] = (r + 1) % 4
                                S.dma("sp", rs[:, r, :], src_rows[t * 128:(t + 1) * 128, s * 256:(s + 1) * 256],
                                      reads=[src_bufs[t][s]] if src_bufs else [], writes=[b_rs[r]])
                                S.op("dve", lambda e: e.tensor_tensor(out=rs[:, r, :], in0=pv, in1=rs[:, r, :], op=ALU.add),
                                     reads=[bp, b_rs[r]], writes=[b_rs[r]])
                                S.dma("sp", dst_rows[t * 128:(t + 1) * 128, s * 256:(s + 1) * 256], rs[:, r, :], reads=[b_rs[r]], writes=[dst_bufs[t][s]])
                            mm_tok(hd[0], hd[1], AT, b_AT, NT, KC, ev)
                        return (ld, cp)
                    jobs.append(mk(s))
                run_jobs(jobs)
                S.barrier()

        load_AT_from(mixT_d, b_mixT)
        resid_linear("wo1", w_out, xs[(NSLAB - 1) * SLAB:NSLAB * SLAB, :], None)

        if stop_after == "A":
            S.finish([b for row in b_x1 for b in row])
            return nc

        with ExitStack() as pb_:
            memT = sb("memT", [128, KC, 256], BF16, pb_); b_memT = S.buf()
            norm_transpose("pm", mem_d, 2, mem_g, memT, b_memT, 0)
            KTm = sb("KTm", [128, KC, 256], BF16, pb_); b_KT = S.buf()
            Vm = sb("Vm", [128, 2, D], BF16, pb_); b_Vm = S.buf()
            jobs = []
            for s in range(16):
                def mkk(s):
                    def ld():
                        return load_wslab(wk[:, s * 256:(s + 1) * 256], KC, 256)

                    def cp(hd):
                        def ev(cc, hf, pv, bp):
                            S.op("act", lambda e: e.activation(out=KTm[:, s * 2 + cc, :], in_=pv, func=AF.Copy), reads=[bp], writes=[b_KT])
                        mm_feat(hd[0], hd[1], memT, b_memT, 256, KC, ev)
                    return (ld, cp)

                def mkv(s):
                    def ld():
                        return load_wslab(wv[:, s * 256:(s + 1) * 256], KC, 256)

                    def cp(hd):
                        def ev(t, pv, bp):
                            S.op("act", lambda e: e.activation(out=Vm[:, t, s * 256:(s + 1) * 256], in_=pv, func=AF.Copy), reads=[bp], writes=[b_Vm])
                        mm_tok(hd[0], hd[1], memT, b_memT, 2, KC, ev)
                    return (ld, cp)
                jobs += [mkk(s), mkv(s)]
            run_jobs(jobs)

            x1_all = [b for row in b_x1 for b in row]
            b_x1tile = S.buf()
            for b in x1_all:
                pass
            sync_tok = S.buf()
            S.op("dve", lambda e: e.memset(sdesc[:, 0:1], 0.0), reads=[], writes=[b_sd])

            def norm_transpose_x1(ph, gvec):
                deps = []
                for b in x1_all:
                    if b.writer is not None:
                        deps.append(b.writer)
                S._wait(S.E["sp"], deps)
                norm_transpose(ph, x1_d, NT, gvec, AT, b_AT, 0)

            if stop_after == "B1":
                S.barrier(); S.finish([]); return nc
            norm_transpose_x1("pb", xat_g)
            if stop_after == "B2":
                S.barrier(); S.finish([]); return nc
            b_oT = S.bufs(32, "oT")
            with ExitStack() as px:
                qx = sb("qx", [128, 8, SLAB], BF16, px); b_qx = S.buf()
                prob = sb("prob", [128, 256], F32, px); b_prob = S.buf()
                probb = sb("probb", [128, 256], BF16, px); b_probb = S.buf()
                pT = sb("pT", [128, 2, SLAB], BF16, px); b_pT = S.buf()
                sm = sb("sm", [128, 8], F32, px); b_sm = S.buf()
                oTs = sb("oTs", [128, 2, 512], BF16, px); b_oTs = S.bufs(2)
                for hx in range(4):
                    jobs = []
                    for s4 in range(4):
                        def mkq(s4):
                            def ld():
                                c0 = hx * 1024 + s4 * 256
                                return load_wslab(wq[:, c0:c0 + 256], KC, 256)

                            def cp(hd):
                                def ev(cc, hf, pv, bp):
                                    S.op("act", lambda e: e.activation(out=qx[:, s4 * 2 + cc, hf * 512:(hf + 1) * 512], in_=pv, func=AF.Copy),
                                         reads=[bp], writes=[b_qx])
                                mm_feat(hd[0], hd[1], AT, b_AT, SLAB, KC, ev)
                            return (ld, cp)
                        jobs.append(mkq(s4))
                    run_jobs(jobs)
                    for t in range(NT):
                        pb = next_pmm(); pv = pmm[:, pb, 0:256]
                        for dc in range(8):
                            S.op("pe", lambda e, dc=dc, pv=pv: e.matmul(pv, lhsT=qx[:, dc, t * 128:(t + 1) * 128], rhs=KTm[:, hx * 8 + dc, :],
                                                                      start=(dc == 0), stop=(dc == 7)), reads=[b_qx, b_KT], writes=[b_pmm[pb]])
                        S.op("dve", lambda e, pv=pv: e.reduce_max(out=sm[:, 0:1], in_=pv, axis=AX.X), reads=[b_pmm[pb]], writes=[b_sm])
                        S.op("dve", lambda e: e.tensor_scalar(out=sm[:, 1:2], in0=sm[:, 0:1], scalar1=-1.0 / 32.0, scalar2=None, op0=ALU.mult),
                             reads=[b_sm], writes=[b_sm])
                        S.op("act", lambda e, pv=pv: e.activation(out=prob[:], in_=pv, func=AF.Exp, scale=1.0 / 32.0, bias=sm[:, 1:2]),
                             reads=[b_pmm[pb], b_sm], writes=[b_prob])
                        S.op("dve", lambda e: e.reduce_sum(out=sm[:, 2:3], in_=prob[:], axis=AX.X), reads=[b_prob], writes=[b_sm])
                        S.op("dve", lambda e: e.reciprocal(out=sm[:, 3:4], in_=sm[:, 2:3]), reads=[b_sm], writes=[b_sm])
                        S.op("dve", lambda e: e.tensor_scalar(out=probb[:], in0=prob[:], scalar1=sm[:, 3:4], scalar2=None, op0=ALU.mult),
                             reads=[b_prob, b_sm], writes=[b_probb])
                        for mc in range(2):
                            S.op("pe", lambda e, mc=mc: e.transpose(out=ptr[:, mc * 128:(mc + 1) * 128], in_=probb[:, mc * 128:(mc + 1) * 128],
                                                                    identity=identb[:]), reads=[b_probb, b_idb], writes=[b_ptr])
                        S.op("act", lambda e, t=t: e.activation(out=pT[:, :, t * 128:(t + 1) * 128],
                                                                in_=ptr[:, 0:256].rearrange("p (k n) -> p k n", k=2), func=AF.Copy),
                             reads=[b_ptr], writes=[b_pT])
                    for dvc in range(8):
                        for hf in range(2):
                            pb = next_pmm(); pv = pmm[:, pb, :]
                            for mc in range(2):
                                S.op("pe", lambda e, mc=mc, pv=pv, hf=hf, dvc=dvc: e.matmul(
                                    pv, lhsT=Vm[:, mc, hx * 1024 + dvc * 128:hx * 1024 + (dvc + 1) * 128], rhs=pT[:, mc, hf * 512:(hf + 1) * 512],
                                    start=(mc == 0), stop=(mc == 1)), reads=[b_Vm, b_pT], writes=[b_pmm[pb]])
                            S.op("act", lambda e, pv=pv, hf=hf: e.activation(out=oTs[:, hf, :], in_=pv, func=AF.Copy), reads=[b_pmm[pb]], writes=[b_oTs[hf]])
                            r0 = hx * 1024 + dvc * 128
                            S.dma("sp", mixT_d[r0:r0 + 128, hf * 512:(hf + 1) * 512], oTs[:, hf, :], reads=[b_oTs[hf], b_AT],
                                  writes=[b_oT[hx * 8 + dvc]])
                S.barrier()
            if stop_after == "B3":
                S.barrier(); S.finish([]); return nc
            load_AT_from(mixT_d, b_oT)
            if stop_after == "B4":
                S.barrier(); S.finish([]); return nc
            resid_linear("wo2", wo, x1_d, b_x1, x2_d, b_x2)
            S.barrier()

        if stop_after == "B":
            S.finish([b for row in b_x2 for b in row])
            return nc

        with ExitStack() as pc:
            Wt = sb("Wt", [128, NT, 32], F32, pc); b_Wt = S.buf()
            wrs = sb("wrs", [128, KC * 36], F32, pc); b_wrs = S.buf()
            whi = sb("whi", [128, KC, 36], BF16, pc); wlo = sb("wlo", [128, KC, 36], BF16, pc); b_whl = S.buf()
            rbb = sb("rbb", [128, 36], F32, pc); b_rbb = S.buf()
            S.dma("sp", wrs[:], wr_d, writes=[b_wrs])
            S.dma("sp", rbb[:], rb_d.partition_broadcast(128), writes=[b_rbb])
            whi2 = whi[:, :, :].rearrange("p k n -> p (k n)"); wlo2 = wlo[:, :, :].rearrange("p k n -> p (k n)")
            S.op("act", lambda e: e.activation(out=whi2, in_=wrs[:], func=AF.Copy), reads=[b_wrs], writes=[b_whl])
            S.op("dve", lambda e: e.tensor_tensor(out=wlo2, in0=wrs[:], in1=whi2, op=ALU.subtract), reads=[b_wrs, b_whl], writes=[b_whl])
            deps = []
            for b in [b for row in b_x2 for b in row]:
                if b.writer is not None:
                    deps.append(b.writer)
            S._wait(S.E["sp"], deps)
            with ExitStack() as pr:
                gbc = sb("c_gbc", [128, D], F32, pr); b_g = S.buf()
                xt = sb("c_xt", [128, D], F32, pr); b_xt = S.buf()
                hit = sb("c_hit", [128, D], BF16, pr); b_hit = S.buf()
                lot = sb("c_lot", [128, D], BF16, pr); b_lot = S.buf()
                loT = sb("c_loT", [128, KC, 128], BF16, pr); b_loT = S.buf()
                ss = sb("c_ss", [128, 2], F32, pr); b_ss = S.buf()
                lg = sb("c_lg", [128, 40], F32, pr); b_lg = S.buf()
                rw = sb("c_rw", [128, 8, 32], F32, pr); b_rw = S.buf()
                S.dma("sp", gbc[:], moe_g.partition_broadcast(128), writes=[b_g])
                for t in range(NT):
                    S.dma("sp", xt[:], x2_d[t * 128:(t + 1) * 128, :], writes=[b_xt])
                    S.op("act", lambda e: e.activation(out=hit[:], in_=xt[:], func=AF.Square), reads=[b_xt], writes=[b_hit])
                    S.op("dve", lambda e: e.reduce_sum(out=ss[:, 0:1], in_=hit[:], axis=AX.X), reads=[b_hit], writes=[b_ss])
                    rms_rstd(ss[:, 0:1], float(D), 1e-6, ss[:, 1:2], b_ss)
                    S.op("dve", lambda e: e.scalar_tensor_tensor(out=xt[:], in0=xt[:], scalar=ss[:, 1:2], in1=gbc[:],
                                                                 op0=ALU.mult, op1=ALU.mult), reads=[b_xt, b_ss, b_g], writes=[b_xt])
                    S.op("act", lambda e: e.activation(out=hit[:], in_=xt[:], func=AF.Copy), reads=[b_xt], writes=[b_hit])
                    S.op("dve", lambda e: e.tensor_tensor(out=lot[:], in0=xt[:], in1=hit[:], op=ALU.subtract), reads=[b_xt, b_hit], writes=[b_lot])
                    for (srcb, bsrc, dstT, bdst, c0) in ((hit, b_hit, AT, b_AT, t * 128), (lot, b_lot, loT, b_loT, 0)):
                        for g8 in range(4):
                            for jx in range(8):
                                k = g8 * 8 + jx
                                S.op("pe", lambda e, k=k, jx=jx, srcb=srcb: e.transpose(out=ptr[:, jx * 128:(jx + 1) * 128],
                                                                                      in_=srcb[:, k * 128:(k + 1) * 128], identity=identb[:]),
                                     reads=[bsrc, b_idb], writes=[b_ptr])
                            src = ptr[:, :].rearrange("p (k n) -> p k n", k=8)
                            S.op("act", lambda e, g8=g8, src=src, dstT=dstT, c0=c0: e.activation(
                                out=dstT[:, g8 * 8:(g8 + 1) * 8, c0:c0 + 128], in_=src, func=AF.Copy), reads=[b_ptr], writes=[bdst])
                    pl_ = pms[:, 0, 0:36]
                    nmm = 0
                    for k in range(KC):
                        for (lh_, bl_, wv_) in ((AT[:, k, t * 128:(t + 1) * 128], b_AT, whi), (AT[:, k, t * 128:(t + 1) * 128], b_AT, wlo),
                                                (loT[:, k, :], b_loT, whi)):
                            S.op("pe", lambda e, k=k, lh_=lh_, wv_=wv_, nmm=nmm: e.matmul(pl_, lhsT=lh_, rhs=wv_[:, k, :],
                                                                                       start=(nmm == 0), stop=(nmm == 3 * KC - 1)),
                                 reads=[bl_, b_whl], writes=[b_pms[0]])
                            nmm += 1
                    S.op("dve", lambda e: e.tensor_tensor(out=lg[:, 0:36], in0=pl_, in1=rbb[:], op=ALU.add), reads=[b_pms[0], b_rbb], writes=[b_lg])
                    S.op("dve", lambda e: e.reduce_max(out=lg[:, 36:37], in_=lg[:, 0:4], axis=AX.X), reads=[b_lg], writes=[b_lg])
                    S.op("dve", lambda e: e.tensor_scalar(out=rw[:, 0, 0:4], in0=lg[:, 0:4], scalar1=lg[:, 36:37], scalar2=None, op0=ALU.subtract),
                         reads=[b_lg], writes=[b_rw])
                    S.op("act", lambda e: e.activation(out=rw[:, 1, 0:4], in_=rw[:, 0, 0:4], func=AF.Exp), reads=[b_rw], writes=[b_rw])
                    S.op("dve", lambda e: e.reduce_sum(out=lg[:, 37:38], in_=rw[:, 1, 0:4], axis=AX.X), reads=[b_rw], writes=[b_lg])
                    S.op("dve", lambda e: e.reciprocal(out=lg[:, 37:38], in_=lg[:, 37:38]), reads=[b_lg], writes=[b_lg])
                    S.op("dve", lambda e: e.tensor_scalar(out=rw[:, 2, 0:4], in0=lg[:, 0:4], scalar1=lg[:, 36:37], scalar2=None, op0=ALU.is_ge),
                         reads=[b_lg], writes=[b_rw])
                    S.op("dve", lambda e: e.tensor_scalar(out=rw[:, 2, 0:4], in0=rw[:, 2, 0:4], scalar1=-1.0, scalar2=1e30, op0=ALU.add, op1=ALU.mult),
                         reads=[b_rw], writes=[b_rw])
                    for g in range(4):
                        S.op("dve", lambda e, g=g: e.tensor_scalar(out=rw[:, 3, g * 8:(g + 1) * 8], in0=lg[:, 4 + g * 8:4 + (g + 1) * 8],
                                                                   scalar1=rw[:, 2, g:g + 1], scalar2=None, op0=ALU.add), reads=[b_lg, b_rw], writes=[b_rw])
                    S.op("dve", lambda e: e.reduce_max(out=lg[:, 38:39], in_=rw[:, 3, :], axis=AX.X), reads=[b_rw], writes=[b_lg])
                    S.op("dve", lambda e: e.tensor_scalar(out=rw[:, 4, :], in0=rw[:, 3, :], scalar1=lg[:, 38:39], scalar2=None, op0=ALU.is_ge),
                         reads=[b_rw, b_lg], writes=[b_rw])
                    S.op("dve", lambda e: e.scalar_tensor_tensor(out=rw[:, 5, :], in0=rw[:, 4, :], scalar=-1e30, in1=rw[:, 3, :], op0=ALU.mult, op1=ALU.add),
                         reads=[b_rw], writes=[b_rw])
                    S.op("dve", lambda e: e.reduce_max(out=lg[:, 39:40], in_=rw[:, 5, :], axis=AX.X), reads=[b_rw], writes=[b_lg])
                    S.op("dve", lambda e: e.tensor_scalar(out=rw[:, 6, :], in0=rw[:, 5, :], scalar1=lg[:, 39:40], scalar2=None, op0=ALU.is_ge),
                         reads=[b_rw, b_lg], writes=[b_rw])
                    S.op("dve", lambda e: e.tensor_tensor(out=ss[:, 0:1], in0=lg[:, 38:39], in1=lg[:, 39:40], op=ALU.subtract), reads=[b_lg], writes=[b_ss])
                    S.op("act", lambda e: e.activation(out=ss[:, 0:1], in_=ss[:, 0:1], func=AF.Sigmoid), reads=[b_ss], writes=[b_ss])
                    S.op("dve", lambda e: e.tensor_tensor(out=ss[:, 0:1], in0=ss[:, 0:1], in1=lg[:, 37:38], op=ALU.mult), reads=[b_ss, b_lg], writes=[b_ss])
                    S.op("dve", lambda e: e.tensor_tensor(out=ss[:, 1:2], in0=lg[:, 37:38], in1=ss[:, 0:1], op=ALU.subtract), reads=[b_ss, b_lg], writes=[b_ss])
                    S.op("dve", lambda e: e.tensor_scalar(out=rw[:, 7, :], in0=rw[:, 6, :], scalar1=ss[:, 1:2], scalar2=None, op0=ALU.mult),
                         reads=[b_rw, b_ss], writes=[b_rw])
                    S.op("dve", lambda e, t=t: e.scalar_tensor_tensor(out=Wt[:, t, :], in0=rw[:, 4, :], scalar=ss[:, 0:1], in1=rw[:, 7, :],
                                                                      op0=ALU.mult, op1=ALU.add), reads=[b_rw, b_ss], writes=[b_Wt])

                S.barrier()
            if stop_after == "C1":
                S.barrier(); S.finish([]); return nc
            with ExitStack() as pe_:
                actT = sb("actT", [128, 8, SLAB], BF16, pe_); b_actT = S.bufs(8)
                sil = sb("sil", [128, 2, 512], F32, pe_); b_sil = S.bufs(2)
                accp = sb("accp", [128, 4, 1024], F32, pe_); b_accp = S.bufs(4)
                pg_hold = {}
                ai = [0]; si = [0]
                b_x3 = [[S.buf() for _ in range(4)] for _ in range(NT)]
                for t in range(NT):
                    for q4 in range(4):
                        b_x3[t][q4].writer = None
                jobs = []
                for ex in range(nexp):
                    def mke(ex):
                        js = []
                        for s4 in range(4):
                            def ldg(s4=s4):
                                return load_wslab(wg[ex, :, s4 * 256:(s4 + 1) * 256], KC, 256)

                            def cpg(hd, s4=s4):
                                pg_hold[s4] = hd
                            def ldu(s4=s4):
                                return load_wslab(wu[ex, :, s4 * 256:(s4 + 1) * 256], KC, 256)

                            def cpu(hd, s4=s4):
                                gw, gb = pg_hold.pop(s4)
                                uw, ub = hd
                                for cc in range(2):
                                    dch = s4 * 2 + cc
                                    for hf in range(2):
                                        pbg = next_pmm(); pvg = pmm[:, pbg, :]
                                        for k in range(KC):
                                            S.op("pe", lambda e, k=k, pvg=pvg, cc=cc, hf=hf: e.matmul(
                                                pvg, lhsT=gw[:, k, cc * 128:(cc + 1) * 128], rhs=AT[:, k, hf * 512:(hf + 1) * 512],
                                                start=(k == 0), stop=(k == KC - 1)), reads=[b_AT, gb], writes=[b_pmm[pbg]])
                                        pbu = next_pmm(); pvu = pmm[:, pbu, :]
                                        for k in range(KC):
                                            S.op("pe", lambda e, k=k, pvu=pvu, cc=cc, hf=hf: e.matmul(
                                                pvu, lhsT=uw[:, k, cc * 128:(cc + 1) * 128], rhs=AT[:, k, hf * 512:(hf + 1) * 512],
                                                start=(k == 0), stop=(k == KC - 1)), reads=[b_AT, ub], writes=[b_pmm[pbu]])
                                        r = si[0]; si[0] = (r + 1) % 2
                                        S.op("act", lambda e, r=r, pvg=pvg: e.activation(out=sil[:, r, :], in_=pvg, func=AF.Silu),
                                             reads=[b_pmm[pbg]], writes=[b_sil[r]])
                                        S.op("dve", lambda e, r=r, pvu=pvu, dch=dch, hf=hf: e.tensor_tensor(
                                            out=actT[:, dch, hf * 512:(hf + 1) * 512], in0=pvu, in1=sil[:, r, :], op=ALU.mult),
                                             reads=[b_pmm[pbu], b_sil[r]], writes=[b_actT[dch]])
                            js += [(ldg, cpg), (ldu, cpu)]
                        for q4 in range(4):
                            def ldd(q4=q4):
                                return load_wslab(wd[ex, :, q4 * 1024:(q4 + 1) * 1024], 8, 1024)

                            def cpd(hd, q4=q4):
                                dw, db = hd
                                for t in range(NT):
                                    a = ai[0]; ai[0] = (a + 1) % 4
                                    srcd = x2_d if ex % 2 == 0 else x3_d
                                    dstd = x3_d if ex % 2 == 0 else x2_d
                                    S.dma("sp", accp[:, a, :], srcd[t * 128:(t + 1) * 128, q4 * 1024:(q4 + 1) * 1024],
                                          reads=[b_x3[t][q4]], writes=[b_accp[a]])
                                    for g2 in range(2):
                                        pb = next_pmm(); pv = pmm[:, pb, :]
                                        for k in range(8):
                                            S.op("pe", lambda e, k=k, pv=pv, t=t, g2=g2: e.matmul(
                                                pv, lhsT=actT[:, k, t * 128:(t + 1) * 128], rhs=dw[:, k, g2 * 512:(g2 + 1) * 512],
                                                start=(k == 0), stop=(k == 7)), reads=b_actT + [db], writes=[b_pmm[pb]])
                                        S.op("dve", lambda e, pv=pv, a=a, g2=g2, t=t: e.scalar_tensor_tensor(
                                            out=accp[:, a, g2 * 512:(g2 + 1) * 512], in0=pv, scalar=Wt[:, t, ex:ex + 1],
                                            in1=accp[:, a, g2 * 512:(g2 + 1) * 512], op0=ALU.mult, op1=ALU.add),
                                             reads=[b_pmm[pb], b_Wt, b_accp[a]], writes=[b_accp[a]])
                                    S.dma("sp", dstd[t * 128:(t + 1) * 128, q4 * 1024:(q4 + 1) * 1024], accp[:, a, :],
                                          reads=[b_accp[a]], writes=[b_x3[t][q4]])
                            js.append((ldd, cpd))
                        return js
                    jobs += mke(ex)
                run_jobs(jobs)
                S.barrier()

            deps = []
            for row in b_x3:
                for b in row:
                    if b.writer is not None:
                        deps.append(b.writer)
            S._wait(S.E["sp"], deps)
            with ExitStack() as pf:
                gbc = sb("f_gbc", [128, D], F32, pf); b_g = S.buf()
                xt = sb("f_xt", [128, 2, D], F32, pf); b_xt = S.bufs(2)
                ss = sb("f_ss", [128, 4], F32, pf); b_ss = S.bufs(2)
                junk = sb("f_junk", [128, D], F32, pf); b_junk = S.buf()
                b_out = S.bufs(NT)
                S.dma("sp", gbc[:], fin_g.partition_broadcast(128), writes=[b_g])
                for t in range(NT):
                    r = t % 2
                    S.dma("sp", xt[:, r, :], x2_d[t * 128:(t + 1) * 128, :], writes=[b_xt[r]])
                    S.op("act", lambda e, r=r: e.activation(out=junk[:], in_=xt[:, r, :], func=AF.Square), reads=[b_xt[r]], writes=[b_junk])
                    S.op("dve", lambda e, r=r: e.reduce_sum(out=ss[:, 2 * r:2 * r + 1], in_=junk[:], axis=AX.X), reads=[b_junk], writes=[b_ss[r]])
                    rms_rstd(ss[:, 2 * r:2 * r + 1], float(D), 1e-6, ss[:, 2 * r + 1:2 * r + 2], b_ss[r])
                    S.op("dve", lambda e, r=r: e.scalar_tensor_tensor(out=xt[:, r, :], in0=xt[:, r, :], scalar=ss[:, 2 * r + 1:2 * r + 2], in1=gbc[:],
                                                                      op0=ALU.mult, op1=ALU.mult), reads=[b_xt[r], b_ss[r], b_g], writes=[b_xt[r]])
                    S.dma("sp", out_d[t * 128:(t + 1) * 128, :], xt[:, r, :], reads=[b_xt[r]], writes=[b_out[t]])
                S.finish(b_out)
    return nc


_CACHE = {}


def _prep_inputs(inp):
    f32 = np.float32
    x = np.asarray(inp["x"], f32)[0]
    pos = np.asarray(inp["positions"])[0].astype(np.int32)
    cst = make_consts()
    lru_cw = np.asarray(inp["lru_conv_w"], f32)[0]
    pp = np.zeros((128, NPP), f32)
    for ch in range(16):
        sl = slice(ch * 128, (ch + 1) * 128)
        for jj in range(4):
            pp[:, P_CW + ch * 4 + jj] = lru_cw[jj, sl]
        pp[:, P_CB + ch] = np.asarray(inp["lru_conv_b"], f32)[0, sl]
        pp[:, P_BA + ch] = np.asarray(inp["lru_b_a"], f32)[0, sl]
        pp[:, P_BI + ch] = np.asarray(inp["lru_b_i"], f32)[0, sl]
        pp[:, P_LAM + ch] = np.asarray(inp["lru_lambda"], f32)[0, sl]
        pp[:, P_LG + ch] = np.asarray(inp["lru_norm_g"], f32)[0, sl]
    wr = np.concatenate([np.asarray(inp["router_group_w"], f32)[0],
                         np.asarray(inp["router_expert_w"], f32)[0]], axis=1)
    wr = np.ascontiguousarray(wr.reshape(KC, 128, 36).transpose(1, 0, 2).reshape(128, KC * 36))
    rb = np.ascontiguousarray(np.concatenate([np.asarray(inp["router_group_b"], f32)[0],
                                              np.asarray(inp["router_expert_b"], f32)[0]])[None, :])
    shared = {
        "mem": np.ascontiguousarray(np.asarray(inp["mem"], f32)[0]),
        "cst": cst, "pp": pp,
        "mix_g": np.asarray(inp["mix_norm_g"], f32).reshape(1, D),
        "gn_g": np.asarray(inp["ret_norm_g"], f32).reshape(1, 2048),
        "xat_g": np.asarray(inp["xattn_norm_g"], f32).reshape(1, D),
        "mem_g": np.asarray(inp["mem_norm_g"], f32).reshape(1, D),
        "moe_g": np.asarray(inp["moe_norm_g"], f32).reshape(1, D),
        "fin_g": np.asarray(inp["final_norm_g"], f32).reshape(1, D),
        "rb": rb, "wr": wr,
        "w_in": np.asarray(inp["w_in"], f32)[0], "w_out": np.asarray(inp["w_out"], f32)[0],
        "wq": np.asarray(inp["xattn_wq"], f32)[0], "wk": np.asarray(inp["xattn_wk"], f32)[0],
        "wv": np.asarray(inp["xattn_wv"], f32)[0], "wo": np.asarray(inp["xattn_wo"], f32)[0],
        "w_a": np.asarray(inp["lru_w_a"], f32)[0], "w_i": np.asarray(inp["lru_w_i"], f32)[0],
        "wg": np.asarray(inp["expert_w_gate"], f32)[0], "wu": np.asarray(inp["expert_w_up"], f32)[0],
        "wd": np.asarray(inp["expert_w_down"], f32)[0],
    }
    in_maps = []
    for c in range(NCORES):
        xs = np.zeros((NSLAB * SLAB, D), f32)
        ps_ = np.zeros((NSLAB * SLAB,), np.int32)
        vm = np.zeros((128, 8), f32)
        for j in range(NSLAB):
            g = c - (NSLAB - 1) + j
            if g >= 0:
                xs[j * SLAB:(j + 1) * SLAB] = x[g * SLAB:(g + 1) * SLAB]
                ps_[j * SLAB:(j + 1) * SLAB] = pos[g * SLAB:(g + 1) * SLAB]
                vm[:, j] = 1.0
        pos_pm = np.ascontiguousarray(ps_.reshape(64, 128).T)
        m = dict(shared)
        m.update(xs=xs, pos_pm=pos_pm, vmask=vm)
        in_maps.append(m)
    return in_maps


def kernel(**inputs):
    if "nc" not in _CACHE:
        _CACHE["nc"] = build_program()
    nc = _CACHE["nc"]
    in_maps = _prep_inputs(inputs)
    res = run_bass_kernel_spmd(nc, in_maps, core_ids=list(range(NCORES)))
    out = np.concatenate([np.asarray(r["out"], np.float32) for r in res.results], axis=0)
    return out.reshape(1, NCORES * SLAB, D)
```

```python
import contextlib
from contextlib import ExitStack
import numpy as np
import concourse.bass as bass
import concourse.mybir as mybir
from concourse.bass_utils import run_bass_kernel_spmd

F32 = mybir.dt.float32
BF16 = mybir.dt.bfloat16
I32 = mybir.dt.int32
ALU = mybir.AluOpType
AF = mybir.ActivationFunctionType
AX = mybir.AxisListType

NCORES = 8
D = 4096
KC = 32
SLAB = 1024
NT = 8
NSLAB = 8
NEXP = 32
DE = 1024

C_INVF = 0
C_DEC = 128
C_XI = C_DEC + 8 * 128
C_ZETA = C_XI + 8
C_CD = C_ZETA + 8
C_WGT = C_CD + 8
C_ID = C_WGT + 8 * 56
NCST = C_ID + 128
P_CW = 0
P_CB = 64
P_BA = 80
P_BI = 96
P_LAM = 112
P_LG = 128
NPP = 144


class Buf:
    __slots__ = ("name", "writer", "readers")

    def __init__(self, name=""):
        self.name = name
        self.writer = None
        self.readers = []


class _Eng:
    def __init__(self, name, eng, sem):
        self.name = name
        self.eng = eng
        self.sem = sem
        self.cnt = 0
        self.seen = {}


class Sched:
    def __init__(self, nc, stack, n_dma_sems=32):
        self.nc = nc
        self.E = {}
        for name, eng in (("pe", nc.tensor), ("act", nc.scalar), ("dve", nc.vector),
                          ("pool", nc.gpsimd), ("sp", nc.sync)):
            sem = stack.enter_context(nc.semaphore("s_" + name))
            self.E[name] = _Eng(name, eng, sem)
        self.dsems = []
        for i in range(n_dma_sems):
            sem = stack.enter_context(nc.semaphore("s_dma%d" % i))
            self.dsems.append([sem, 0])
        self.drr = 0

    def buf(self, name=""):
        return Buf(name)

    def bufs(self, n, name=""):
        return [Buf(name + str(i)) for i in range(n)]

    def _wait(self, E, deps):
        need = {}
        for (sem, val, ename) in deps:
            if ename == E.name and ename == "pe":
                continue
            k = id(sem)
            if E.seen.get(k, 0) >= val:
                continue
            if k not in need or need[k][1] < val:
                need[k] = (sem, val)
        for k, (sem, val) in need.items():
            E.eng.wait_ge(sem, val)
            E.seen[k] = val

    @staticmethod
    def _deps(reads, writes):
        deps = []
        for b in reads:
            if b.writer is not None:
                deps.append(b.writer)
        for b in writes:
            if b.writer is not None:
                deps.append(b.writer)
            deps.extend(b.readers)
        return deps

    @staticmethod
    def _commit(tok, reads, writes):
        for b in writes:
            b.writer = tok
            b.readers = []
        for b in reads:
            b.readers.append(tok)

    def op(self, engname, fn, reads=(), writes=()):
        E = self.E[engname]
        self._wait(E, self._deps(reads, writes))
        ins = fn(E.eng)
        E.cnt += 1
        ins.then_inc(E.sem, 1)
        self._commit((E.sem, E.cnt, engname), reads, writes)
        return ins

    def dma(self, qname, out, in_, reads=(), writes=(), **kw):
        E = self.E[qname]
        slot = self.dsems[self.drr]
        self.drr = (self.drr + 1) % len(self.dsems)
        deps = self._deps(reads, writes)
        if slot[1] > 0:
            deps.append((slot[0], slot[1], "dma"))
        self._wait(E, deps)
        ins = E.eng.dma_start(out=out, in_=in_, **kw)
        slot[1] += 16
        ins.then_inc(slot[0], 16)
        self._commit((slot[0], slot[1], "dma"), reads, writes)
        return ins

    def barrier(self):
        toks = [(E.sem, E.cnt, n) for n, E in self.E.items() if E.cnt > 0]
        toks += [(d[0], d[1], "dma") for d in self.dsems if d[1] > 0]
        for E in self.E.values():
            self._wait(E, [t for t in toks if t[2] != E.name])

    def finish(self, bufs):
        deps = []
        for b in bufs:
            if b.writer is not None:
                deps.append(b.writer)
            deps.extend(b.readers)
        self._wait(self.E["sp"], deps)


def make_consts():
    lg = np.log1p(-np.exp2(-5.0 - np.arange(8, dtype=np.float64)))
    cst = np.zeros((128, NCST), np.float64)
    invf = np.float32(10000.0) ** (-(np.arange(0, 256, 2, dtype=np.float32)) / np.float32(256.0))
    cst[:, C_INVF:C_INVF + 128] = invf[None, :].astype(np.float64)
    idx = np.arange(128, dtype=np.float64)
    for h in range(8):
        diff = idx[None, :] - idx[:, None]
        dec = np.where(diff >= 0, np.exp(lg[h] * np.maximum(diff, 0.0)), 0.0) / 16.0
        cst[:, C_DEC + h * 128:C_DEC + (h + 1) * 128] = dec
        cst[:, C_XI + h] = np.exp(lg[h] * (idx + 1.0))
        cst[:, C_ZETA + h] = np.exp(lg[h] * (127.0 - idx)) / 16.0
        cst[:, C_CD + h] = np.exp(lg[h] * 128.0)
        for j in range(56):
            cst[:, C_WGT + h * 56 + j] = np.exp(lg[h] * (7167.0 - (128.0 * j + idx))) / 16.0
    cst[:, C_ID:C_ID + 128] = np.eye(128)
    return cst.astype(np.float32)


def build_program(stop_after=None, skipA=False):
    nc = bass.Bass("TRN2", target_bir_lowering=False)

    def din(name, shape, dt=F32):
        return nc.dram_tensor(name, list(shape), dt, kind="ExternalInput").ap()

    xs = din("xs", [NSLAB * SLAB, D])
    w_outs = None
    pos_pm = din("pos_pm", [128, 64], I32)
    vmask_d = din("vmask", [128, 8])
    mem_d = din("mem", [256, D])
    cst_d = din("cst", [128, NCST])
    pp_d = din("pp", [128, NPP])
    mix_g = din("mix_g", [1, D]); gn_g = din("gn_g", [1, 2048]); xat_g = din("xat_g", [1, D])
    mem_g = din("mem_g", [1, D]); moe_g = din("moe_g", [1, D]); fin_g = din("fin_g", [1, D])
    rb_d = din("rb", [1, 36])
    wr_d = din("wr", [128, KC * 36])
    w_in = din("w_in", [D, 12288] if not skipA else [1, 1])
    w_out = din("w_out", [D, D]); wq = din("wq", [D, D]); wk = din("wk", [D, D])
    wv = din("wv", [D, D]); wo = din("wo", [D, D])
    w_a = din("w_a", [8, 256, 256]); w_i = din("w_i", [8, 256, 256])
    big = stop_after is None
    nexp = NEXP if big else 2
    wg = din("wg", [nexp, D, DE]); wu = din("wu", [nexp, D, DE]); wd = din("wd", [nexp, DE, D])
    out_d = nc.dram_tensor("out", [SLAB, D], F32, kind="ExternalOutput").ap()
    dbg = stop_after is not None
    kind_s = "ExternalOutput" if dbg else "Internal"
    x1_d = nc.dram_tensor("x1_d", [SLAB, D], F32, kind=kind_s).ap()
    x2_d = nc.dram_tensor("x2_d", [SLAB, D], F32, kind=kind_s).ap()
    x3_d = nc.dram_tensor("x3_d", [SLAB, D], F32, kind="Internal").ap()
    mixT_d = nc.dram_tensor("mixT_d", [D, SLAB], BF16, kind="Internal").ap()
    yT_d = nc.dram_tensor("yT_d", [2048, SLAB], F32, kind="Internal").ap()

    with ExitStack() as st:
        S = Sched(nc, st)

        def sb(name, shape, dt, stack=st):
            return stack.enter_context(nc.sbuf_tensor("sb_" + name, list(shape), dt))

        def ps(name, shape, dt):
            return st.enter_context(nc.psum_tensor("ps_" + name, list(shape), dt))

        cst = sb("cst", [128, NCST], F32); b_cst = S.buf()
        pp = sb("pp", [128, NPP], F32); b_pp = S.buf()
        vmask = sb("vmaskt", [128, 8], F32); b_vm = S.buf()
        posi = sb("posi", [128, 64], I32); posf = sb("posf", [128, 64], F32); b_pos = S.buf()
        identb = sb("identb", [128, 128], BF16); b_idb = S.buf()
        onesf = sb("onesf", [128, 128], F32); b_ones = S.buf()
        AT = sb("AT", [128, KC, SLAB], BF16); b_AT = S.buf()
        NQ = 3
        ring = sb("ring", [128, NQ * 8192], BF16); b_ring = S.bufs(NQ)
        ring_i = [0]
        pmm = ps("pmm", [128, 4, 512], F32); b_pmm = S.bufs(4); pmm_i = [0]
        ptr = ps("ptr", [128, 1024], BF16); b_ptr = S.buf()
        ptf = ps("ptf", [128, 512], F32); b_ptf = S.buf()
        pms = ps("pms", [128, 2, 512], F32); b_pms = S.bufs(4)
        sdesc = sb("sdesc", [128, 16], F32); b_sd = S.buf()

        S.dma("sp", cst[:], cst_d, writes=[b_cst])
        S.dma("sp", pp[:], pp_d, writes=[b_pp])
        S.dma("sp", vmask[:], vmask_d, writes=[b_vm])
        S.dma("sp", posi[:], pos_pm, writes=[b_pos])
        S.op("dve", lambda e: e.tensor_copy(out=posf[:], in_=posi[:]), reads=[b_pos], writes=[b_pos])
        S.op("dve", lambda e: e.tensor_copy(out=identb[:], in_=cst[:, C_ID:C_ID + 128]), reads=[b_cst], writes=[b_idb])
        S.op("dve", lambda e: e.memset(onesf[:], 1.0), writes=[b_ones])
        identf = cst[:, C_ID:C_ID + 128]

        def next_pmm():
            i = pmm_i[0]
            pmm_i[0] = (i + 1) % 4
            return i

        def load_wslab(W2d, kc, ncols):
            assert kc * ncols == 8192
            q = ring_i[0]
            ring_i[0] = (q + 1) % NQ
            view = ring[:, q * 8192:(q + 1) * 8192].rearrange("p (k n) -> p k n", k=kc)
            S.dma("pool", view, W2d.rearrange("(k p) n -> p k n", p=128), writes=[b_ring[q]])
            return view, b_ring[q]

        def run_jobs(jobs, depth=2):
            handles = {}
            n = len(jobs)
            for i in range(min(depth, n)):
                handles[i] = jobs[i][0]()
            for i in range(n):
                jobs[i][1](handles.pop(i))
                if i + depth < n:
                    handles[i + depth] = jobs[i + depth][0]()

        def mm_tok(w, bw, A, bA, ntiles, kc, evac, ncols=256):
            for t in range(ntiles):
                pb = next_pmm()
                pv = pmm[:, pb, 0:ncols]
                for k in range(kc):
                    S.op("pe", lambda e, k=k, t=t, pv=pv: e.matmul(pv, lhsT=A[:, k, t * 128:(t + 1) * 128], rhs=w[:, k, 0:ncols],
                                                                  start=(k == 0), stop=(k == kc - 1)),
                         reads=[bA, bw], writes=[b_pmm[pb]])
                evac(t, pv, b_pmm[pb])

        def mm_feat(w, bw, A, bA, ntok, kc, evac, ncols=256):
            for cc in range(ncols // 128):
                for hf in range(max(1, ntok // 512)):
                    n = min(512, ntok)
                    pb = next_pmm()
                    pv = pmm[:, pb, 0:n]
                    for k in range(kc):
                        S.op("pe", lambda e, k=k, cc=cc, hf=hf, pv=pv, n=n: e.matmul(
                            pv, lhsT=w[:, k, cc * 128:(cc + 1) * 128], rhs=A[:, k, hf * 512:hf * 512 + n],
                            start=(k == 0), stop=(k == kc - 1)),
                             reads=[bA, bw], writes=[b_pmm[pb]])
                    evac(cc, hf, pv, b_pmm[pb])

        def rms_rstd(ss_ap, n, eps, out_ap, b):
            S.op("dve", lambda e: e.tensor_scalar(out=out_ap, in0=ss_ap, scalar1=1.0 / n, scalar2=eps,
                                                  op0=ALU.mult, op1=ALU.add), reads=[b], writes=[b])
            S.op("act", lambda e: e.activation(out=out_ap, in_=out_ap, func=AF.Sqrt), reads=[b], writes=[b])
            S.op("dve", lambda e: e.reciprocal(out=out_ap, in_=out_ap), reads=[b], writes=[b])

        tr_cnt = [0]

        def norm_transpose(ph, src_rows, ntiles, gvec, AT_dst, b_dst, col0, f32_out=None):
            with ExitStack() as ls:
                gbc = sb(ph + "gbc", [128, D], F32, ls); b_g = S.buf()
                xt = sb(ph + "xt", [128, D], F32, ls); b_xt = S.buf()
                hb = sb(ph + "hb", [128, D // 2], BF16, ls); b_hb = S.buf()
                ss = sb(ph + "ss", [128, 4], F32, ls); b_ss = S.buf()
                S.dma("sp", gbc[:], gvec.partition_broadcast(128), writes=[b_g])
                HD = D // 2
                for t in range(ntiles):
                    S.dma("sp", xt[:], src_rows[t * 128:(t + 1) * 128, :], writes=[b_xt])
                    for hh in range(2):
                        S.op("act", lambda e, hh=hh: e.activation(out=hb[:], in_=xt[:, hh * HD:(hh + 1) * HD], func=AF.Square), reads=[b_xt], writes=[b_hb])
                        S.op("dve", lambda e, hh=hh: e.reduce_sum(out=ss[:, 2 + hh:3 + hh], in_=hb[:], axis=AX.X), reads=[b_hb], writes=[b_ss])
                    S.op("dve", lambda e: e.tensor_tensor(out=ss[:, 0:1], in0=ss[:, 2:3], in1=ss[:, 3:4], op=ALU.add), reads=[b_ss], writes=[b_ss])
                    rms_rstd(ss[:, 0:1], float(D), 1e-6, ss[:, 1:2], b_ss)
                    for hh in range(2):
                        S.op("dve", lambda e, hh=hh: e.scalar_tensor_tensor(out=hb[:], in0=xt[:, hh * HD:(hh + 1) * HD], scalar=ss[:, 1:2],
                                                                            in1=gbc[:, hh * HD:(hh + 1) * HD], op0=ALU.mult, op1=ALU.mult),
                             reads=[b_xt, b_ss, b_g], writes=[b_hb])
                        for g8 in range(2):
                            for j in range(8):
                                k = g8 * 8 + j
                                S.op("pe", lambda e, k=k, j=j: e.transpose(out=ptr[:, j * 128:(j + 1) * 128],
                                                                           in_=hb[:, k * 128:(k + 1) * 128], identity=identb[:]),
                                     reads=[b_hb, b_idb], writes=[b_ptr])
                            eng = "act" if (tr_cnt[0] % 2 == 0) else "dve"
                            tr_cnt[0] += 1
                            k0 = hh * 16 + g8 * 8
                            dst = AT_dst[:, k0:k0 + 8, col0 + t * 128:col0 + (t + 1) * 128]
                            src = ptr[:, :].rearrange("p (k n) -> p k n", k=8)
                            if eng == "act":
                                S.op("act", lambda e, dst=dst, src=src: e.activation(out=dst, in_=src, func=AF.Copy),
                                     reads=[b_ptr], writes=[b_dst])
                            else:
                                S.op("dve", lambda e, dst=dst, src=src: e.tensor_copy(out=dst, in_=src),
                                     reads=[b_ptr], writes=[b_dst])
                S.barrier()

        with ExitStack() as pa:
          if skipA:
            b_mixT = S.bufs(32, "mixT")
          else:
              Racc = sb("Racc", [128, 16, 256], F32, pa); b_R = S.bufs(16)
              for i in range(16):
                  S.op("dve", lambda e, i=i: e.memset(Racc[:, i, :], 0.0), writes=[b_R[i]])
              lstate = sb("lstate", [128, 16], F32, pa); b_ls = S.bufs(16)
              halo = sb("halo", [128, 16, 4], F32, pa); b_halo = S.bufs(16)
              S.op("dve", lambda e: e.memset(lstate[:], 0.0), writes=b_ls)
              S.op("dve", lambda e: e.memset(halo[:], 0.0), writes=b_halo)
              wab = wib = b_wab = None
              lsc = sb("lsc", [128, 48], F32, pa); b_lsc = S.buf()
              S.op("act", lambda e: e.activation(out=lsc[:, 0:16], in_=pp[:, P_LAM:P_LAM + 16], func=AF.Exp, scale=-1.0),
                   reads=[b_pp], writes=[b_lsc])
              S.op("act", lambda e: e.activation(out=lsc[:, 0:16], in_=lsc[:, 0:16], func=AF.Ln, bias=1.0),
                   reads=[b_lsc], writes=[b_lsc])
              S.op("dve", lambda e: e.tensor_scalar(out=lsc[:, 16:32], in0=lsc[:, 0:16], scalar1=-8.0, scalar2=None, op0=ALU.mult),
                   reads=[b_lsc], writes=[b_lsc])
              S.op("dve", lambda e: e.tensor_scalar(out=lsc[:, 32:48], in0=lsc[:, 0:16], scalar1=-16.0, scalar2=None, op0=ALU.mult),
                   reads=[b_lsc], writes=[b_lsc])
              cosT = sinT = b_cs = rtmp = b_rt = rki = kt = b_kt = vt = b_vt = rpt = b_rpt = None
              ret_n = [0]

              def alloc_ret(stack):
                  nonlocal cosT, sinT, b_cs, rtmp, b_rt, rki, kt, b_kt, vt, b_vt, rpt, b_rpt
                  n = "%d" % ret_n[0]; ret_n[0] += 1
                  cosT = sb("cosT" + n, [128, NT, 128], F32, stack); sinT = sb("sinT" + n, [128, NT, 128], F32, stack); b_cs = S.buf()
                  rtmp = sb("rtmp" + n, [128, 3, 128], F32, stack); b_rt = S.buf()
                  rki = sb("rki" + n, [128, 128], I32, stack)
                  kt = sb("kt" + n, [128, NT, 256], BF16, stack); b_kt = S.buf()
                  vt = sb("vt" + n, [128, NT, 256], BF16, stack); b_vt = S.buf()
                  rpt = sb("rpt" + n, [128, 2, 256], F32, stack); b_rpt = S.buf()
              LB = {}
              lru_n = [0]

              def alloc_lru(stack):
                  nonlocal wab, wib, b_wab
                  n = lru_n[0]; lru_n[0] += 1
                  wab = sb("wab%d" % n, [128, 8, 2, 256], BF16, stack); wib = sb("wib%d" % n, [128, 8, 2, 256], BF16, stack); b_wab = S.buf()
                  S.dma("pool", wab[:], w_a.rearrange("b (c p) d -> p b c d", p=128), writes=[b_wab])
                  S.dma("pool", wib[:], w_i.rearrange("b (c p) d -> p b c d", p=128), writes=[b_wab])
                  LB["xb"] = sb("xb%d" % n, [128, 2, 1028], F32, stack); LB["b_xb"] = S.bufs(2)
                  LB["xc"] = sb("xc%d" % n, [128, 2, 1024], F32, stack); LB["b_xc"] = S.bufs(2)
                  LB["xcb"] = sb("xcb%d" % n, [128, 2, 1024], BF16, stack); LB["b_xcb"] = S.bufs(2)
                  for nm in ("lr", "li", "l2", "lh"):
                      LB[nm] = sb(nm + "%d" % n, [128, 1024], F32, stack); LB["b_" + nm] = S.buf()

              def rope_tables(j):
                  for t in range(NT):
                      pcol = posf[:, j * NT + t:j * NT + t + 1]
                      ang = rtmp[:, 0, :]; y = rtmp[:, 1, :]; kf = rtmp[:, 2, :]
                      S.op("dve", lambda e, pcol=pcol: e.tensor_scalar(out=ang, in0=cst[:, C_INVF:C_INVF + 128], scalar1=pcol,
                                                                       scalar2=None, op0=ALU.mult), reads=[b_cst, b_pos], writes=[b_rt])
                      S.op("dve", lambda e: e.tensor_scalar(out=y, in0=ang, scalar1=float(1.0 / (2 * np.pi)), scalar2=0.5,
                                                            op0=ALU.mult, op1=ALU.add), reads=[b_rt], writes=[b_rt])
                      S.op("dve", lambda e: e.tensor_copy(out=rki[:], in_=y), reads=[b_rt], writes=[b_rt])
                      S.op("dve", lambda e: e.tensor_copy(out=kf, in_=rki[:]), reads=[b_rt], writes=[b_rt])
                      S.op("dve", lambda e: e.scalar_tensor_tensor(out=y, in0=kf, scalar=-6.28125, in1=ang, op0=ALU.mult, op1=ALU.add),
                           reads=[b_rt], writes=[b_rt])
                      S.op("dve", lambda e: e.scalar_tensor_tensor(out=y, in0=kf, scalar=-0.0019353071795864769, in1=y,
                                                                   op0=ALU.mult, op1=ALU.add), reads=[b_rt], writes=[b_rt])
                      S.op("dve", lambda e: e.tensor_scalar(out=kf, in0=y, scalar1=-float(np.pi), scalar2=float(2 * np.pi),
                                                            op0=ALU.is_lt, op1=ALU.mult), reads=[b_rt], writes=[b_rt])
                      S.op("dve", lambda e: e.tensor_tensor(out=y, in0=y, in1=kf, op=ALU.add), reads=[b_rt], writes=[b_rt])
                      S.op("dve", lambda e: e.tensor_scalar(out=kf, in0=y, scalar1=float(np.pi), scalar2=-float(2 * np.pi),
                                                            op0=ALU.is_gt, op1=ALU.mult), reads=[b_rt], writes=[b_rt])
                      S.op("dve", lambda e: e.tensor_tensor(out=y, in0=y, in1=kf, op=ALU.add), reads=[b_rt], writes=[b_rt])
                      S.op("act", lambda e, t=t: e.activation(out=sinT[:, t, :], in_=y, func=AF.Sin), reads=[b_rt], writes=[b_cs])
                      S.op("act", lambda e: e.activation(out=kf, in_=y, func=AF.Abs), reads=[b_rt], writes=[b_rt])
                      S.op("act", lambda e, t=t: e.activation(out=cosT[:, t, :], in_=kf, func=AF.Sin, scale=-1.0, bias=float(np.pi / 2)),
                           reads=[b_rt], writes=[b_cs])

              def rope_evac(dst, bdst):
                  def f(t, pv, bp):
                      t1 = pv[:, 0:128]; t2 = pv[:, 128:256]
                      a = rpt[:, 0, 0:128]; b = rpt[:, 0, 128:256]; c = rpt[:, 1, 0:128]; d = rpt[:, 1, 128:256]
                      S.op("dve", lambda e: e.tensor_tensor(out=a, in0=t1, in1=cosT[:, t, :], op=ALU.mult), reads=[bp, b_cs], writes=[b_rpt])
                      S.op("dve", lambda e: e.tensor_tensor(out=b, in0=t2, in1=sinT[:, t, :], op=ALU.mult), reads=[bp, b_cs], writes=[b_rpt])
                      S.op("dve", lambda e: e.tensor_tensor(out=c, in0=t1, in1=sinT[:, t, :], op=ALU.mult), reads=[bp, b_cs], writes=[b_rpt])
                      S.op("dve", lambda e: e.tensor_tensor(out=d, in0=t2, in1=cosT[:, t, :], op=ALU.mult), reads=[bp, b_cs], writes=[b_rpt])
                      S.op("dve", lambda e: e.tensor_tensor(out=dst[:, t, 0:128], in0=a, in1=b, op=ALU.subtract), reads=[b_rpt], writes=[bdst])
                      S.op("dve", lambda e: e.tensor_tensor(out=dst[:, t, 128:256], in0=c, in1=d, op=ALU.add), reads=[b_rpt], writes=[bdst])
                  return f

              def copy_evac(dst, bdst, eng="act"):
                  def f(t, pv, bp):
                      if eng == "act":
                          S.op("act", lambda e: e.activation(out=dst[:, t, :], in_=pv, func=AF.Copy), reads=[bp], writes=[bdst])
                      else:
                          S.op("dve", lambda e: e.tensor_copy(out=dst[:, t, :], in_=pv), reads=[bp], writes=[bdst])
                  return f

              def lru_block(j, blk, own, gate_handles=None):
                  xb, xc, xcb, lr, li, l2, lh = (LB[n_] for n_ in ("xb", "xc", "xcb", "lr", "li", "l2", "lh"))
                  b_xb, b_xc, b_xcb, b_lr, b_li, b_l2, b_lh = (LB["b_" + n_] for n_ in ("xb", "xc", "xcb", "lr", "li", "l2", "lh"))
                  for cc in range(2):
                      ch = blk * 2 + cc
                      S.op("dve", lambda e, cc=cc, ch=ch: e.tensor_copy(out=xb[:, cc, 0:4], in_=halo[:, ch, :]),
                           reads=[b_halo[ch]], writes=[b_xb[cc]])
                      cw = lambda jj, ch=ch: pp[:, P_CW + ch * 4 + jj:P_CW + ch * 4 + jj + 1]
                      S.op("dve", lambda e, cc=cc, ch=ch: e.tensor_scalar(out=xc[:, cc, :], in0=xb[:, cc, 4:1028], scalar1=cw(3),
                                                                          scalar2=pp[:, P_CB + ch:P_CB + ch + 1], op0=ALU.mult, op1=ALU.add),
                           reads=[b_xb[cc], b_pp], writes=[b_xc[cc]])
                      for jj in range(3):
                          S.op("dve", lambda e, cc=cc, jj=jj: e.scalar_tensor_tensor(out=xc[:, cc, :], in0=xb[:, cc, 1 + jj:1025 + jj],
                                                                                     scalar=cw(jj), in1=xc[:, cc, :], op0=ALU.mult, op1=ALU.add),
                               reads=[b_xb[cc], b_pp, b_xc[cc]], writes=[b_xc[cc]])
                      S.op("dve", lambda e, cc=cc, ch=ch: e.tensor_copy(out=halo[:, ch, :], in_=xb[:, cc, 1024:1028]),
                           reads=[b_xb[cc]], writes=[b_halo[ch]])
                      S.op("act", lambda e, cc=cc: e.activation(out=xcb[:, cc, :], in_=xc[:, cc, :], func=AF.Copy),
                           reads=[b_xc[cc]], writes=[b_xcb[cc]])
                  for dc in range(2):
                      ch = blk * 2 + dc
                      for (wmat, bias_off, dst, bd) in ((wab, P_BA, lr, b_lr), (wib, P_BI, li, b_li)):
                          for hf in range(2):
                              pb = next_pmm(); pv = pmm[:, pb, :]
                              for c2 in range(2):
                                  S.op("pe", lambda e, c2=c2, hf=hf, pv=pv, wmat=wmat, dc=dc: e.matmul(
                                      pv, lhsT=wmat[:, blk, c2, dc * 128:(dc + 1) * 128], rhs=xcb[:, c2, hf * 512:(hf + 1) * 512],
                                      start=(c2 == 0), stop=(c2 == 1)), reads=[b_wab, b_xcb[0], b_xcb[1]], writes=[b_pmm[pb]])
                              S.op("act", lambda e, hf=hf, pv=pv, dst=dst, bias_off=bias_off, ch=ch: e.activation(
                                  out=dst[:, hf * 512:(hf + 1) * 512], in_=pv, func=AF.Sigmoid,
                                  bias=pp[:, bias_off + ch:bias_off + ch + 1]), reads=[b_pmm[pb], b_pp], writes=[bd])
                      S.op("act", lambda e, ch=ch: e.activation(out=l2[:], in_=lr[:], func=AF.Exp, scale=lsc[:, 32 + ch:33 + ch]),
                           reads=[b_lr, b_lsc], writes=[b_l2])
                      S.op("act", lambda e, ch=ch: e.activation(out=lr[:], in_=lr[:], func=AF.Exp, scale=lsc[:, 16 + ch:17 + ch]),
                           reads=[b_lr, b_lsc], writes=[b_lr])
                      S.op("dve", lambda e: e.tensor_scalar(out=l2[:], in0=l2[:], scalar1=-1.0, scalar2=1.0, op0=ALU.mult, op1=ALU.add),
                           reads=[b_l2], writes=[b_l2])
                      S.op("dve", lambda e: e.tensor_scalar(out=l2[:], in0=l2[:], scalar1=0.0, scalar2=None, op0=ALU.max),
                           reads=[b_l2], writes=[b_l2])
                      S.op("act", lambda e: e.activation(out=l2[:], in_=l2[:], func=AF.Sqrt), reads=[b_l2], writes=[b_l2])
                      S.op("dve", lambda e: e.scalar_tensor_tensor(out=li[:], in0=l2[:], scalar=vmask[:, j:j + 1], in1=li[:],
                                                                   op0=ALU.mult, op1=ALU.mult), reads=[b_l2, b_li, b_vm], writes=[b_li])
                      S.op("dve", lambda e, dc=dc: e.tensor_tensor(out=l2[:], in0=li[:], in1=xc[:, dc, :], op=ALU.mult),
                           reads=[b_li, b_xc[dc]], writes=[b_l2])
                      S.op("dve", lambda e, ch=ch: e.tensor_tensor_scan(out=lh[:], data0=lr[:], data1=l2[:], initial=lstate[:, ch:ch + 1],
                                                                        op0=ALU.mult, op1=ALU.add),
                           reads=[b_lr, b_l2, b_ls[ch]], writes=[b_lh])
                      S.op("dve", lambda e, ch=ch: e.tensor_copy(out=lstate[:, ch:ch + 1], in_=lh[:, 1023:1024]),
                           reads=[b_lh], writes=[b_ls[ch]])
                      if own:
                          own_lru_out(ch, dc)

              own_ctx = {}

              def own_lru_out(ch, dc):
                  xb, xc, xcb, lr, li, l2, lh = (LB[n_] for n_ in ("xb", "xc", "xcb", "lr", "li", "l2", "lh"))
                  b_xb, b_xc, b_xcb, b_lr, b_li, b_l2, b_lh = (LB["b_" + n_] for n_ in ("xb", "xc", "xcb", "lr", "li", "l2", "lh"))
                  gt = own_ctx["gt"]; b_gt = own_ctx["b_gt"]; ssum = own_ctx["ssum"]; b_ssum = own_ctx["b_ssum"]
                  g = gt[:, dc, :]
                  u = li[:]
                  S.op("dve", lambda e: e.tensor_tensor(out=u, in0=g, in1=g, op=ALU.mult), reads=[b_gt[dc]], writes=[b_li])
                  S.op("dve", lambda e: e.tensor_scalar(out=u, in0=u, scalar1=0.044715, scalar2=1.0, op0=ALU.mult, op1=ALU.add),
                       reads=[b_li], writes=[b_li])
                  S.op("dve", lambda e: e.tensor_tensor(out=u, in0=u, in1=g, op=ALU.mult), reads=[b_li, b_gt[dc]], writes=[b_li])
                  S.op("act", lambda e: e.activation(out=u, in_=u, func=AF.Sigmoid, scale=1.5957691216057308), reads=[b_li], writes=[b_li])
                  S.op("dve", lambda e: e.tensor_tensor(out=u, in0=u, in1=g, op=ALU.mult), reads=[b_li, b_gt[dc]], writes=[b_li])
                  S.op("dve", lambda e: e.tensor_tensor(out=lh[:], in0=lh[:], in1=u, op=ALU.mult), reads=[b_li, b_lh], writes=[b_lh])
                  S.dma("sp", yT_d[ch * 128:(ch + 1) * 128, :], lh[:], reads=[b_lh], writes=[own_ctx["b_yT"][ch]])
                  S.op("act", lambda e: e.activation(out=l2[:], in_=lh[:], func=AF.Square), reads=[b_lh], writes=[b_l2])
                  for hf in range(2):
                      S.op("pe", lambda e, hf=hf: e.matmul(ptf[:, :], lhsT=onesf[:], rhs=l2[:, hf * 512:(hf + 1) * 512], start=True, stop=True),
                           reads=[b_ones, b_l2], writes=[b_ptf])
                      S.op("dve", lambda e, hf=hf: e.tensor_tensor(out=ssum[:, hf * 512:(hf + 1) * 512], in0=ssum[:, hf * 512:(hf + 1) * 512],
                                                                   in1=ptf[:, :], op=ALU.add), reads=[b_ptf, b_ssum], writes=[b_ssum])

              def xb_evac(cc_base):
                  def f(cc, hf, pv, bp):
                      xb = LB["xb"]; b_xb = LB["b_xb"]
                      S.op("act", lambda e: e.activation(out=xb[:, cc, 4 + hf * 512:4 + (hf + 1) * 512], in_=pv, func=AF.Copy),
                           reads=[bp], writes=[b_xb[cc]])
                  return f

              for j in range(NSLAB - 1):
                  norm_transpose("pa%d" % j, xs[j * SLAB:(j + 1) * SLAB, :], NT, mix_g, AT, b_AT, 0)
                  rsc_ = ExitStack()
                  alloc_ret(rsc_)
                  rope_tables(j)
                  jobs = []
                  for h in range(8):
                      def mk(h):
                          def ld_k():
                              return load_wslab(w_in[:, 2048 + h * 256:2048 + (h + 1) * 256], KC, 256)

                          def cp_k(hd):
                              mm_tok(hd[0], hd[1], AT, b_AT, NT, KC, rope_evac(kt, b_kt))
                              for t in range(NT):
                                  S.op("dve", lambda e, t=t: e.tensor_scalar(out=kt[:, t, :], in0=kt[:, t, :],
                                                                             scalar1=cst[:, C_WGT + h * 56 + j * NT + t:C_WGT + h * 56 + j * NT + t + 1],
                                                                             scalar2=None, op0=ALU.mult), reads=[b_kt, b_cst], writes=[b_kt])

                          def ld_v():
                              return load_wslab(w_in[:, 4096 + h * 256:4096 + (h + 1) * 256], KC, 256)

                          def cp_v(hd):
                              mm_tok(hd[0], hd[1], AT, b_AT, NT, KC, copy_evac(vt, b_vt))
                              for dc in range(2):
                                  r = dc
                                  pv = pms[:, r, 0:256]
                                  for t in range(NT):
                                      S.op("pe", lambda e, t=t, dc=dc, pv=pv: e.matmul(pv, lhsT=kt[:, t, dc * 128:(dc + 1) * 128], rhs=vt[:, t, :],
                                                                                      start=(t == 0), stop=(t == NT - 1)),
                                           reads=[b_kt, b_vt], writes=[b_pms[r]])
                                  S.op("dve", lambda e, dc=dc, pv=pv: e.tensor_tensor(out=Racc[:, h * 2 + dc, :], in0=Racc[:, h * 2 + dc, :], in1=pv, op=ALU.add),
                                       reads=[b_pms[r]], writes=[b_R[h * 2 + dc]])
                          return [(ld_k, cp_k), (ld_v, cp_v)]
                      jobs += mk(h)
                  run_jobs(jobs)
                  S.barrier()
                  rsc_.close()
                  jobs = []
                  lsc_ = ExitStack()
                  alloc_lru(lsc_)
                  for blk in range(8):
                      def mkl(blk):
                          def ld():
                              return load_wslab(w_in[:, 8192 + blk * 256:8192 + (blk + 1) * 256], KC, 256)

                          def cp(hd):
                              mm_feat(hd[0], hd[1], AT, b_AT, SLAB, KC, xb_evac(0))
                              lru_block(j, blk, False)
                          return [(ld, cp)]
                      jobs += mkl(blk)
                  run_jobs(jobs)
                  S.barrier()
                  lsc_.close()

              j = NSLAB - 1
              norm_transpose("pa7", xs[j * SLAB:(j + 1) * SLAB, :], NT, mix_g, AT, b_AT, 0)
              with ExitStack() as po:
                  alloc_ret(po)
                  rope_tables(j)
                  qt = sb("qt", [128, NT, 256], BF16, po); b_qt = S.buf()
                  sg = sb("sg", [128, NT, 256], F32, po); b_sg = S.buf()
                  qT = sb("qT", [128, 2, SLAB], BF16, po); b_qT = S.buf()
                  kT = sb("kT", [128, 2, SLAB], BF16, po); b_kT = S.buf()
                  Rb = sb("Rb", [128, 2, 256], BF16, po); b_Rb = S.buf()
                  PT = sb("PT", [128, 128], BF16, po); b_PT = S.buf()
                  oc = sb("oc", [128, 256], F32, po); b_oc = S.buf()
                  osb = sb("osb", [128, 256], F32, po); b_osb = S.buf()
                  rtb = sb("rtb", [128, 256], BF16, po); b_rtb = S.buf()
                  rTs = sb("rTs", [128, 2, 128], BF16, po); b_rTs = S.buf()
                  kz = sb("kz", [128, 256], BF16, po); b_kz = S.buf()
                  gng = sb("gng", [128, 2048], F32, po); b_gng = S.buf()
                  st6 = sb("st6", [128, 16], F32, po); b_st6 = S.buf()
                  b_mixT = S.bufs(32, "mixT")
                  S.dma("sp", gng[:], gn_g.partition_broadcast(128), writes=[b_gng])

                  def retention_head(h):
                      for (src, bs, dstT, bdT) in ((qt, b_qt, qT, b_qT), (kt, b_kt, kT, b_kT)):
                          for t in range(NT):
                              for dc in range(2):
                                  S.op("pe", lambda e, t=t, dc=dc, src=src: e.transpose(out=ptr[:, dc * 128:(dc + 1) * 128],
                                                                                      in_=src[:, t, dc * 128:(dc + 1) * 128], identity=identb[:]),
                                       reads=[bs, b_idb], writes=[b_ptr])
                              S.op("act", lambda e, t=t, dstT=dstT: e.activation(out=dstT[:, :, t * 128:(t + 1) * 128],
                                                                                 in_=ptr[:, 0:256].rearrange("p (k n) -> p k n", k=2), func=AF.Copy),
                                   reads=[b_ptr], writes=[bdT])
                      for dc in range(2):
                          S.op("act", lambda e, dc=dc: e.activation(out=Rb[:, dc, :], in_=Racc[:, h * 2 + dc, :], func=AF.Copy),
                               reads=[b_R[h * 2 + dc]], writes=[b_Rb])
                      for i in range(NT):
                          cs = slice(i * 128, (i + 1) * 128)
                          pST = pms[:, 0, 0:128]
                          for dc in range(2):
                              S.op("pe", lambda e, dc=dc: e.matmul(pST, lhsT=kT[:, dc, cs], rhs=qT[:, dc, cs], start=(dc == 0), stop=(dc == 1)),
                                   reads=[b_kT, b_qT], writes=[b_pms[0]])
                          S.op("dve", lambda e: e.tensor_tensor(out=PT[:], in0=pST, in1=cst[:, C_DEC + h * 128:C_DEC + (h + 1) * 128], op=ALU.mult),
                               reads=[b_pms[0], b_cst], writes=[b_PT])
                          pOI = pms[:, 0, 256:512]
                          S.op("pe", lambda e: e.matmul(pOI, lhsT=PT[:], rhs=vt[:, i, :], start=True, stop=True),
                               reads=[b_PT, b_vt], writes=[b_pms[1]])
                          pOC = pms[:, 1, 0:256]
                          for dc in range(2):
                              S.op("pe", lambda e, dc=dc: e.matmul(pOC, lhsT=qT[:, dc, cs], rhs=Rb[:, dc, :], start=(dc == 0), stop=(dc == 1)),
                                   reads=[b_qT, b_Rb], writes=[b_pms[2]])
                          S.op("act", lambda e: e.activation(out=oc[:], in_=pOC, func=AF.Copy, scale=cst[:, C_XI + h:C_XI + h + 1]),
                               reads=[b_pms[2], b_cst], writes=[b_oc])
                          S.op("dve", lambda e: e.tensor_tensor(out=osb[:], in0=pOI, in1=oc[:], op=ALU.add),
                               reads=[b_pms[1], b_oc], writes=[b_osb])
                          S.op("dve", lambda e: e.bn_stats(out=st6[:, 0:6], in_=osb[:]), reads=[b_osb], writes=[b_st6])
                          S.op("dve", lambda e: e.bn_aggr(out=st6[:, 8:10], in_=st6[:, 0:6]), reads=[b_st6], writes=[b_st6])
                          S.op("dve", lambda e: e.tensor_scalar(out=st6[:, 10:11], in0=st6[:, 9:10], scalar1=1e-5, scalar2=None, op0=ALU.add),
                               reads=[b_st6], writes=[b_st6])
                          S.op("act", lambda e: e.activation(out=st6[:, 10:11], in_=st6[:, 10:11], func=AF.Sqrt), reads=[b_st6], writes=[b_st6])
                          S.op("dve", lambda e: e.reciprocal(out=st6[:, 11:12], in_=st6[:, 10:11]), reads=[b_st6], writes=[b_st6])
                          S.op("dve", lambda e: e.tensor_scalar(out=osb[:], in0=osb[:], scalar1=st6[:, 8:9], scalar2=st6[:, 11:12],
                                                                op0=ALU.subtract, op1=ALU.mult), reads=[b_osb, b_st6], writes=[b_osb])
                          S.op("dve", lambda e: e.tensor_tensor(out=osb[:], in0=osb[:], in1=gng[:, h * 256:(h + 1) * 256], op=ALU.mult),
                               reads=[b_osb, b_gng], writes=[b_osb])
                          S.op("dve", lambda e: e.tensor_tensor(out=rtb[:], in0=osb[:], in1=sg[:, i, :], op=ALU.mult),
                               reads=[b_osb, b_sg], writes=[b_rtb])
                          for dc in range(2):
                              S.op("pe", lambda e, dc=dc: e.transpose(out=ptr[:, 512 + dc * 128:512 + (dc + 1) * 128], in_=rtb[:, dc * 128:(dc + 1) * 128],
                                                                      identity=identb[:]), reads=[b_rtb, b_idb], writes=[b_ptr])
                          S.op("act", lambda e: e.activation(out=rTs[:], in_=ptr[:, 512:768].rearrange("p (k n) -> p k n", k=2), func=AF.Copy),
                               reads=[b_ptr], writes=[b_rTs])
                          for dc in range(2):
                              r0 = h * 256 + dc * 128
                              S.dma("sp", mixT_d[r0:r0 + 128, cs], rTs[:, dc, :], reads=[b_rTs], writes=[b_mixT[h * 2 + dc]])
                          if i < NT - 1:
                              S.op("dve", lambda e: e.tensor_scalar(out=kz[:], in0=kt[:, i, :], scalar1=cst[:, C_ZETA + h:C_ZETA + h + 1],
                                                                    scalar2=None, op0=ALU.mult), reads=[b_kt, b_cst], writes=[b_kz])
                              for dc in range(2):
                                  pRU = pms[:, 1, 256:512]
                                  S.op("pe", lambda e, dc=dc: e.matmul(pRU, lhsT=kz[:, dc * 128:(dc + 1) * 128], rhs=vt[:, i, :], start=True, stop=True),
                                       reads=[b_kz, b_vt], writes=[b_pms[3]])
                                  S.op("dve", lambda e, dc=dc: e.scalar_tensor_tensor(out=Racc[:, h * 2 + dc, :], in0=Racc[:, h * 2 + dc, :],
                                                                                      scalar=cst[:, C_CD + h:C_CD + h + 1], in1=pRU,
                                                                                      op0=ALU.mult, op1=ALU.add),
                                       reads=[b_pms[3], b_cst], writes=[b_R[h * 2 + dc]])
                                  S.op("act", lambda e, dc=dc: e.activation(out=Rb[:, dc, :], in_=Racc[:, h * 2 + dc, :], func=AF.Copy),
                                       reads=[b_R[h * 2 + dc]], writes=[b_Rb])

                  jobs = []
                  for h in range(8):
                      def mko(h):
                          def ld(c0):
                              return lambda: load_wslab(w_in[:, c0 + h * 256:c0 + (h + 1) * 256], KC, 256)

                          def cp_q(hd):
                              mm_tok(hd[0], hd[1], AT, b_AT, NT, KC, rope_evac(qt, b_qt))

                          def cp_k(hd):
                              mm_tok(hd[0], hd[1], AT, b_AT, NT, KC, rope_evac(kt, b_kt))

                          def cp_v(hd):
                              mm_tok(hd[0], hd[1], AT, b_AT, NT, KC, copy_evac(vt, b_vt))

                          def cp_g(hd):
                              def ev(t, pv, bp):
                                  S.op("act", lambda e: e.activation(out=sg[:, t, :], in_=pv, func=AF.Silu), reads=[bp], writes=[b_sg])
                              mm_tok(hd[0], hd[1], AT, b_AT, NT, KC, ev)
                              retention_head(h)
                          return [(ld(0), cp_q), (ld(2048), cp_k), (ld(4096), cp_v), (ld(6144), cp_g)]
                      jobs += mko(h)
                  run_jobs(jobs)
                  S.barrier()

              with ExitStack() as pl:
                  alloc_lru(pl)
                  gt = sb("gt", [128, 2, 1024], F32, pl); b_gt = S.bufs(2)
                  ssum = sb("ssum", [128, 1024], F32, pl); b_ssum = S.buf()
                  b_yT = S.bufs(16, "yT")
                  S.op("dve", lambda e: e.memset(ssum[:], 0.0), writes=[b_ssum])
                  own_ctx.update(gt=gt, b_gt=b_gt, ssum=ssum, b_ssum=b_ssum, b_yT=b_yT)
                  jobs = []
                  for blk in range(8):
                      def mkl2(blk):
                          def ld_x():
                              return load_wslab(w_in[:, 8192 + blk * 256:8192 + (blk + 1) * 256], KC, 256)

                          def cp_x(hd):
                              mm_feat(hd[0], hd[1], AT, b_AT, SLAB, KC, xb_evac(0))

                          def ld_g():
                              return load_wslab(w_in[:, 10240 + blk * 256:10240 + (blk + 1) * 256], KC, 256)

                          def cp_g(hd):
                              def ev(cc, hf, pv, bp):
                                  S.op("act", lambda e: e.activation(out=gt[:, cc, hf * 512:(hf + 1) * 512], in_=pv, func=AF.Copy),
                                       reads=[bp], writes=[b_gt[cc]])
                              mm_feat(hd[0], hd[1], AT, b_AT, SLAB, KC, ev)
                              lru_block(NSLAB - 1, blk, True)
                          return [(ld_x, cp_x), (ld_g, cp_g)]
                      jobs += mkl2(blk)
                  run_jobs(jobs)
                  rms_rstd(ssum[:], 2048.0, 1e-6, ssum[:], b_ssum)
                  lh = LB["lh"]; b_lh = LB["b_lh"]; xcb = LB["xcb"]; b_xcb = LB["b_xcb"]
                  for ch in range(16):
                      S.dma("sp", lh[:], yT_d[ch * 128:(ch + 1) * 128, :], reads=[b_yT[ch]], writes=[b_lh])
                      S.op("dve", lambda e, ch=ch: e.scalar_tensor_tensor(out=xcb[:, 0, :], in0=lh[:], scalar=pp[:, P_LG + ch:P_LG + ch + 1],
                                                                          in1=ssum[:], op0=ALU.mult, op1=ALU.mult),
                           reads=[b_lh, b_pp, b_ssum], writes=[b_xcb[0]])
                      S.dma("sp", mixT_d[2048 + ch * 128:2048 + (ch + 1) * 128, :], xcb[:, 0, :], reads=[b_xcb[0]], writes=[b_mixT[16 + ch]])
                  S.barrier()
              S.barrier()

        b_x1 = [[S.buf() for _ in range(16)] for _ in range(NT)]
        b_x2 = [[S.buf() for _ in range(16)] for _ in range(NT)]

        def load_AT_from(dram_T, deps):
            for k in range(KC):
                S.dma("sp", AT[:, k, :], dram_T[k * 128:(k + 1) * 128, :], reads=[deps[k]], writes=[b_AT])

        def resid_linear(ph, W, src_rows, src_bufs, dst_rows=None, dst_bufs=None):
            dst_rows = x1_d if dst_rows is None else dst_rows
            dst_bufs = b_x1 if dst_bufs is None else dst_bufs
            with ExitStack() as ls:
                rs = sb(ph + "rs", [128, 4, 256], F32, ls); b_rs = S.bufs(4)
                ri = [0]
                jobs = []
                for s in range(16):
                    def mk(s):
                        def ld():
                            return load_wslab(W[:, s * 256:(s + 1) * 256], KC, 256)

                        def cp(hd):
                            def ev(t, pv, bp):
                                r = ri[0]; ri[0] = (r + 1) % 4
                                S.dma("sp", rs[:, r, :], src_rows[t * 128:(t + 1) * 128, s * 256:(s + 1) * 256],
                                      reads=[src_bufs[t][s]] if src_bufs else [], writes=[b_rs[r]])
                                S.op("dve", lambda e: e.tensor_tensor(out=rs[:, r, :], in0=pv, in1=rs[:, r, :], op=ALU.add),
                                     reads=[bp, b_rs[r]], writes=[b_rs[r]])
                                S.dma("sp", dst_rows[t * 128:(t + 1) * 128, s * 256:(s + 1) * 256], rs[:, r, :], reads=[b_rs[r]], writes=[dst_bufs[t][s]])
                            mm_tok(hd[0], hd[1], AT, b_AT, NT, KC, ev)
                        return (ld, cp)
                    jobs.append(mk(s))
                run_jobs(jobs)
                S.barrier()

        load_AT_from(mixT_d, b_mixT)
        resid_linear("wo1", w_out, xs[(NSLAB - 1) * SLAB:NSLAB * SLAB, :], None)

        if stop_after == "A":
            S.finish([b for row in b_x1 for b in row])
            return nc

        with ExitStack() as pb_:
            memT = sb("memT", [128, KC, 256], BF16, pb_); b_memT = S.buf()
            norm_transpose("pm", mem_d, 2, mem_g, memT, b_memT, 0)
            KTm = sb("KTm", [128, KC, 256], BF16, pb_); b_KT = S.buf()
            Vm = sb("Vm", [128, 2, D], BF16, pb_); b_Vm = S.buf()
            jobs = []
            for s in range(16):
                def mkk(s):
                    def ld():
                        return load_wslab(wk[:, s * 256:(s + 1) * 256], KC, 256)

                    def cp(hd):
                        def ev(cc, hf, pv, bp):
                            S.op("act", lambda e: e.activation(out=KTm[:, s * 2 + cc, :], in_=pv, func=AF.Copy), reads=[bp], writes=[b_KT])
                        mm_feat(hd[0], hd[1], memT, b_memT, 256, KC, ev)
                    return (ld, cp)

                def mkv(s):
                    def ld():
                        return load_wslab(wv[:, s * 256:(s + 1) * 256], KC, 256)

                    def cp(hd):
                        def ev(t, pv, bp):
                            S.op("act", lambda e: e.activation(out=Vm[:, t, s * 256:(s + 1) * 256], in_=pv, func=AF.Copy), reads=[bp], writes=[b_Vm])
                        mm_tok(hd[0], hd[1], memT, b_memT, 2, KC, ev)
                    return (ld, cp)
                jobs += [mkk(s), mkv(s)]
            run_jobs(jobs)

            x1_all = [b for row in b_x1 for b in row]
            b_x1tile = S.buf()
            for b in x1_all:
                pass
            sync_tok = S.buf()
            S.op("dve", lambda e: e.memset(sdesc[:, 0:1], 0.0), reads=[], writes=[b_sd])

            def norm_transpose_x1(ph, gvec):
                deps = []
                for b in x1_all:
                    if b.writer is not None:
                        deps.append(b.writer)
                S._wait(S.E["sp"], deps)
                norm_transpose(ph, x1_d, NT, gvec, AT, b_AT, 0)

            if stop_after == "B1":
                S.barrier(); S.finish([]); return nc
            norm_transpose_x1("pb", xat_g)
            if stop_after == "B2":
                S.barrier(); S.finish([]); return nc
            b_oT = S.bufs(32, "oT")
            with ExitStack() as px:
                qx = sb("qx", [128, 8, SLAB], BF16, px); b_qx = S.buf()
                prob = sb("prob", [128, 256], F32, px); b_prob = S.buf()
                probb = sb("probb", [128, 256], BF16, px); b_probb = S.buf()
                pT = sb("pT", [128, 2, SLAB], BF16, px); b_pT = S.buf()
                sm = sb("sm", [128, 8], F32, px); b_sm = S.buf()
                oTs = sb("oTs", [128, 2, 512], BF16, px); b_oTs = S.bufs(2)
                for hx in range(4):
                    jobs = []
                    for s4 in range(4):
                        def mkq(s4):
                            def ld():
                                c0 = hx * 1024 + s4 * 256
                                return load_wslab(wq[:, c0:c0 + 256], KC, 256)

                            def cp(hd):
                                def ev(cc, hf, pv, bp):
                                    S.op("act", lambda e: e.activation(out=qx[:, s4 * 2 + cc, hf * 512:(hf + 1) * 512], in_=pv, func=AF.Copy),
                                         reads=[bp], writes=[b_qx])
                                mm_feat(hd[0], hd[1], AT, b_AT, SLAB, KC, ev)
                            return (ld, cp)
                        jobs.append(mkq(s4))
                    run_jobs(jobs)
                    for t in range(NT):
                        pb = next_pmm(); pv = pmm[:, pb, 0:256]
                        for dc in range(8):
                            S.op("pe", lambda e, dc=dc, pv=pv: e.matmul(pv, lhsT=qx[:, dc, t * 128:(t + 1) * 128], rhs=KTm[:, hx * 8 + dc, :],
                                                                      start=(dc == 0), stop=(dc == 7)), reads=[b_qx, b_KT], writes=[b_pmm[pb]])
                        S.op("dve", lambda e, pv=pv: e.reduce_max(out=sm[:, 0:1], in_=pv, axis=AX.X), reads=[b_pmm[pb]], writes=[b_sm])
                        S.op("dve", lambda e: e.tensor_scalar(out=sm[:, 1:2], in0=sm[:, 0:1], scalar1=-1.0 / 32.0, scalar2=None, op0=ALU.mult),
                             reads=[b_sm], writes=[b_sm])
                        S.op("act", lambda e, pv=pv: e.activation(out=prob[:], in_=pv, func=AF.Exp, scale=1.0 / 32.0, bias=sm[:, 1:2]),
                             reads=[b_pmm[pb], b_sm], writes=[b_prob])
                        S.op("dve", lambda e: e.reduce_sum(out=sm[:, 2:3], in_=prob[:], axis=AX.X), reads=[b_prob], writes=[b_sm])
                        S.op("dve", lambda e: e.reciprocal(out=sm[:, 3:4], in_=sm[:, 2:3]), reads=[b_sm], writes=[b_sm])
                        S.op("dve", lambda e: e.tensor_scalar(out=probb[:], in0=prob[:], scalar1=sm[:, 3:4], scalar2=None, op0=ALU.mult),
                             reads=[b_prob, b_sm], writes=[b_probb])
                        for mc in range(2):
                            S.op("pe", lambda e, mc=mc: e.transpose(out=ptr[:, mc * 128:(mc + 1) * 128], in_=probb[:, mc * 128:(mc + 1) * 128],
                                                                    identity=identb[:]), reads=[b_probb, b_idb], writes=[b_ptr])
                        S.op("act", lambda e, t=t: e.activation(out=pT[:, :, t * 128:(t + 1) * 128],
                                                                in_=ptr[:, 0:256].rearrange("p (k n) -> p k n", k=2), func=AF.Copy),
                             reads=[b_ptr], writes=[b_pT])
                    for dvc in range(8):
                        for hf in range(2):
                            pb = next_pmm(); pv = pmm[:, pb, :]
                            for mc in range(2):
                                S.op("pe", lambda e, mc=mc, pv=pv, hf=hf, dvc=dvc: e.matmul(
                                    pv, lhsT=Vm[:, mc, hx * 1024 + dvc * 128:hx * 1024 + (dvc + 1) * 128], rhs=pT[:, mc, hf * 512:(hf + 1) * 512],
                                    start=(mc == 0), stop=(mc == 1)), reads=[b_Vm, b_pT], writes=[b_pmm[pb]])
                            S.op("act", lambda e, pv=pv, hf=hf: e.activation(out=oTs[:, hf, :], in_=pv, func=AF.Copy), reads=[b_pmm[pb]], writes=[b_oTs[hf]])
                            r0 = hx * 1024 + dvc * 128
                            S.dma("sp", mixT_d[r0:r0 + 128, hf * 512:(hf + 1) * 512], oTs[:, hf, :], reads=[b_oTs[hf], b_AT],
                                  writes=[b_oT[hx * 8 + dvc]])
                S.barrier()
            if stop_after == "B3":
                S.barrier(); S.finish([]); return nc
            load_AT_from(mixT_d, b_oT)
            if stop_after == "B4":
                S.barrier(); S.finish([]); return nc
            resid_linear("wo2", wo, x1_d, b_x1, x2_d, b_x2)
            S.barrier()

        if stop_after == "B":
            S.finish([b for row in b_x2 for b in row])
            return nc

        with ExitStack() as pc:
            Wt = sb("Wt", [128, NT, 32], F32, pc); b_Wt = S.buf()
            wrs = sb("wrs", [128, KC * 36], F32, pc); b_wrs = S.buf()
            whi = sb("whi", [128, KC, 36], BF16, pc); wlo = sb("wlo", [128, KC, 36], BF16, pc); b_whl = S.buf()
            rbb = sb("rbb", [128, 36], F32, pc); b_rbb = S.buf()
            S.dma("sp", wrs[:], wr_d, writes=[b_wrs])
            S.dma("sp", rbb[:], rb_d.partition_broadcast(128), writes=[b_rbb])
            whi2 = whi[:, :, :].rearrange("p k n -> p (k n)"); wlo2 = wlo[:, :, :].rearrange("p k n -> p (k n)")
            S.op("act", lambda e: e.activation(out=whi2, in_=wrs[:], func=AF.Copy), reads=[b_wrs], writes=[b_whl])
            S.op("dve", lambda e: e.tensor_tensor(out=wlo2, in0=wrs[:], in1=whi2, op=ALU.subtract), reads=[b_wrs, b_whl], writes=[b_whl])
            deps = []
            for b in [b for row in b_x2 for b in row]:
                if b.writer is not None:
                    deps.append(b.writer)
            S._wait(S.E["sp"], deps)
            with ExitStack() as pr:
                gbc = sb("c_gbc", [128, D], F32, pr); b_g = S.buf()
                xt = sb("c_xt", [128, D], F32, pr); b_xt = S.buf()
                hit = sb("c_hit", [128, D], BF16, pr); b_hit = S.buf()
                lot = sb("c_lot", [128, D], BF16, pr); b_lot = S.buf()
                loT = sb("c_loT", [128, KC, 128], BF16, pr); b_loT = S.buf()
                ss = sb("c_ss", [128, 2], F32, pr); b_ss = S.buf()
                lg = sb("c_lg", [128, 40], F32, pr); b_lg = S.buf()
                rw = sb("c_rw", [128, 8, 32], F32, pr); b_rw = S.buf()
                S.dma("sp", gbc[:], moe_g.partition_broadcast(128), writes=[b_g])
                for t in range(NT):
                    S.dma("sp", xt[:], x2_d[t * 128:(t + 1) * 128, :], writes=[b_xt])
                    S.op("act", lambda e: e.activation(out=hit[:], in_=xt[:], func=AF.Square), reads=[b_xt], writes=[b_hit])
                    S.op("dve", lambda e: e.reduce_sum(out=ss[:, 0:1], in_=hit[:], axis=AX.X), reads=[b_hit], writes=[b_ss])
                    rms_rstd(ss[:, 0:1], float(D), 1e-6, ss[:, 1:2], b_ss)
                    S.op("dve", lambda e: e.scalar_tensor_tensor(out=xt[:], in0=xt[:], scalar=ss[:, 1:2], in1=gbc[:],
                                                                 op0=ALU.mult, op1=ALU.mult), reads=[b_xt, b_ss, b_g], writes=[b_xt])
                    S.op("act", lambda e: e.activation(out=hit[:], in_=xt[:], func=AF.Copy), reads=[b_xt], writes=[b_hit])
                    S.op("dve", lambda e: e.tensor_tensor(out=lot[:], in0=xt[:], in1=hit[:], op=ALU.subtract), reads=[b_xt, b_hit], writes=[b_lot])
                    for (srcb, bsrc, dstT, bdst, c0) in ((hit, b_hit, AT, b_AT, t * 128), (lot, b_lot, loT, b_loT, 0)):
                        for g8 in range(4):
                            for jx in range(8):
                                k = g8 * 8 + jx
                                S.op("pe", lambda e, k=k, jx=jx, srcb=srcb: e.transpose(out=ptr[:, jx * 128:(jx + 1) * 128],
                                                                                      in_=srcb[:, k * 128:(k + 1) * 128], identity=identb[:]),
                                     reads=[bsrc, b_idb], writes=[b_ptr])
                            src = ptr[:, :].rearrange("p (k n) -> p k n", k=8)
                            S.op("act", lambda e, g8=g8, src=src, dstT=dstT, c0=c0: e.activation(
                                out=dstT[:, g8 * 8:(g8 + 1) * 8, c0:c0 + 128], in_=src, func=AF.Copy), reads=[b_ptr], writes=[bdst])
                    pl_ = pms[:, 0, 0:36]
                    nmm = 0
                    for k in range(KC):
                        for (lh_, bl_, wv_) in ((AT[:, k, t * 128:(t + 1) * 128], b_AT, whi), (AT[:, k, t * 128:(t + 1) * 128], b_AT, wlo),
                                                (loT[:, k, :], b_loT, whi)):
                            S.op("pe", lambda e, k=k, lh_=lh_, wv_=wv_, nmm=nmm: e.matmul(pl_, lhsT=lh_, rhs=wv_[:, k, :],
                                                                                       start=(nmm == 0), stop=(nmm == 3 * KC - 1)),
                                 reads=[bl_, b_whl], writes=[b_pms[0]])
                            nmm += 1
                    S.op("dve", lambda e: e.tensor_tensor(out=lg[:, 0:36], in0=pl_, in1=rbb[:], op=ALU.add), reads=[b_pms[0], b_rbb], writes=[b_lg])
                    S.op("dve", lambda e: e.reduce_max(out=lg[:, 36:37], in_=lg[:, 0:4], axis=AX.X), reads=[b_lg], writes=[b_lg])
                    S.op("dve", lambda e: e.tensor_scalar(out=rw[:, 0, 0:4], in0=lg[:, 0:4], scalar1=lg[:, 36:37], scalar2=None, op0=ALU.subtract),
                         reads=[b_lg], writes=[b_rw])
                    S.op("act", lambda e: e.activation(out=rw[:, 1, 0:4], in_=rw[:, 0, 0:4], func=AF.Exp), reads=[b_rw], writes=[b_rw])
                    S.op("dve", lambda e: e.reduce_sum(out=lg[:, 37:38], in_=rw[:, 1, 0:4], axis=AX.X), reads=[b_rw], writes=[b_lg])
                    S.op("dve", lambda e: e.reciprocal(out=lg[:, 37:38], in_=lg[:, 37:38]), reads=[b_lg], writes=[b_lg])
                    S.op("dve", lambda e: e.tensor_scalar(out=rw[:, 2, 0:4], in0=lg[:, 0:4], scalar1=lg[:, 36:37], scalar2=None, op0=ALU.is_ge),
                         reads=[b_lg], writes=[b_rw])
                    S.op("dve", lambda e: e.tensor_scalar(out=rw[:, 2, 0:4], in0=rw[:, 2, 0:4], scalar1=-1.0, scalar2=1e30, op0=ALU.add, op1=ALU.mult),
                         reads=[b_rw], writes=[b_rw])
                    for g in range(4):
                        S.op("dve", lambda e, g=g: e.tensor_scalar(out=rw[:, 3, g * 8:(g + 1) * 8], in0=lg[:, 4 + g * 8:4 + (g + 1) * 8],
                                                                   scalar1=rw[:, 2, g:g + 1], scalar2=None, op0=ALU.add), reads=[b_lg, b_rw], writes=[b_rw])
                    S.op("dve", lambda e: e.reduce_max(out=lg[:, 38:39], in_=rw[:, 3, :], axis=AX.X), reads=[b_rw], writes=[b_lg])
                    S.op("dve", lambda e: e.tensor_scalar(out=rw[:, 4, :], in0=rw[:, 3, :], scalar1=lg[:, 38:39], scalar2=None, op0=ALU.is_ge),
                         reads=[b_rw, b_lg], writes=[b_rw])
                    S.op("dve", lambda e: e.scalar_tensor_tensor(out=rw[:, 5, :], in0=rw[:, 4, :], scalar=-1e30, in1=rw[:, 3, :], op0=ALU.mult, op1=ALU.add),
                         reads=[b_rw], writes=[b_rw])
                    S.op("dve", lambda e: e.reduce_max(out=lg[:, 39:40], in_=rw[:, 5, :], axis=AX.X), reads=[b_rw], writes=[b_lg])
                    S.op("dve", lambda e: e.tensor_scalar(out=rw[:, 6, :], in0=rw[:, 5, :], scalar1=lg[:, 39:40], scalar2=None, op0=ALU.is_ge),
                         reads=[b_rw, b_lg], writes=[b_rw])
                    S.op("dve", lambda e: e.tensor_tensor(out=ss[:, 0:1], in0=lg[:, 38:39], in1=lg[:, 39:40], op=ALU.subtract), reads=[b_lg], writes=[b_ss])
                    S.op("act", lambda e: e.activation(out=ss[:, 0:1], in_=ss[:, 0:1], func=AF.Sigmoid), reads=[b_ss], writes=[b_ss])
                    S.op("dve", lambda e: e.tensor_tensor(out=ss[:, 0:1], in0=ss[:, 0:1], in1=lg[:, 37:38], op=ALU.mult), reads=[b_ss, b_lg], writes=[b_ss])
                    S.op("dve", lambda e: e.tensor_tensor(out=ss[:, 1:2], in0=lg[:, 37:38], in1=ss[:, 0:1], op=ALU.subtract), reads=[b_ss, b_lg], writes=[b_ss])
                    S.op("dve", lambda e: e.tensor_scalar(out=rw[:, 7, :], in0=rw[:, 6, :], scalar1=ss[:, 1:2], scalar2=None, op0=ALU.mult),
                         reads=[b_rw, b_ss], writes=[b_rw])
                    S.op("dve", lambda e, t=t: e.scalar_tensor_tensor(out=Wt[:, t, :], in0=rw[:, 4, :], scalar=ss[:, 0:1], in1=rw[:, 7, :],
                                                                      op0=ALU.mult, op1=ALU.add), reads=[b_rw, b_ss], writes=[b_Wt])

                S.barrier()
            if stop_after == "C1":
                S.barrier(); S.finish([]); return nc
            with ExitStack() as pe_:
                actT = sb("actT", [128, 8, SLAB], BF16, pe_); b_actT = S.bufs(8)
                sil = sb("sil", [128, 2, 512], F32, pe_); b_sil = S.bufs(2)
                accp = sb("accp", [128, 4, 1024], F32, pe_); b_accp = S.bufs(4)
                pg_hold = {}
                ai = [0]; si = [0]
                b_x3 = [[S.buf() for _ in range(4)] for _ in range(NT)]
                for t in range(NT):
                    for q4 in range(4):
                        b_x3[t][q4].writer = None
                jobs = []
                for ex in range(nexp):
                    def mke(ex):
                        js = []
                        for s4 in range(4):
                            def ldg(s4=s4):
                                return load_wslab(wg[ex, :, s4 * 256:(s4 + 1) * 256], KC, 256)

                            def cpg(hd, s4=s4):
                                pg_hold[s4] = hd
                            def ldu(s4=s4):
                                return load_wslab(wu[ex, :, s4 * 256:(s4 + 1) * 256], KC, 256)

                            def cpu(hd, s4=s4):
                                gw, gb = pg_hold.pop(s4)
                                uw, ub = hd
                                for cc in range(2):
                                    dch = s4 * 2 + cc
                                    for hf in range(2):
                                        pbg = next_pmm(); pvg = pmm[:, pbg, :]
                                        for k in range(KC):
                                            S.op("pe", lambda e, k=k, pvg=pvg, cc=cc, hf=hf: e.matmul(
                                                pvg, lhsT=gw[:, k, cc * 128:(cc + 1) * 128], rhs=AT[:, k, hf * 512:(hf + 1) * 512],
                                                start=(k == 0), stop=(k == KC - 1)), reads=[b_AT, gb], writes=[b_pmm[pbg]])
                                        pbu = next_pmm(); pvu = pmm[:, pbu, :]
                                        for k in range(KC):
                                            S.op("pe", lambda e, k=k, pvu=pvu, cc=cc, hf=hf: e.matmul(
                                                pvu, lhsT=uw[:, k, cc * 128:(cc + 1) * 128], rhs=AT[:, k, hf * 512:(hf + 1) * 512],
                                                start=(k == 0), stop=(k == KC - 1)), reads=[b_AT, ub], writes=[b_pmm[pbu]])
                                        r = si[0]; si[0] = (r + 1) % 2
                                        S.op("act", lambda e, r=r, pvg=pvg: e.activation(out=sil[:, r, :], in_=pvg, func=AF.Silu),
                                             reads=[b_pmm[pbg]], writes=[b_sil[r]])
                                        S.op("dve", lambda e, r=r, pvu=pvu, dch=dch, hf=hf: e.tensor_tensor(
                                            out=actT[:, dch, hf * 512:(hf + 1) * 512], in0=pvu, in1=sil[:, r, :], op=ALU.mult),
                                             reads=[b_pmm[pbu], b_sil[r]], writes=[b_actT[dch]])
                            js += [(ldg, cpg), (ldu, cpu)]
                        for q4 in range(4):
                            def ldd(q4=q4):
                                return load_wslab(wd[ex, :, q4 * 1024:(q4 + 1) * 1024], 8, 1024)

                            def cpd(hd, q4=q4):
                                dw, db = hd
                                srcd = x2_d if ex % 2 == 0 else x3_d
                                dstd = x3_d if ex % 2 == 0 else x2_d
                                pend = {}

                                def issue_load(t):
                                    a = ai[0]; ai[0] = (a + 1) % 4
                                    S.dma("sp", accp[:, a, :], srcd[t * 128:(t + 1) * 128, q4 * 1024:(q4 + 1) * 1024],
                                          reads=[b_x3[t][q4]], writes=[b_accp[a]])
                                    pend[t] = a
                                issue_load(0); issue_load(1)
                                for t in range(NT):
                                    a = pend.pop(t)
                                    if t + 2 < NT:
                                        issue_load(t + 2)
                                    for g2 in range(2):
                                        pb = next_pmm(); pv = pmm[:, pb, :]
                                        for k in range(8):
                                            S.op("pe", lambda e, k=k, pv=pv, t=t, g2=g2: e.matmul(
                                                pv, lhsT=actT[:, k, t * 128:(t + 1) * 128], rhs=dw[:, k, g2 * 512:(g2 + 1) * 512],
                                                start=(k == 0), stop=(k == 7)), reads=b_actT + [db], writes=[b_pmm[pb]])
                                        S.op("dve", lambda e, pv=pv, a=a, g2=g2, t=t: e.scalar_tensor_tensor(
                                            out=accp[:, a, g2 * 512:(g2 + 1) * 512], in0=pv, scalar=Wt[:, t, ex:ex + 1],
                                            in1=accp[:, a, g2 * 512:(g2 + 1) * 512], op0=ALU.mult, op1=ALU.add),
                                             reads=[b_pmm[pb], b_Wt, b_accp[a]], writes=[b_accp[a]])
                                    S.dma("sp", dstd[t * 128:(t + 1) * 128, q4 * 1024:(q4 + 1) * 1024], accp[:, a, :],
                                          reads=[b_accp[a]], writes=[b_x3[t][q4]])
                            js.append((ldd, cpd))
                        return js
                    jobs += mke(ex)
                run_jobs(jobs)
                S.barrier()

            deps = []
            for row in b_x3:
                for b in row:
                    if b.writer is not None:
                        deps.append(b.writer)
            S._wait(S.E["sp"], deps)
            with ExitStack() as pf:
                gbc = sb("f_gbc", [128, D], F32, pf); b_g = S.buf()
                xt = sb("f_xt", [128, 2, D], F32, pf); b_xt = S.bufs(2)
                ss = sb("f_ss", [128, 4], F32, pf); b_ss = S.bufs(2)
                junk = sb("f_junk", [128, D], F32, pf); b_junk = S.buf()
                b_out = S.bufs(NT)
                S.dma("sp", gbc[:], fin_g.partition_broadcast(128), writes=[b_g])
                for t in range(NT):
                    r = t % 2
                    S.dma("sp", xt[:, r, :], x2_d[t * 128:(t + 1) * 128, :], writes=[b_xt[r]])
                    S.op("act", lambda e, r=r: e.activation(out=junk[:], in_=xt[:, r, :], func=AF.Square), reads=[b_xt[r]], writes=[b_junk])
                    S.op("dve", lambda e, r=r: e.reduce_sum(out=ss[:, 2 * r:2 * r + 1], in_=junk[:], axis=AX.X), reads=[b_junk], writes=[b_ss[r]])
                    rms_rstd(ss[:, 2 * r:2 * r + 1], float(D), 1e-6, ss[:, 2 * r + 1:2 * r + 2], b_ss[r])
                    S.op("dve", lambda e, r=r: e.scalar_tensor_tensor(out=xt[:, r, :], in0=xt[:, r, :], scalar=ss[:, 2 * r + 1:2 * r + 2], in1=gbc[:],
                                                                      op0=ALU.mult, op1=ALU.mult), reads=[b_xt[r], b_ss[r], b_g], writes=[b_xt[r]])
                    S.dma("sp", out_d[t * 128:(t + 1) * 128, :], xt[:, r, :], reads=[b_xt[r]], writes=[b_out[t]])
                S.finish(b_out)
    return nc


_CACHE = {}


def _prep_inputs(inp):
    f32 = np.float32
    x = np.asarray(inp["x"], f32)[0]
    pos = np.asarray(inp["positions"])[0].astype(np.int32)
    cst = make_consts()
    lru_cw = np.asarray(inp["lru_conv_w"], f32)[0]
    pp = np.zeros((128, NPP), f32)
    for ch in range(16):
        sl = slice(ch * 128, (ch + 1) * 128)
        for jj in range(4):
            pp[:, P_CW + ch * 4 + jj] = lru_cw[jj, sl]
        pp[:, P_CB + ch] = np.asarray(inp["lru_conv_b"], f32)[0, sl]
        pp[:, P_BA + ch] = np.asarray(inp["lru_b_a"], f32)[0, sl]
        pp[:, P_BI + ch] = np.asarray(inp["lru_b_i"], f32)[0, sl]
        pp[:, P_LAM + ch] = np.asarray(inp["lru_lambda"], f32)[0, sl]
        pp[:, P_LG + ch] = np.asarray(inp["lru_norm_g"], f32)[0, sl]
    wr = np.concatenate([np.asarray(inp["router_group_w"], f32)[0],
                         np.asarray(inp["router_expert_w"], f32)[0]], axis=1)
    wr = np.ascontiguousarray(wr.reshape(KC, 128, 36).transpose(1, 0, 2).reshape(128, KC * 36))
    rb = np.ascontiguousarray(np.concatenate([np.asarray(inp["router_group_b"], f32)[0],
                                              np.asarray(inp["router_expert_b"], f32)[0]])[None, :])
    shared = {
        "mem": np.ascontiguousarray(np.asarray(inp["mem"], f32)[0]),
        "cst": cst, "pp": pp,
        "mix_g": np.asarray(inp["mix_norm_g"], f32).reshape(1, D),
        "gn_g": np.asarray(inp["ret_norm_g"], f32).reshape(1, 2048),
        "xat_g": np.asarray(inp["xattn_norm_g"], f32).reshape(1, D),
        "mem_g": np.asarray(inp["mem_norm_g"], f32).reshape(1, D),
        "moe_g": np.asarray(inp["moe_norm_g"], f32).reshape(1, D),
        "fin_g": np.asarray(inp["final_norm_g"], f32).reshape(1, D),
        "rb": rb, "wr": wr,
        "w_in": np.asarray(inp["w_in"], f32)[0], "w_out": np.asarray(inp["w_out"], f32)[0],
        "wq": np.asarray(inp["xattn_wq"], f32)[0], "wk": np.asarray(inp["xattn_wk"], f32)[0],
        "wv": np.asarray(inp["xattn_wv"], f32)[0], "wo": np.asarray(inp["xattn_wo"], f32)[0],
        "w_a": np.asarray(inp["lru_w_a"], f32)[0], "w_i": np.asarray(inp["lru_w_i"], f32)[0],
        "wg": np.asarray(inp["expert_w_gate"], f32)[0], "wu": np.asarray(inp["expert_w_up"], f32)[0],
        "wd": np.asarray(inp["expert_w_down"], f32)[0],
    }
    in_maps = []
    for c in range(NCORES):
        xs = np.zeros((NSLAB * SLAB, D), f32)
        ps_ = np.zeros((NSLAB * SLAB,), np.int32)
        vm = np.zeros((128, 8), f32)
        for j in range(NSLAB):
            g = c - (NSLAB - 1) + j
            if g >= 0:
                xs[j * SLAB:(j + 1) * SLAB] = x[g * SLAB:(g + 1) * SLAB]
                ps_[j * SLAB:(j + 1) * SLAB] = pos[g * SLAB:(g + 1) * SLAB]
                vm[:, j] = 1.0
        pos_pm = np.ascontiguousarray(ps_.reshape(64, 128).T)
        m = dict(shared)
        m.update(xs=xs, pos_pm=pos_pm, vmask=vm)
        in_maps.append(m)
    return in_maps


def kernel(**inputs):
    if "nc" not in _CACHE:
        _CACHE["nc"] = build_program()
    nc = _CACHE["nc"]
    in_maps = _prep_inputs(inputs)
    res = run_bass_kernel_spmd(nc, in_maps, core_ids=list(range(NCORES)))
    out = np.concatenate([np.asarray(r["out"], np.float32) for r in res.results], axis=0)
    return out.reshape(1, NCORES * SLAB, D)
```

```python
import contextlib
from contextlib import ExitStack
import numpy as np
import concourse.bass as bass
import concourse.mybir as mybir
from concourse.bass_utils import run_bass_kernel_spmd

F32 = mybir.dt.float32
BF16 = mybir.dt.bfloat16
I32 = mybir.dt.int32
ALU = mybir.AluOpType
AF = mybir.ActivationFunctionType
AX = mybir.AxisListType

NCORES = 8
D = 4096
KC = 32
SLAB = 1024
NT = 8
NSLAB = 8
NEXP = 32
DE = 1024

C_INVF = 0
C_DEC = 128
C_XI = C_DEC + 8 * 128
C_ZETA = C_XI + 8
C_CD = C_ZETA + 8
C_WGT = C_CD + 8
C_ID = C_WGT + 8 * 56
NCST = C_ID + 128
P_CW = 0
P_CB = 64
P_BA = 80
P_BI = 96
P_LAM = 112
P_LG = 128
NPP = 144


class Buf:
    __slots__ = ("name", "writer", "readers")

    def __init__(self, name=""):
        self.name = name
        self.writer = None
        self.readers = []


class _Eng:
    def __init__(self, name, eng, sem):
        self.name = name
        self.eng = eng
        self.sem = sem
        self.cnt = 0
        self.seen = {}


class Sched:
    def __init__(self, nc, stack, n_dma_sems=32):
        self.nc = nc
        self.E = {}
        for name, eng in (("pe", nc.tensor), ("act", nc.scalar), ("dve", nc.vector),
                          ("pool", nc.gpsimd), ("sp", nc.sync)):
            sem = stack.enter_context(nc.semaphore("s_" + name))
            self.E[name] = _Eng(name, eng, sem)
        self.dsems = []
        for i in range(n_dma_sems):
            sem = stack.enter_context(nc.semaphore("s_dma%d" % i))
            self.dsems.append([sem, 0])
        self.drr = 0

    def buf(self, name=""):
        return Buf(name)

    def bufs(self, n, name=""):
        return [Buf(name + str(i)) for i in range(n)]

    def _wait(self, E, deps):
        need = {}
        for (sem, val, ename) in deps:
            if ename == E.name and ename == "pe":
                continue
            k = id(sem)
            if E.seen.get(k, 0) >= val:
                continue
            if k not in need or need[k][1] < val:
                need[k] = (sem, val)
        for k, (sem, val) in need.items():
            E.eng.wait_ge(sem, val)
            E.seen[k] = val

    @staticmethod
    def _deps(reads, writes):
        deps = []
        for b in reads:
            if b.writer is not None:
                deps.append(b.writer)
        for b in writes:
            if b.writer is not None:
                deps.append(b.writer)
            deps.extend(b.readers)
        return deps

    @staticmethod
    def _commit(tok, reads, writes):
        for b in writes:
            b.writer = tok
            b.readers = []
        for b in reads:
            b.readers.append(tok)

    def op(self, engname, fn, reads=(), writes=()):
        E = self.E[engname]
        self._wait(E, self._deps(reads, writes))
        ins = fn(E.eng)
        E.cnt += 1
        ins.then_inc(E.sem, 1)
        self._commit((E.sem, E.cnt, engname), reads, writes)
        return ins

    def dma(self, qname, out, in_, reads=(), writes=(), **kw):
        E = self.E[qname]
        slot = self.dsems[self.drr]
        self.drr = (self.drr + 1) % len(self.dsems)
        deps = self._deps(reads, writes)
        if slot[1] > 0:
            deps.append((slot[0], slot[1], "dma"))
        self._wait(E, deps)
        ins = E.eng.dma_start(out=out, in_=in_, **kw)
        slot[1] += 16
        ins.then_inc(slot[0], 16)
        self._commit((slot[0], slot[1], "dma"), reads, writes)
        return ins

    def barrier(self):
        toks = [(E.sem, E.cnt, n) for n, E in self.E.items() if E.cnt > 0]
        toks += [(d[0], d[1], "dma") for d in self.dsems if d[1] > 0]
        for E in self.E.values():
            self._wait(E, [t for t in toks if t[2] != E.name])

    def finish(self, bufs):
        deps = []
        for b in bufs:
            if b.writer is not None:
                deps.append(b.writer)
            deps.extend(b.readers)
        self._wait(self.E["sp"], deps)


def make_consts():
    lg = np.log1p(-np.exp2(-5.0 - np.arange(8, dtype=np.float64)))
    cst = np.zeros((128, NCST), np.float64)
    invf = np.float32(10000.0) ** (-(np.arange(0, 256, 2, dtype=np.float32)) / np.float32(256.0))
    cst[:, C_INVF:C_INVF + 128] = invf[None, :].astype(np.float64)
    idx = np.arange(128, dtype=np.float64)
    for h in range(8):
        diff = idx[None, :] - idx[:, None]
        dec = np.where(diff >= 0, np.exp(lg[h] * np.maximum(diff, 0.0)), 0.0) / 16.0
        cst[:, C_DEC + h * 128:C_DEC + (h + 1) * 128] = dec
        cst[:, C_XI + h] = np.exp(lg[h] * (idx + 1.0))
        cst[:, C_ZETA + h] = np.exp(lg[h] * (127.0 - idx)) / 16.0
        cst[:, C_CD + h] = np.exp(lg[h] * 128.0)
        for j in range(56):
            cst[:, C_WGT + h * 56 + j] = np.exp(lg[h] * (7167.0 - (128.0 * j + idx))) / 16.0
    cst[:, C_ID:C_ID + 128] = np.eye(128)
    return cst.astype(np.float32)


def build_program(stop_after=None, skipA=False):
    nc = bass.Bass("TRN2", target_bir_lowering=False)

    def din(name, shape, dt=F32):
        return nc.dram_tensor(name, list(shape), dt, kind="ExternalInput").ap()

    xs = din("xs", [NSLAB * SLAB, D])
    w_outs = None
    pos_pm = din("pos_pm", [128, 64], I32)
    vmask_d = din("vmask", [128, 8])
    mem_d = din("mem", [256, D])
    cst_d = din("cst", [128, NCST])
    pp_d = din("pp", [128, NPP])
    mix_g = din("mix_g", [1, D]); gn_g = din("gn_g", [1, 2048]); xat_g = din("xat_g", [1, D])
    mem_g = din("mem_g", [1, D]); moe_g = din("moe_g", [1, D]); fin_g = din("fin_g", [1, D])
    rb_d = din("rb", [1, 36])
    wr_d = din("wr", [128, KC * 36])
    w_in = din("w_in", [D, 12288] if not skipA else [1, 1])
    w_out = din("w_out", [D, D]); wq = din("wq", [D, D]); wk = din("wk", [D, D])
    wv = din("wv", [D, D]); wo = din("wo", [D, D])
    w_a = din("w_a", [8, 256, 256]); w_i = din("w_i", [8, 256, 256])
    big = stop_after is None
    nexp = NEXP if big else 2
    wg = din("wg", [nexp, D, DE]); wu = din("wu", [nexp, D, DE]); wd = din("wd", [nexp, DE, D])
    out_d = nc.dram_tensor("out", [SLAB, D], F32, kind="ExternalOutput").ap()
    dbg = stop_after is not None
    kind_s = "ExternalOutput" if dbg else "Internal"
    x1_d = nc.dram_tensor("x1_d", [SLAB, D], F32, kind=kind_s).ap()
    x2_d = nc.dram_tensor("x2_d", [SLAB, D], F32, kind=kind_s).ap()
    x3_d = nc.dram_tensor("x3_d", [SLAB, D], F32, kind="Internal").ap()
    mixT_d = nc.dram_tensor("mixT_d", [D, SLAB], BF16, kind="Internal").ap()
    yT_d = nc.dram_tensor("yT_d", [2048, SLAB], F32, kind="Internal").ap()

    with ExitStack() as st:
        S = Sched(nc, st)

        def sb(name, shape, dt, stack=st):
            return stack.enter_context(nc.sbuf_tensor("sb_" + name, list(shape), dt))

        def ps(name, shape, dt):
            return st.enter_context(nc.psum_tensor("ps_" + name, list(shape), dt))

        cst = sb("cst", [128, NCST], F32); b_cst = S.buf()
        pp = sb("pp", [128, NPP], F32); b_pp = S.buf()
        vmask = sb("vmaskt", [128, 8], F32); b_vm = S.buf()
        posi = sb("posi", [128, 64], I32); posf = sb("posf", [128, 64], F32); b_pos = S.buf()
        identb = sb("identb", [128, 128], BF16); b_idb = S.buf()
        onesf = sb("onesf", [128, 128], F32); b_ones = S.buf()
        AT = sb("AT", [128, KC, SLAB], BF16); b_AT = S.buf()
        NQ = 3
        ring = sb("ring", [128, NQ * 8192], BF16); b_ring = S.bufs(NQ)
        ring_i = [0]
        pmm = ps("pmm", [128, 4, 512], F32); b_pmm = S.bufs(4); pmm_i = [0]
        ptr = ps("ptr", [128, 1024], BF16); b_ptr = S.buf()
        ptf = ps("ptf", [128, 512], F32); b_ptf = S.buf()
        pms = ps("pms", [128, 2, 512], F32); b_pms = S.bufs(4)
        sdesc = sb("sdesc", [128, 16], F32); b_sd = S.buf()

        S.dma("sp", cst[:], cst_d, writes=[b_cst])
        S.dma("sp", pp[:], pp_d, writes=[b_pp])
        S.dma("sp", vmask[:], vmask_d, writes=[b_vm])
        S.dma("sp", posi[:], pos_pm, writes=[b_pos])
        S.op("dve", lambda e: e.tensor_copy(out=posf[:], in_=posi[:]), reads=[b_pos], writes=[b_pos])
        S.op("dve", lambda e: e.tensor_copy(out=identb[:], in_=cst[:, C_ID:C_ID + 128]), reads=[b_cst], writes=[b_idb])
        S.op("dve", lambda e: e.memset(onesf[:], 1.0), writes=[b_ones])
        identf = cst[:, C_ID:C_ID + 128]

        def next_pmm():
            i = pmm_i[0]
            pmm_i[0] = (i + 1) % 4
            return i

        def load_wslab(W2d, kc, ncols):
            assert kc * ncols == 8192
            q = ring_i[0]
            ring_i[0] = (q + 1) % NQ
            view = ring[:, q * 8192:(q + 1) * 8192].rearrange("p (k n) -> p k n", k=kc)
            S.dma("pool", view, W2d.rearrange("(k p) n -> p k n", p=128), writes=[b_ring[q]])
            return view, b_ring[q]

        def run_jobs(jobs, depth=2):
            handles = {}
            n = len(jobs)
            for i in range(min(depth, n)):
                handles[i] = jobs[i][0]()
            for i in range(n):
                jobs[i][1](handles.pop(i))
                if i + depth < n:
                    handles[i + depth] = jobs[i + depth][0]()

        def mm_tok(w, bw, A, bA, ntiles, kc, evac, ncols=256):
            for t in range(ntiles):
                pb = next_pmm()
                pv = pmm[:, pb, 0:ncols]
                for k in range(kc):
                    S.op("pe", lambda e, k=k, t=t, pv=pv: e.matmul(pv, lhsT=A[:, k, t * 128:(t + 1) * 128], rhs=w[:, k, 0:ncols],
                                                                  start=(k == 0), stop=(k == kc - 1)),
                         reads=[bA, bw], writes=[b_pmm[pb]])
                evac(t, pv, b_pmm[pb])

        def mm_feat(w, bw, A, bA, ntok, kc, evac, ncols=256):
            for cc in range(ncols // 128):
                for hf in range(max(1, ntok // 512)):
                    n = min(512, ntok)
                    pb = next_pmm()
                    pv = pmm[:, pb, 0:n]
                    for k in range(kc):
                        S.op("pe", lambda e, k=k, cc=cc, hf=hf, pv=pv, n=n: e.matmul(
                            pv, lhsT=w[:, k, cc * 128:(cc + 1) * 128], rhs=A[:, k, hf * 512:hf * 512 + n],
                            start=(k == 0), stop=(k == kc - 1)),
                             reads=[bA, bw], writes=[b_pmm[pb]])
                    evac(cc, hf, pv, b_pmm[pb])

        def rms_rstd(ss_ap, n, eps, out_ap, b):
            S.op("dve", lambda e: e.tensor_scalar(out=out_ap, in0=ss_ap, scalar1=1.0 / n, scalar2=eps,
                                                  op0=ALU.mult, op1=ALU.add), reads=[b], writes=[b])
            S.op("act", lambda e: e.activation(out=out_ap, in_=out_ap, func=AF.Sqrt), reads=[b], writes=[b])
            S.op("dve", lambda e: e.reciprocal(out=out_ap, in_=out_ap), reads=[b], writes=[b])

        tr_cnt = [0]

        def norm_transpose(ph, src_rows, ntiles, gvec, AT_dst, b_dst, col0, f32_out=None):
            with ExitStack() as ls:
                gbc = sb(ph + "gbc", [128, D], F32, ls); b_g = S.buf()
                xt = sb(ph + "xt", [128, D], F32, ls); b_xt = S.buf()
                hb = sb(ph + "hb", [128, D // 2], BF16, ls); b_hb = S.buf()
                ss = sb(ph + "ss", [128, 4], F32, ls); b_ss = S.buf()
                S.dma("sp", gbc[:], gvec.partition_broadcast(128), writes=[b_g])
                HD = D // 2
                for t in range(ntiles):
                    S.dma("sp", xt[:], src_rows[t * 128:(t + 1) * 128, :], writes=[b_xt])
                    for hh in range(2):
                        S.op("act", lambda e, hh=hh: e.activation(out=hb[:], in_=xt[:, hh * HD:(hh + 1) * HD], func=AF.Square), reads=[b_xt], writes=[b_hb])
                        S.op("dve", lambda e, hh=hh: e.reduce_sum(out=ss[:, 2 + hh:3 + hh], in_=hb[:], axis=AX.X), reads=[b_hb], writes=[b_ss])
                    S.op("dve", lambda e: e.tensor_tensor(out=ss[:, 0:1], in0=ss[:, 2:3], in1=ss[:, 3:4], op=ALU.add), reads=[b_ss], writes=[b_ss])
                    rms_rstd(ss[:, 0:1], float(D), 1e-6, ss[:, 1:2], b_ss)
                    for hh in range(2):
                        S.op("dve", lambda e, hh=hh: e.scalar_tensor_tensor(out=hb[:], in0=xt[:, hh * HD:(hh + 1) * HD], scalar=ss[:, 1:2],
                                                                            in1=gbc[:, hh * HD:(hh + 1) * HD], op0=ALU.mult, op1=ALU.mult),
                             reads=[b_xt, b_ss, b_g], writes=[b_hb])
                        for g8 in range(2):
                            for j in range(8):
                                k = g8 * 8 + j
                                S.op("pe", lambda e, k=k, j=j: e.transpose(out=ptr[:, j * 128:(j + 1) * 128],
                                                                           in_=hb[:, k * 128:(k + 1) * 128], identity=identb[:]),
                                     reads=[b_hb, b_idb], writes=[b_ptr])
                            eng = "act" if (tr_cnt[0] % 2 == 0) else "dve"
                            tr_cnt[0] += 1
                            k0 = hh * 16 + g8 * 8
                            dst = AT_dst[:, k0:k0 + 8, col0 + t * 128:col0 + (t + 1) * 128]
                            src = ptr[:, :].rearrange("p (k n) -> p k n", k=8)
                            if eng == "act":
                                S.op("act", lambda e, dst=dst, src=src: e.activation(out=dst, in_=src, func=AF.Copy),
                                     reads=[b_ptr], writes=[b_dst])
                            else:
                                S.op("dve", lambda e, dst=dst, src=src: e.tensor_copy(out=dst, in_=src),
                                     reads=[b_ptr], writes=[b_dst])
                S.barrier()

        with ExitStack() as pa:
          if skipA:
            b_mixT = S.bufs(32, "mixT")
          else:
              Racc = sb("Racc", [128, 16, 256], F32, pa); b_R = S.bufs(16)
              for i in range(16):
                  S.op("dve", lambda e, i=i: e.memset(Racc[:, i, :], 0.0), writes=[b_R[i]])
              lstate = sb("lstate", [128, 16], F32, pa); b_ls = S.bufs(16)
              halo = sb("halo", [128, 16, 4], F32, pa); b_halo = S.bufs(16)
              S.op("dve", lambda e: e.memset(lstate[:], 0.0), writes=b_ls)
              S.op("dve", lambda e: e.memset(halo[:], 0.0), writes=b_halo)
              wab = wib = b_wab = None
              lsc = sb("lsc", [128, 48], F32, pa); b_lsc = S.buf()
              S.op("act", lambda e: e.activation(out=lsc[:, 0:16], in_=pp[:, P_LAM:P_LAM + 16], func=AF.Exp, scale=-1.0),
                   reads=[b_pp], writes=[b_lsc])
              S.op("act", lambda e: e.activation(out=lsc[:, 0:16], in_=lsc[:, 0:16], func=AF.Ln, bias=1.0),
                   reads=[b_lsc], writes=[b_lsc])
              S.op("dve", lambda e: e.tensor_scalar(out=lsc[:, 16:32], in0=lsc[:, 0:16], scalar1=-8.0, scalar2=None, op0=ALU.mult),
                   reads=[b_lsc], writes=[b_lsc])
              S.op("dve", lambda e: e.tensor_scalar(out=lsc[:, 32:48], in0=lsc[:, 0:16], scalar1=-16.0, scalar2=None, op0=ALU.mult),
                   reads=[b_lsc], writes=[b_lsc])
              cosT = sinT = b_cs = rtmp = b_rt = rki = kt = b_kt = vt = b_vt = rpt = b_rpt = None
              ret_n = [0]

              def alloc_ret(stack):
                  nonlocal cosT, sinT, b_cs, rtmp, b_rt, rki, kt, b_kt, vt, b_vt, rpt, b_rpt
                  n = "%d" % ret_n[0]; ret_n[0] += 1
                  cosT = sb("cosT" + n, [128, NT, 128], F32, stack); sinT = sb("sinT" + n, [128, NT, 128], F32, stack); b_cs = S.buf()
                  rtmp = sb("rtmp" + n, [128, 3, 128], F32, stack); b_rt = S.buf()
                  rki = sb("rki" + n, [128, 128], I32, stack)
                  kt = sb("kt" + n, [128, NT, 256], BF16, stack); b_kt = S.buf()
                  vt = sb("vt" + n, [128, NT, 256], BF16, stack); b_vt = S.buf()
                  rpt = sb("rpt" + n, [128, 2, 256], F32, stack); b_rpt = S.buf()
              LB = {}
              lru_n = [0]

              def alloc_lru(stack, nset=1):
                  nonlocal wab, wib, b_wab
                  LB["nset"] = nset
                  n = lru_n[0]; lru_n[0] += 1
                  wab = sb("wab%d" % n, [128, 8, 2, 256], BF16, stack); wib = sb("wib%d" % n, [128, 8, 2, 256], BF16, stack); b_wab = S.buf()
                  S.dma("pool", wab[:], w_a.rearrange("b (c p) d -> p b c d", p=128), writes=[b_wab])
                  S.dma("pool", wib[:], w_i.rearrange("b (c p) d -> p b c d", p=128), writes=[b_wab])
                  LB["xb"] = sb("xb%d" % n, [128, 2 * nset, 1028], F32, stack); LB["b_xb"] = S.bufs(2 * nset)
                  LB["xc"] = sb("xc%d" % n, [128, 2, 1024], F32, stack); LB["b_xc"] = S.bufs(2)
                  LB["xcb"] = sb("xcb%d" % n, [128, 2, 1024], BF16, stack); LB["b_xcb"] = S.bufs(2)
                  for nm in ("lr", "li", "l2", "lh"):
                      LB[nm] = sb(nm + "%d" % n, [128, 1024], F32, stack); LB["b_" + nm] = S.buf()

              def rope_tables(j):
                  for t in range(NT):
                      pcol = posf[:, j * NT + t:j * NT + t + 1]
                      ang = rtmp[:, 0, :]; y = rtmp[:, 1, :]; kf = rtmp[:, 2, :]
                      S.op("dve", lambda e, pcol=pcol: e.tensor_scalar(out=ang, in0=cst[:, C_INVF:C_INVF + 128], scalar1=pcol,
                                                                       scalar2=None, op0=ALU.mult), reads=[b_cst, b_pos], writes=[b_rt])
                      S.op("dve", lambda e: e.tensor_scalar(out=y, in0=ang, scalar1=float(1.0 / (2 * np.pi)), scalar2=0.5,
                                                            op0=ALU.mult, op1=ALU.add), reads=[b_rt], writes=[b_rt])
                      S.op("dve", lambda e: e.tensor_copy(out=rki[:], in_=y), reads=[b_rt], writes=[b_rt])
                      S.op("dve", lambda e: e.tensor_copy(out=kf, in_=rki[:]), reads=[b_rt], writes=[b_rt])
                      S.op("dve", lambda e: e.scalar_tensor_tensor(out=y, in0=kf, scalar=-6.28125, in1=ang, op0=ALU.mult, op1=ALU.add),
                           reads=[b_rt], writes=[b_rt])
                      S.op("dve", lambda e: e.scalar_tensor_tensor(out=y, in0=kf, scalar=-0.0019353071795864769, in1=y,
                                                                   op0=ALU.mult, op1=ALU.add), reads=[b_rt], writes=[b_rt])
                      S.op("dve", lambda e: e.tensor_scalar(out=kf, in0=y, scalar1=-float(np.pi), scalar2=float(2 * np.pi),
                                                            op0=ALU.is_lt, op1=ALU.mult), reads=[b_rt], writes=[b_rt])
                      S.op("dve", lambda e: e.tensor_tensor(out=y, in0=y, in1=kf, op=ALU.add), reads=[b_rt], writes=[b_rt])
                      S.op("dve", lambda e: e.tensor_scalar(out=kf, in0=y, scalar1=float(np.pi), scalar2=-float(2 * np.pi),
                                                            op0=ALU.is_gt, op1=ALU.mult), reads=[b_rt], writes=[b_rt])
                      S.op("dve", lambda e: e.tensor_tensor(out=y, in0=y, in1=kf, op=ALU.add), reads=[b_rt], writes=[b_rt])
                      S.op("act", lambda e, t=t: e.activation(out=sinT[:, t, :], in_=y, func=AF.Sin), reads=[b_rt], writes=[b_cs])
                      S.op("act", lambda e: e.activation(out=kf, in_=y, func=AF.Abs), reads=[b_rt], writes=[b_rt])
                      S.op("act", lambda e, t=t: e.activation(out=cosT[:, t, :], in_=kf, func=AF.Sin, scale=-1.0, bias=float(np.pi / 2)),
                           reads=[b_rt], writes=[b_cs])

              def rope_evac(dst, bdst):
                  def f(t, pv, bp):
                      t1 = pv[:, 0:128]; t2 = pv[:, 128:256]
                      a = rpt[:, 0, 0:128]; b = rpt[:, 0, 128:256]; c = rpt[:, 1, 0:128]; d = rpt[:, 1, 128:256]
                      S.op("dve", lambda e: e.tensor_tensor(out=a, in0=t1, in1=cosT[:, t, :], op=ALU.mult), reads=[bp, b_cs], writes=[b_rpt])
                      S.op("dve", lambda e: e.tensor_tensor(out=b, in0=t2, in1=sinT[:, t, :], op=ALU.mult), reads=[bp, b_cs], writes=[b_rpt])
                      S.op("dve", lambda e: e.tensor_tensor(out=c, in0=t1, in1=sinT[:, t, :], op=ALU.mult), reads=[bp, b_cs], writes=[b_rpt])
                      S.op("dve", lambda e: e.tensor_tensor(out=d, in0=t2, in1=cosT[:, t, :], op=ALU.mult), reads=[bp, b_cs], writes=[b_rpt])
                      S.op("dve", lambda e: e.tensor_tensor(out=dst[:, t, 0:128], in0=a, in1=b, op=ALU.subtract), reads=[b_rpt], writes=[bdst])
                      S.op("dve", lambda e: e.tensor_tensor(out=dst[:, t, 128:256], in0=c, in1=d, op=ALU.add), reads=[b_rpt], writes=[bdst])
                  return f

              def copy_evac(dst, bdst, eng="act"):
                  def f(t, pv, bp):
                      if eng == "act":
                          S.op("act", lambda e: e.activation(out=dst[:, t, :], in_=pv, func=AF.Copy), reads=[bp], writes=[bdst])
                      else:
                          S.op("dve", lambda e: e.tensor_copy(out=dst[:, t, :], in_=pv), reads=[bp], writes=[bdst])
                  return f

              def lru_block(j, blk, own, gate_handles=None):
                  xb, xc, xcb, lr, li, l2, lh = (LB[n_] for n_ in ("xb", "xc", "xcb", "lr", "li", "l2", "lh"))
                  b_xb, b_xc, b_xcb, b_lr, b_li, b_l2, b_lh = (LB["b_" + n_] for n_ in ("xb", "xc", "xcb", "lr", "li", "l2", "lh"))
                  xbase = 2 * (blk % LB["nset"])
                  for cc in range(2):
                      ch = blk * 2 + cc
                      S.op("dve", lambda e, cc=cc, ch=ch: e.tensor_copy(out=xb[:, xbase + cc, 0:4], in_=halo[:, ch, :]),
                           reads=[b_halo[ch]], writes=[b_xb[xbase + cc]])
                      cw = lambda jj, ch=ch: pp[:, P_CW + ch * 4 + jj:P_CW + ch * 4 + jj + 1]
                      S.op("dve", lambda e, cc=cc, ch=ch: e.tensor_scalar(out=xc[:, cc, :], in0=xb[:, xbase + cc, 4:1028], scalar1=cw(3),
                                                                          scalar2=pp[:, P_CB + ch:P_CB + ch + 1], op0=ALU.mult, op1=ALU.add),
                           reads=[b_xb[xbase + cc], b_pp], writes=[b_xc[cc]])
                      for jj in range(3):
                          S.op("dve", lambda e, cc=cc, jj=jj: e.scalar_tensor_tensor(out=xc[:, cc, :], in0=xb[:, xbase + cc, 1 + jj:1025 + jj],
                                                                                     scalar=cw(jj), in1=xc[:, cc, :], op0=ALU.mult, op1=ALU.add),
                               reads=[b_xb[xbase + cc], b_pp, b_xc[cc]], writes=[b_xc[cc]])
                      S.op("dve", lambda e, cc=cc, ch=ch: e.tensor_copy(out=halo[:, ch, :], in_=xb[:, xbase + cc, 1024:1028]),
                           reads=[b_xb[xbase + cc]], writes=[b_halo[ch]])
                      S.op("act", lambda e, cc=cc: e.activation(out=xcb[:, cc, :], in_=xc[:, cc, :], func=AF.Copy),
                           reads=[b_xc[cc]], writes=[b_xcb[cc]])
                  for dc in range(2):
                      ch = blk * 2 + dc
                      for (wmat, bias_off, dst, bd) in ((wab, P_BA, lr, b_lr), (wib, P_BI, li, b_li)):
                          for hf in range(2):
                              pb = next_pmm(); pv = pmm[:, pb, :]
                              for c2 in range(2):
                                  S.op("pe", lambda e, c2=c2, hf=hf, pv=pv, wmat=wmat, dc=dc: e.matmul(
                                      pv, lhsT=wmat[:, blk, c2, dc * 128:(dc + 1) * 128], rhs=xcb[:, c2, hf * 512:(hf + 1) * 512],
                                      start=(c2 == 0), stop=(c2 == 1)), reads=[b_wab, b_xcb[0], b_xcb[1]], writes=[b_pmm[pb]])
                              S.op("act", lambda e, hf=hf, pv=pv, dst=dst, bias_off=bias_off, ch=ch: e.activation(
                                  out=dst[:, hf * 512:(hf + 1) * 512], in_=pv, func=AF.Sigmoid,
                                  bias=pp[:, bias_off + ch:bias_off + ch + 1]), reads=[b_pmm[pb], b_pp], writes=[bd])
                      S.op("act", lambda e, ch=ch: e.activation(out=l2[:], in_=lr[:], func=AF.Exp, scale=lsc[:, 32 + ch:33 + ch]),
                           reads=[b_lr, b_lsc], writes=[b_l2])
                      S.op("act", lambda e, ch=ch: e.activation(out=lr[:], in_=lr[:], func=AF.Exp, scale=lsc[:, 16 + ch:17 + ch]),
                           reads=[b_lr, b_lsc], writes=[b_lr])
                      S.op("dve", lambda e: e.tensor_scalar(out=l2[:], in0=l2[:], scalar1=-1.0, scalar2=1.0, op0=ALU.mult, op1=ALU.add),
                           reads=[b_l2], writes=[b_l2])
                      S.op("dve", lambda e: e.tensor_scalar(out=l2[:], in0=l2[:], scalar1=0.0, scalar2=None, op0=ALU.max),
                           reads=[b_l2], writes=[b_l2])
                      S.op("act", lambda e: e.activation(out=l2[:], in_=l2[:], func=AF.Sqrt), reads=[b_l2], writes=[b_l2])
                      S.op("dve", lambda e: e.scalar_tensor_tensor(out=li[:], in0=l2[:], scalar=vmask[:, j:j + 1], in1=li[:],
                                                                   op0=ALU.mult, op1=ALU.mult), reads=[b_l2, b_li, b_vm], writes=[b_li])
                      S.op("dve", lambda e, dc=dc: e.tensor_tensor(out=l2[:], in0=li[:], in1=xc[:, dc, :], op=ALU.mult),
                           reads=[b_li, b_xc[dc]], writes=[b_l2])
                      S.op("dve", lambda e, ch=ch: e.tensor_tensor_scan(out=lh[:], data0=lr[:], data1=l2[:], initial=lstate[:, ch:ch + 1],
                                                                        op0=ALU.mult, op1=ALU.add),
                           reads=[b_lr, b_l2, b_ls[ch]], writes=[b_lh])
                      S.op("dve", lambda e, ch=ch: e.tensor_copy(out=lstate[:, ch:ch + 1], in_=lh[:, 1023:1024]),
                           reads=[b_lh], writes=[b_ls[ch]])
                      if own:
                          own_lru_out(ch, dc)

              own_ctx = {}

              def own_lru_out(ch, dc):
                  xb, xc, xcb, lr, li, l2, lh = (LB[n_] for n_ in ("xb", "xc", "xcb", "lr", "li", "l2", "lh"))
                  b_xb, b_xc, b_xcb, b_lr, b_li, b_l2, b_lh = (LB["b_" + n_] for n_ in ("xb", "xc", "xcb", "lr", "li", "l2", "lh"))
                  gt = own_ctx["gt"]; b_gt = own_ctx["b_gt"]; ssum = own_ctx["ssum"]; b_ssum = own_ctx["b_ssum"]
                  g = gt[:, dc, :]
                  u = li[:]
                  S.op("dve", lambda e: e.tensor_tensor(out=u, in0=g, in1=g, op=ALU.mult), reads=[b_gt[dc]], writes=[b_li])
                  S.op("dve", lambda e: e.tensor_scalar(out=u, in0=u, scalar1=0.044715, scalar2=1.0, op0=ALU.mult, op1=ALU.add),
                       reads=[b_li], writes=[b_li])
                  S.op("dve", lambda e: e.tensor_tensor(out=u, in0=u, in1=g, op=ALU.mult), reads=[b_li, b_gt[dc]], writes=[b_li])
                  S.op("act", lambda e: e.activation(out=u, in_=u, func=AF.Sigmoid, scale=1.5957691216057308), reads=[b_li], writes=[b_li])
                  S.op("dve", lambda e: e.tensor_tensor(out=u, in0=u, in1=g, op=ALU.mult), reads=[b_li, b_gt[dc]], writes=[b_li])
                  S.op("dve", lambda e: e.tensor_tensor(out=lh[:], in0=lh[:], in1=u, op=ALU.mult), reads=[b_li, b_lh], writes=[b_lh])
                  S.dma("sp", yT_d[ch * 128:(ch + 1) * 128, :], lh[:], reads=[b_lh], writes=[own_ctx["b_yT"][ch]])
                  S.op("act", lambda e: e.activation(out=l2[:], in_=lh[:], func=AF.Square), reads=[b_lh], writes=[b_l2])
                  for hf in range(2):
                      S.op("pe", lambda e, hf=hf: e.matmul(ptf[:, :], lhsT=onesf[:], rhs=l2[:, hf * 512:(hf + 1) * 512], start=True, stop=True),
                           reads=[b_ones, b_l2], writes=[b_ptf])
                      S.op("dve", lambda e, hf=hf: e.tensor_tensor(out=ssum[:, hf * 512:(hf + 1) * 512], in0=ssum[:, hf * 512:(hf + 1) * 512],
                                                                   in1=ptf[:, :], op=ALU.add), reads=[b_ptf, b_ssum], writes=[b_ssum])

              def xb_evac(cc_base):
                  def f(cc, hf, pv, bp):
                      xb = LB["xb"]; b_xb = LB["b_xb"]
                      S.op("act", lambda e: e.activation(out=xb[:, cc_base + cc, 4 + hf * 512:4 + (hf + 1) * 512], in_=pv, func=AF.Copy),
                           reads=[bp], writes=[b_xb[cc_base + cc]])
                  return f

              for j in range(NSLAB - 1):
                  norm_transpose("pa%d" % j, xs[j * SLAB:(j + 1) * SLAB, :], NT, mix_g, AT, b_AT, 0)
                  rsc_ = ExitStack()
                  alloc_ret(rsc_)
                  rope_tables(j)
                  jobs = []
                  for h in range(8):
                      def mk(h):
                          def ld_k():
                              return load_wslab(w_in[:, 2048 + h * 256:2048 + (h + 1) * 256], KC, 256)

                          def cp_k(hd):
                              mm_tok(hd[0], hd[1], AT, b_AT, NT, KC, rope_evac(kt, b_kt))
                              for t in range(NT):
                                  S.op("dve", lambda e, t=t: e.tensor_scalar(out=kt[:, t, :], in0=kt[:, t, :],
                                                                             scalar1=cst[:, C_WGT + h * 56 + j * NT + t:C_WGT + h * 56 + j * NT + t + 1],
                                                                             scalar2=None, op0=ALU.mult), reads=[b_kt, b_cst], writes=[b_kt])

                          def ld_v():
                              return load_wslab(w_in[:, 4096 + h * 256:4096 + (h + 1) * 256], KC, 256)

                          def cp_v(hd):
                              mm_tok(hd[0], hd[1], AT, b_AT, NT, KC, copy_evac(vt, b_vt))
                              for dc in range(2):
                                  r = dc
                                  pv = pms[:, r, 0:256]
                                  for t in range(NT):
                                      S.op("pe", lambda e, t=t, dc=dc, pv=pv: e.matmul(pv, lhsT=kt[:, t, dc * 128:(dc + 1) * 128], rhs=vt[:, t, :],
                                                                                      start=(t == 0), stop=(t == NT - 1)),
                                           reads=[b_kt, b_vt], writes=[b_pms[r]])
                                  S.op("dve", lambda e, dc=dc, pv=pv: e.tensor_tensor(out=Racc[:, h * 2 + dc, :], in0=Racc[:, h * 2 + dc, :], in1=pv, op=ALU.add),
                                       reads=[b_pms[r]], writes=[b_R[h * 2 + dc]])
                          return [(ld_k, cp_k), (ld_v, cp_v)]
                      jobs += mk(h)
                  run_jobs(jobs)
                  S.barrier()
                  rsc_.close()
                  jobs = []
                  lsc_ = ExitStack()
                  alloc_lru(lsc_, 2)
                  for blk in range(8):
                      def mkl(blk):
                          def ld():
                              return load_wslab(w_in[:, 8192 + blk * 256:8192 + (blk + 1) * 256], KC, 256)

                          def cp(hd):
                              mm_feat(hd[0], hd[1], AT, b_AT, SLAB, KC, xb_evac(2 * (blk % 2)))
                              lru_block(j, blk, False)
                          return [(ld, cp)]
                      jobs += mkl(blk)
                  run_jobs(jobs)
                  S.barrier()
                  lsc_.close()

              j = NSLAB - 1
              norm_transpose("pa7", xs[j * SLAB:(j + 1) * SLAB, :], NT, mix_g, AT, b_AT, 0)
              with ExitStack() as po:
                  alloc_ret(po)
                  rope_tables(j)
                  qt = sb("qt", [128, NT, 256], BF16, po); b_qt = S.buf()
                  sg = sb("sg", [128, NT, 256], F32, po); b_sg = S.buf()
                  qT = sb("qT", [128, 2, SLAB], BF16, po); b_qT = S.buf()
                  kT = sb("kT", [128, 2, SLAB], BF16, po); b_kT = S.buf()
                  Rb = sb("Rb", [128, 2, 256], BF16, po); b_Rb = S.buf()
                  PT = sb("PT", [128, 128], BF16, po); b_PT = S.buf()
                  oc = sb("oc", [128, 256], F32, po); b_oc = S.buf()
                  osb = sb("osb", [128, 256], F32, po); b_osb = S.buf()
                  rtb = sb("rtb", [128, 256], BF16, po); b_rtb = S.buf()
                  rTs = sb("rTs", [128, 2, 128], BF16, po); b_rTs = S.buf()
                  kz = sb("kz", [128, 256], BF16, po); b_kz = S.buf()
                  gng = sb("gng", [128, 2048], F32, po); b_gng = S.buf()
                  st6 = sb("st6", [128, 16], F32, po); b_st6 = S.buf()
                  b_mixT = S.bufs(32, "mixT")
                  S.dma("sp", gng[:], gn_g.partition_broadcast(128), writes=[b_gng])

                  def retention_head(h):
                      for (src, bs, dstT, bdT) in ((qt, b_qt, qT, b_qT), (kt, b_kt, kT, b_kT)):
                          for t in range(NT):
                              for dc in range(2):
                                  S.op("pe", lambda e, t=t, dc=dc, src=src: e.transpose(out=ptr[:, dc * 128:(dc + 1) * 128],
                                                                                      in_=src[:, t, dc * 128:(dc + 1) * 128], identity=identb[:]),
                                       reads=[bs, b_idb], writes=[b_ptr])
                              S.op("act", lambda e, t=t, dstT=dstT: e.activation(out=dstT[:, :, t * 128:(t + 1) * 128],
                                                                                 in_=ptr[:, 0:256].rearrange("p (k n) -> p k n", k=2), func=AF.Copy),
                                   reads=[b_ptr], writes=[bdT])
                      for dc in range(2):
                          S.op("act", lambda e, dc=dc: e.activation(out=Rb[:, dc, :], in_=Racc[:, h * 2 + dc, :], func=AF.Copy),
                               reads=[b_R[h * 2 + dc]], writes=[b_Rb])
                      for i in range(NT):
                          cs = slice(i * 128, (i + 1) * 128)
                          pST = pms[:, 0, 0:128]
                          for dc in range(2):
                              S.op("pe", lambda e, dc=dc: e.matmul(pST, lhsT=kT[:, dc, cs], rhs=qT[:, dc, cs], start=(dc == 0), stop=(dc == 1)),
                                   reads=[b_kT, b_qT], writes=[b_pms[0]])
                          S.op("dve", lambda e: e.tensor_tensor(out=PT[:], in0=pST, in1=cst[:, C_DEC + h * 128:C_DEC + (h + 1) * 128], op=ALU.mult),
                               reads=[b_pms[0], b_cst], writes=[b_PT])
                          pOI = pms[:, 0, 256:512]
                          S.op("pe", lambda e: e.matmul(pOI, lhsT=PT[:], rhs=vt[:, i, :], start=True, stop=True),
                               reads=[b_PT, b_vt], writes=[b_pms[1]])
                          pOC = pms[:, 1, 0:256]
                          for dc in range(2):
                              S.op("pe", lambda e, dc=dc: e.matmul(pOC, lhsT=qT[:, dc, cs], rhs=Rb[:, dc, :], start=(dc == 0), stop=(dc == 1)),
                                   reads=[b_qT, b_Rb], writes=[b_pms[2]])
                          S.op("act", lambda e: e.activation(out=oc[:], in_=pOC, func=AF.Copy, scale=cst[:, C_XI + h:C_XI + h + 1]),
                               reads=[b_pms[2], b_cst], writes=[b_oc])
                          S.op("dve", lambda e: e.tensor_tensor(out=osb[:], in0=pOI, in1=oc[:], op=ALU.add),
                               reads=[b_pms[1], b_oc], writes=[b_osb])
                          S.op("dve", lambda e: e.bn_stats(out=st6[:, 0:6], in_=osb[:]), reads=[b_osb], writes=[b_st6])
                          S.op("dve", lambda e: e.bn_aggr(out=st6[:, 8:10], in_=st6[:, 0:6]), reads=[b_st6], writes=[b_st6])
                          S.op("dve", lambda e: e.tensor_scalar(out=st6[:, 10:11], in0=st6[:, 9:10], scalar1=1e-5, scalar2=None, op0=ALU.add),
                               reads=[b_st6], writes=[b_st6])
                          S.op("act", lambda e: e.activation(out=st6[:, 10:11], in_=st6[:, 10:11], func=AF.Sqrt), reads=[b_st6], writes=[b_st6])
                          S.op("dve", lambda e: e.reciprocal(out=st6[:, 11:12], in_=st6[:, 10:11]), reads=[b_st6], writes=[b_st6])
                          S.op("dve", lambda e: e.tensor_scalar(out=osb[:], in0=osb[:], scalar1=st6[:, 8:9], scalar2=st6[:, 11:12],
                                                                op0=ALU.subtract, op1=ALU.mult), reads=[b_osb, b_st6], writes=[b_osb])
                          S.op("dve", lambda e: e.tensor_tensor(out=osb[:], in0=osb[:], in1=gng[:, h * 256:(h + 1) * 256], op=ALU.mult),
                               reads=[b_osb, b_gng], writes=[b_osb])
                          S.op("dve", lambda e: e.tensor_tensor(out=rtb[:], in0=osb[:], in1=sg[:, i, :], op=ALU.mult),
                               reads=[b_osb, b_sg], writes=[b_rtb])
                          for dc in range(2):
                              S.op("pe", lambda e, dc=dc: e.transpose(out=ptr[:, 512 + dc * 128:512 + (dc + 1) * 128], in_=rtb[:, dc * 128:(dc + 1) * 128],
                                                                      identity=identb[:]), reads=[b_rtb, b_idb], writes=[b_ptr])
                          S.op("act", lambda e: e.activation(out=rTs[:], in_=ptr[:, 512:768].rearrange("p (k n) -> p k n", k=2), func=AF.Copy),
                               reads=[b_ptr], writes=[b_rTs])
                          for dc in range(2):
                              r0 = h * 256 + dc * 128
                              S.dma("sp", mixT_d[r0:r0 + 128, cs], rTs[:, dc, :], reads=[b_rTs], writes=[b_mixT[h * 2 + dc]])
                          if i < NT - 1:
                              S.op("dve", lambda e: e.tensor_scalar(out=kz[:], in0=kt[:, i, :], scalar1=cst[:, C_ZETA + h:C_ZETA + h + 1],
                                                                    scalar2=None, op0=ALU.mult), reads=[b_kt, b_cst], writes=[b_kz])
                              for dc in range(2):
                                  pRU = pms[:, 1, 256:512]
                                  S.op("pe", lambda e, dc=dc: e.matmul(pRU, lhsT=kz[:, dc * 128:(dc + 1) * 128], rhs=vt[:, i, :], start=True, stop=True),
                                       reads=[b_kz, b_vt], writes=[b_pms[3]])
                                  S.op("dve", lambda e, dc=dc: e.scalar_tensor_tensor(out=Racc[:, h * 2 + dc, :], in0=Racc[:, h * 2 + dc, :],
                                                                                      scalar=cst[:, C_CD + h:C_CD + h + 1], in1=pRU,
                                                                                      op0=ALU.mult, op1=ALU.add),
                                       reads=[b_pms[3], b_cst], writes=[b_R[h * 2 + dc]])
                                  S.op("act", lambda e, dc=dc: e.activation(out=Rb[:, dc, :], in_=Racc[:, h * 2 + dc, :], func=AF.Copy),
                                       reads=[b_R[h * 2 + dc]], writes=[b_Rb])

                  jobs = []
                  for h in range(8):
                      def mko(h):
                          def ld(c0):
                              return lambda: load_wslab(w_in[:, c0 + h * 256:c0 + (h + 1) * 256], KC, 256)

                          def cp_q(hd):
                              mm_tok(hd[0], hd[1], AT, b_AT, NT, KC, rope_evac(qt, b_qt))

                          def cp_k(hd):
                              mm_tok(hd[0], hd[1], AT, b_AT, NT, KC, rope_evac(kt, b_kt))

                          def cp_v(hd):
                              mm_tok(hd[0], hd[1], AT, b_AT, NT, KC, copy_evac(vt, b_vt))

                          def cp_g(hd):
                              def ev(t, pv, bp):
                                  S.op("act", lambda e: e.activation(out=sg[:, t, :], in_=pv, func=AF.Silu), reads=[bp], writes=[b_sg])
                              mm_tok(hd[0], hd[1], AT, b_AT, NT, KC, ev)
                              retention_head(h)
                          return [(ld(0), cp_q), (ld(2048), cp_k), (ld(4096), cp_v), (ld(6144), cp_g)]
                      jobs += mko(h)
                  run_jobs(jobs)
                  S.barrier()

              with ExitStack() as pl:
                  alloc_lru(pl)
                  gt = sb("gt", [128, 2, 1024], F32, pl); b_gt = S.bufs(2)
                  ssum = sb("ssum", [128, 1024], F32, pl); b_ssum = S.buf()
                  b_yT = S.bufs(16, "yT")
                  S.op("dve", lambda e: e.memset(ssum[:], 0.0), writes=[b_ssum])
                  own_ctx.update(gt=gt, b_gt=b_gt, ssum=ssum, b_ssum=b_ssum, b_yT=b_yT)
                  jobs = []
                  for blk in range(8):
                      def mkl2(blk):
                          def ld_x():
                              return load_wslab(w_in[:, 8192 + blk * 256:8192 + (blk + 1) * 256], KC, 256)

                          def cp_x(hd):
                              mm_feat(hd[0], hd[1], AT, b_AT, SLAB, KC, xb_evac(0))

                          def ld_g():
                              return load_wslab(w_in[:, 10240 + blk * 256:10240 + (blk + 1) * 256], KC, 256)

                          def cp_g(hd):
                              def ev(cc, hf, pv, bp):
                                  S.op("act", lambda e: e.activation(out=gt[:, cc, hf * 512:(hf + 1) * 512], in_=pv, func=AF.Copy),
                                       reads=[bp], writes=[b_gt[cc]])
                              mm_feat(hd[0], hd[1], AT, b_AT, SLAB, KC, ev)
                              lru_block(NSLAB - 1, blk, True)
                          return [(ld_x, cp_x), (ld_g, cp_g)]
                      jobs += mkl2(blk)
                  run_jobs(jobs)
                  rms_rstd(ssum[:], 2048.0, 1e-6, ssum[:], b_ssum)
                  lh = LB["lh"]; b_lh = LB["b_lh"]; xcb = LB["xcb"]; b_xcb = LB["b_xcb"]
                  for ch in range(16):
                      S.dma("sp", lh[:], yT_d[ch * 128:(ch + 1) * 128, :], reads=[b_yT[ch]], writes=[b_lh])
                      S.op("dve", lambda e, ch=ch: e.scalar_tensor_tensor(out=xcb[:, 0, :], in0=lh[:], scalar=pp[:, P_LG + ch:P_LG + ch + 1],
                                                                          in1=ssum[:], op0=ALU.mult, op1=ALU.mult),
                           reads=[b_lh, b_pp, b_ssum], writes=[b_xcb[0]])
                      S.dma("sp", mixT_d[2048 + ch * 128:2048 + (ch + 1) * 128, :], xcb[:, 0, :], reads=[b_xcb[0]], writes=[b_mixT[16 + ch]])
                  S.barrier()
              S.barrier()

        b_x1 = [[S.buf() for _ in range(16)] for _ in range(NT)]
        b_x2 = [[S.buf() for _ in range(16)] for _ in range(NT)]

        def load_AT_from(dram_T, deps):
            for k in range(KC):
                S.dma("sp", AT[:, k, :], dram_T[k * 128:(k + 1) * 128, :], reads=[deps[k]], writes=[b_AT])

        def resid_linear(ph, W, src_rows, src_bufs, dst_rows=None, dst_bufs=None):
            dst_rows = x1_d if dst_rows is None else dst_rows
            dst_bufs = b_x1 if dst_bufs is None else dst_bufs
            with ExitStack() as ls:
                rs = sb(ph + "rs", [128, 4, 256], F32, ls); b_rs = S.bufs(4)
                ri = [0]
                jobs = []
                for s in range(16):
                    def mk(s):
                        def ld():
                            return load_wslab(W[:, s * 256:(s + 1) * 256], KC, 256)

                        def cp(hd):
                            def ev(t, pv, bp):
                                r = ri[0]; ri[0] = (r + 1) % 4
                                S.dma("sp", rs[:, r, :], src_rows[t * 128:(t + 1) * 128, s * 256:(s + 1) * 256],
                                      reads=[src_bufs[t][s]] if src_bufs else [], writes=[b_rs[r]])
                                S.op("dve", lambda e: e.tensor_tensor(out=rs[:, r, :], in0=pv, in1=rs[:, r, :], op=ALU.add),
                                     reads=[bp, b_rs[r]], writes=[b_rs[r]])
                                S.dma("sp", dst_rows[t * 128:(t + 1) * 128, s * 256:(s + 1) * 256], rs[:, r, :], reads=[b_rs[r]], writes=[dst_bufs[t][s]])
                            mm_tok(hd[0], hd[1], AT, b_AT, NT, KC, ev)
                        return (ld, cp)
                    jobs.append(mk(s))
                run_jobs(jobs)
                S.barrier()

        load_AT_from(mixT_d, b_mixT)
        resid_linear("wo1", w_out, xs[(NSLAB - 1) * SLAB:NSLAB * SLAB, :], None)

        if stop_after == "A":
            S.finish([b for row in b_x1 for b in row])
            return nc

        with ExitStack() as pb_:
            memT = sb("memT", [128, KC, 256], BF16, pb_); b_memT = S.buf()
            norm_transpose("pm", mem_d, 2, mem_g, memT, b_memT, 0)
            KTm = sb("KTm", [128, KC, 256], BF16, pb_); b_KT = S.buf()
            Vm = sb("Vm", [128, 2, D], BF16, pb_); b_Vm = S.buf()
            jobs = []
            for s in range(16):
                def mkk(s):
                    def ld():
                        return load_wslab(wk[:, s * 256:(s + 1) * 256], KC, 256)

                    def cp(hd):
                        def ev(cc, hf, pv, bp):
                            S.op("act", lambda e: e.activation(out=KTm[:, s * 2 + cc, :], in_=pv, func=AF.Copy), reads=[bp], writes=[b_KT])
                        mm_feat(hd[0], hd[1], memT, b_memT, 256, KC, ev)
                    return (ld, cp)

                def mkv(s):
                    def ld():
                        return load_wslab(wv[:, s * 256:(s + 1) * 256], KC, 256)

                    def cp(hd):
                        def ev(t, pv, bp):
                            S.op("act", lambda e: e.activation(out=Vm[:, t, s * 256:(s + 1) * 256], in_=pv, func=AF.Copy), reads=[bp], writes=[b_Vm])
                        mm_tok(hd[0], hd[1], memT, b_memT, 2, KC, ev)
                    return (ld, cp)
                jobs += [mkk(s), mkv(s)]
            run_jobs(jobs)

            x1_all = [b for row in b_x1 for b in row]
            b_x1tile = S.buf()
            for b in x1_all:
                pass
            sync_tok = S.buf()
            S.op("dve", lambda e: e.memset(sdesc[:, 0:1], 0.0), reads=[], writes=[b_sd])

            def norm_transpose_x1(ph, gvec):
                deps = []
                for b in x1_all:
                    if b.writer is not None:
                        deps.append(b.writer)
                S._wait(S.E["sp"], deps)
                norm_transpose(ph, x1_d, NT, gvec, AT, b_AT, 0)

            if stop_after == "B1":
                S.barrier(); S.finish([]); return nc
            norm_transpose_x1("pb", xat_g)
            if stop_after == "B2":
                S.barrier(); S.finish([]); return nc
            b_oT = S.bufs(32, "oT")
            with ExitStack() as px:
                qx = sb("qx", [128, 8, SLAB], BF16, px); b_qx = S.buf()
                prob = sb("prob", [128, 256], F32, px); b_prob = S.buf()
                probb = sb("probb", [128, 256], BF16, px); b_probb = S.buf()
                pT = sb("pT", [128, 2, SLAB], BF16, px); b_pT = S.buf()
                sm = sb("sm", [128, 8], F32, px); b_sm = S.buf()
                oTs = sb("oTs", [128, 2, 512], BF16, px); b_oTs = S.bufs(2)
                for hx in range(4):
                    jobs = []
                    for s4 in range(4):
                        def mkq(s4):
                            def ld():
                                c0 = hx * 1024 + s4 * 256
                                return load_wslab(wq[:, c0:c0 + 256], KC, 256)

                            def cp(hd):
                                def ev(cc, hf, pv, bp):
                                    S.op("act", lambda e: e.activation(out=qx[:, s4 * 2 + cc, hf * 512:(hf + 1) * 512], in_=pv, func=AF.Copy),
                                         reads=[bp], writes=[b_qx])
                                mm_feat(hd[0], hd[1], AT, b_AT, SLAB, KC, ev)
                            return (ld, cp)
                        jobs.append(mkq(s4))
                    run_jobs(jobs)
                    for t in range(NT):
                        pb = next_pmm(); pv = pmm[:, pb, 0:256]
                        for dc in range(8):
                            S.op("pe", lambda e, dc=dc, pv=pv: e.matmul(pv, lhsT=qx[:, dc, t * 128:(t + 1) * 128], rhs=KTm[:, hx * 8 + dc, :],
                                                                      start=(dc == 0), stop=(dc == 7)), reads=[b_qx, b_KT], writes=[b_pmm[pb]])
                        S.op("dve", lambda e, pv=pv: e.reduce_max(out=sm[:, 0:1], in_=pv, axis=AX.X), reads=[b_pmm[pb]], writes=[b_sm])
                        S.op("dve", lambda e: e.tensor_scalar(out=sm[:, 1:2], in0=sm[:, 0:1], scalar1=-1.0 / 32.0, scalar2=None, op0=ALU.mult),
                             reads=[b_sm], writes=[b_sm])
                        S.op("act", lambda e, pv=pv: e.activation(out=prob[:], in_=pv, func=AF.Exp, scale=1.0 / 32.0, bias=sm[:, 1:2]),
                             reads=[b_pmm[pb], b_sm], writes=[b_prob])
                        S.op("dve", lambda e: e.reduce_sum(out=sm[:, 2:3], in_=prob[:], axis=AX.X), reads=[b_prob], writes=[b_sm])
                        S.op("dve", lambda e: e.reciprocal(out=sm[:, 3:4], in_=sm[:, 2:3]), reads=[b_sm], writes=[b_sm])
                        S.op("dve", lambda e: e.tensor_scalar(out=probb[:], in0=prob[:], scalar1=sm[:, 3:4], scalar2=None, op0=ALU.mult),
                             reads=[b_prob, b_sm], writes=[b_probb])
                        for mc in range(2):
                            S.op("pe", lambda e, mc=mc: e.transpose(out=ptr[:, mc * 128:(mc + 1) * 128], in_=probb[:, mc * 128:(mc + 1) * 128],
                                                                    identity=identb[:]), reads=[b_probb, b_idb], writes=[b_ptr])
                        S.op("act", lambda e, t=t: e.activation(out=pT[:, :, t * 128:(t + 1) * 128],
                                                                in_=ptr[:, 0:256].rearrange("p (k n) -> p k n", k=2), func=AF.Copy),
                             reads=[b_ptr], writes=[b_pT])
                    for dvc in range(8):
                        for hf in range(2):
                            pb = next_pmm(); pv = pmm[:, pb, :]
                            for mc in range(2):
                                S.op("pe", lambda e, mc=mc, pv=pv, hf=hf, dvc=dvc: e.matmul(
                                    pv, lhsT=Vm[:, mc, hx * 1024 + dvc * 128:hx * 1024 + (dvc + 1) * 128], rhs=pT[:, mc, hf * 512:(hf + 1) * 512],
                                    start=(mc == 0), stop=(mc == 1)), reads=[b_Vm, b_pT], writes=[b_pmm[pb]])
                            S.op("act", lambda e, pv=pv, hf=hf: e.activation(out=oTs[:, hf, :], in_=pv, func=AF.Copy), reads=[b_pmm[pb]], writes=[b_oTs[hf]])
                            r0 = hx * 1024 + dvc * 128
                            S.dma("sp", mixT_d[r0:r0 + 128, hf * 512:(hf + 1) * 512], oTs[:, hf, :], reads=[b_oTs[hf], b_AT],
                                  writes=[b_oT[hx * 8 + dvc]])
                S.barrier()
            if stop_after == "B3":
                S.barrier(); S.finish([]); return nc
            load_AT_from(mixT_d, b_oT)
            if stop_after == "B4":
                S.barrier(); S.finish([]); return nc
            resid_linear("wo2", wo, x1_d, b_x1, x2_d, b_x2)
            S.barrier()

        if stop_after == "B":
            S.finish([b for row in b_x2 for b in row])
            return nc

        with ExitStack() as pc:
            Wt = sb("Wt", [128, NT, 32], F32, pc); b_Wt = S.buf()
            wrs = sb("wrs", [128, KC * 36], F32, pc); b_wrs = S.buf()
            whi = sb("whi", [128, KC, 36], BF16, pc); wlo = sb("wlo", [128, KC, 36], BF16, pc); b_whl = S.buf()
            rbb = sb("rbb", [128, 36], F32, pc); b_rbb = S.buf()
            S.dma("sp", wrs[:], wr_d, writes=[b_wrs])
            S.dma("sp", rbb[:], rb_d.partition_broadcast(128), writes=[b_rbb])
            whi2 = whi[:, :, :].rearrange("p k n -> p (k n)"); wlo2 = wlo[:, :, :].rearrange("p k n -> p (k n)")
            S.op("act", lambda e: e.activation(out=whi2, in_=wrs[:], func=AF.Copy), reads=[b_wrs], writes=[b_whl])
            S.op("dve", lambda e: e.tensor_tensor(out=wlo2, in0=wrs[:], in1=whi2, op=ALU.subtract), reads=[b_wrs, b_whl], writes=[b_whl])
            deps = []
            for b in [b for row in b_x2 for b in row]:
                if b.writer is not None:
                    deps.append(b.writer)
            S._wait(S.E["sp"], deps)
            with ExitStack() as pr:
                gbc = sb("c_gbc", [128, D], F32, pr); b_g = S.buf()
                xt = sb("c_xt", [128, D], F32, pr); b_xt = S.buf()
                hit = sb("c_hit", [128, D], BF16, pr); b_hit = S.buf()
                lot = sb("c_lot", [128, D], BF16, pr); b_lot = S.buf()
                loT = sb("c_loT", [128, KC, 128], BF16, pr); b_loT = S.buf()
                ss = sb("c_ss", [128, 2], F32, pr); b_ss = S.buf()
                lg = sb("c_lg", [128, 40], F32, pr); b_lg = S.buf()
                rw = sb("c_rw", [128, 8, 32], F32, pr); b_rw = S.buf()
                S.dma("sp", gbc[:], moe_g.partition_broadcast(128), writes=[b_g])
                for t in range(NT):
                    S.dma("sp", xt[:], x2_d[t * 128:(t + 1) * 128, :], writes=[b_xt])
                    S.op("act", lambda e: e.activation(out=hit[:], in_=xt[:], func=AF.Square), reads=[b_xt], writes=[b_hit])
                    S.op("dve", lambda e: e.reduce_sum(out=ss[:, 0:1], in_=hit[:], axis=AX.X), reads=[b_hit], writes=[b_ss])
                    rms_rstd(ss[:, 0:1], float(D), 1e-6, ss[:, 1:2], b_ss)
                    S.op("dve", lambda e: e.scalar_tensor_tensor(out=xt[:], in0=xt[:], scalar=ss[:, 1:2], in1=gbc[:],
                                                                 op0=ALU.mult, op1=ALU.mult), reads=[b_xt, b_ss, b_g], writes=[b_xt])
                    S.op("act", lambda e: e.activation(out=hit[:], in_=xt[:], func=AF.Copy), reads=[b_xt], writes=[b_hit])
                    S.op("dve", lambda e: e.tensor_tensor(out=lot[:], in0=xt[:], in1=hit[:], op=ALU.subtract), reads=[b_xt, b_hit], writes=[b_lot])
                    for (srcb, bsrc, dstT, bdst, c0) in ((hit, b_hit, AT, b_AT, t * 128), (lot, b_lot, loT, b_loT, 0)):
                        for g8 in range(4):
                            for jx in range(8):
                                k = g8 * 8 + jx
                                S.op("pe", lambda e, k=k, jx=jx, srcb=srcb: e.transpose(out=ptr[:, jx * 128:(jx + 1) * 128],
                                                                                      in_=srcb[:, k * 128:(k + 1) * 128], identity=identb[:]),
                                     reads=[bsrc, b_idb], writes=[b_ptr])
                            src = ptr[:, :].rearrange("p (k n) -> p k n", k=8)
                            S.op("act", lambda e, g8=g8, src=src, dstT=dstT, c0=c0: e.activation(
                                out=dstT[:, g8 * 8:(g8 + 1) * 8, c0:c0 + 128], in_=src, func=AF.Copy), reads=[b_ptr], writes=[bdst])
                    pl_ = pms[:, 0, 0:36]
                    nmm = 0
                    for k in range(KC):
                        for (lh_, bl_, wv_) in ((AT[:, k, t * 128:(t + 1) * 128], b_AT, whi), (AT[:, k, t * 128:(t + 1) * 128], b_AT, wlo),
                                                (loT[:, k, :], b_loT, whi)):
                            S.op("pe", lambda e, k=k, lh_=lh_, wv_=wv_, nmm=nmm: e.matmul(pl_, lhsT=lh_, rhs=wv_[:, k, :],
                                                                                       start=(nmm == 0), stop=(nmm == 3 * KC - 1)),
                                 reads=[bl_, b_whl], writes=[b_pms[0]])
                            nmm += 1
                    S.op("dve", lambda e: e.tensor_tensor(out=lg[:, 0:36], in0=pl_, in1=rbb[:], op=ALU.add), reads=[b_pms[0], b_rbb], writes=[b_lg])
                    S.op("dve", lambda e: e.reduce_max(out=lg[:, 36:37], in_=lg[:, 0:4], axis=AX.X), reads=[b_lg], writes=[b_lg])
                    S.op("dve", lambda e: e.tensor_scalar(out=rw[:, 0, 0:4], in0=lg[:, 0:4], scalar1=lg[:, 36:37], scalar2=None, op0=ALU.subtract),
                         reads=[b_lg], writes=[b_rw])
                    S.op("act", lambda e: e.activation(out=rw[:, 1, 0:4], in_=rw[:, 0, 0:4], func=AF.Exp), reads=[b_rw], writes=[b_rw])
                    S.op("dve", lambda e: e.reduce_sum(out=lg[:, 37:38], in_=rw[:, 1, 0:4], axis=AX.X), reads=[b_rw], writes=[b_lg])
                    S.op("dve", lambda e: e.reciprocal(out=lg[:, 37:38], in_=lg[:, 37:38]), reads=[b_lg], writes=[b_lg])
                    S.op("dve", lambda e: e.tensor_scalar(out=rw[:, 2, 0:4], in0=lg[:, 0:4], scalar1=lg[:, 36:37], scalar2=None, op0=ALU.is_ge),
                         reads=[b_lg], writes=[b_rw])
                    S.op("dve", lambda e: e.tensor_scalar(out=rw[:, 2, 0:4], in0=rw[:, 2, 0:4], scalar1=-1.0, scalar2=1e30, op0=ALU.add, op1=ALU.mult),
                         reads=[b_rw], writes=[b_rw])
                    for g in range(4):
                        S.op("dve", lambda e, g=g: e.tensor_scalar(out=rw[:, 3, g * 8:(g + 1) * 8], in0=lg[:, 4 + g * 8:4 + (g + 1) * 8],
                                                                   scalar1=rw[:, 2, g:g + 1], scalar2=None, op0=ALU.add), reads=[b_lg, b_rw], writes=[b_rw])
                    S.op("dve", lambda e: e.reduce_max(out=lg[:, 38:39], in_=rw[:, 3, :], axis=AX.X), reads=[b_rw], writes=[b_lg])
                    S.op("dve", lambda e: e.tensor_scalar(out=rw[:, 4, :], in0=rw[:, 3, :], scalar1=lg[:, 38:39], scalar2=None, op0=ALU.is_ge),
                         reads=[b_rw, b_lg], writes=[b_rw])
                    S.op("dve", lambda e: e.scalar_tensor_tensor(out=rw[:, 5, :], in0=rw[:, 4, :], scalar=-1e30, in1=rw[:, 3, :], op0=ALU.mult, op1=ALU.add),
                         reads=[b_rw], writes=[b_rw])
                    S.op("dve", lambda e: e.reduce_max(out=lg[:, 39:40], in_=rw[:, 5, :], axis=AX.X), reads=[b_rw], writes=[b_lg])
                    S.op("dve", lambda e: e.tensor_scalar(out=rw[:, 6, :], in0=rw[:, 5, :], scalar1=lg[:, 39:40], scalar2=None, op0=ALU.is_ge),
                         reads=[b_rw, b_lg], writes=[b_rw])
                    S.op("dve", lambda e: e.tensor_tensor(out=ss[:, 0:1], in0=lg[:, 38:39], in1=lg[:, 39:40], op=ALU.subtract), reads=[b_lg], writes=[b_ss])
                    S.op("act", lambda e: e.activation(out=ss[:, 0:1], in_=ss[:, 0:1], func=AF.Sigmoid), reads=[b_ss], writes=[b_ss])
                    S.op("dve", lambda e: e.tensor_tensor(out=ss[:, 0:1], in0=ss[:, 0:1], in1=lg[:, 37:38], op=ALU.mult), reads=[b_ss, b_lg], writes=[b_ss])
                    S.op("dve", lambda e: e.tensor_tensor(out=ss[:, 1:2], in0=lg[:, 37:38], in1=ss[:, 0:1], op=ALU.subtract), reads=[b_ss, b_lg], writes=[b_ss])
                    S.op("dve", lambda e: e.tensor_scalar(out=rw[:, 7, :], in0=rw[:, 6, :], scalar1=ss[:, 1:2], scalar2=None, op0=ALU.mult),
                         reads=[b_rw, b_ss], writes=[b_rw])
                    S.op("dve", lambda e, t=t: e.scalar_tensor_tensor(out=Wt[:, t, :], in0=rw[:, 4, :], scalar=ss[:, 0:1], in1=rw[:, 7, :],
                                                                      op0=ALU.mult, op1=ALU.add), reads=[b_rw, b_ss], writes=[b_Wt])

                S.barrier()
            if stop_after == "C1":
                S.barrier(); S.finish([]); return nc
            with ExitStack() as pe_:
                actT = sb("actT", [128, 8, SLAB], BF16, pe_); b_actT = S.bufs(8)
                sil = sb("sil", [128, 2, 512], F32, pe_); b_sil = S.bufs(2)
                accp = sb("accp", [128, 4, 1024], F32, pe_); b_accp = S.bufs(4)
                pg_hold = {}
                ai = [0]; si = [0]
                b_x3 = [[S.buf() for _ in range(4)] for _ in range(NT)]
                for t in range(NT):
                    for q4 in range(4):
                        b_x3[t][q4].writer = None
                jobs = []
                for ex in range(nexp):
                    def mke(ex):
                        js = []
                        for s4 in range(4):
                            def ldg(s4=s4):
                                return load_wslab(wg[ex, :, s4 * 256:(s4 + 1) * 256], KC, 256)

                            def cpg(hd, s4=s4):
                                pg_hold[s4] = hd
                            def ldu(s4=s4):
                                return load_wslab(wu[ex, :, s4 * 256:(s4 + 1) * 256], KC, 256)

                            def cpu(hd, s4=s4):
                                gw, gb = pg_hold.pop(s4)
                                uw, ub = hd
                                for cc in range(2):
                                    dch = s4 * 2 + cc
                                    for hf in range(2):
                                        pbg = next_pmm(); pvg = pmm[:, pbg, :]
                                        for k in range(KC):
                                            S.op("pe", lambda e, k=k, pvg=pvg, cc=cc, hf=hf: e.matmul(
                                                pvg, lhsT=gw[:, k, cc * 128:(cc + 1) * 128], rhs=AT[:, k, hf * 512:(hf + 1) * 512],
                                                start=(k == 0), stop=(k == KC - 1)), reads=[b_AT, gb], writes=[b_pmm[pbg]])
                                        pbu = next_pmm(); pvu = pmm[:, pbu, :]
                                        for k in range(KC):
                                            S.op("pe", lambda e, k=k, pvu=pvu, cc=cc, hf=hf: e.matmul(
                                                pvu, lhsT=uw[:, k, cc * 128:(cc + 1) * 128], rhs=AT[:, k, hf * 512:(hf + 1) * 512],
                                                start=(k == 0), stop=(k == KC - 1)), reads=[b_AT, ub], writes=[b_pmm[pbu]])
                                        r = si[0]; si[0] = (r + 1) % 2
                                        S.op("act", lambda e, r=r, pvg=pvg: e.activation(out=sil[:, r, :], in_=pvg, func=AF.Silu),
                                             reads=[b_pmm[pbg]], writes=[b_sil[r]])
                                        S.op("dve", lambda e, r=r, pvu=pvu, dch=dch, hf=hf: e.tensor_tensor(
                                            out=actT[:, dch, hf * 512:(hf + 1) * 512], in0=pvu, in1=sil[:, r, :], op=ALU.mult),
                                             reads=[b_pmm[pbu], b_sil[r]], writes=[b_actT[dch]])
                            js += [(ldg, cpg), (ldu, cpu)]
                        for q4 in range(4):
                            def ldd(q4=q4):
                                return load_wslab(wd[ex, :, q4 * 1024:(q4 + 1) * 1024], 8, 1024)

                            def cpd(hd, q4=q4):
                                dw, db = hd
                                srcd = x2_d if ex % 2 == 0 else x3_d
                                dstd = x3_d if ex % 2 == 0 else x2_d
                                pend = {}

                                def issue_load(t):
                                    a = ai[0]; ai[0] = (a + 1) % 4
                                    S.dma("sp", accp[:, a, :], srcd[t * 128:(t + 1) * 128, q4 * 1024:(q4 + 1) * 1024],
                                          reads=[b_x3[t][q4]], writes=[b_accp[a]])
                                    pend[t] = a
                                issue_load(0); issue_load(1)
                                for t in range(NT):
                                    a = pend.pop(t)
                                    if t + 2 < NT:
                                        issue_load(t + 2)
                                    for g2 in range(2):
                                        pb = next_pmm(); pv = pmm[:, pb, :]
                                        for k in range(8):
                                            S.op("pe", lambda e, k=k, pv=pv, t=t, g2=g2: e.matmul(
                                                pv, lhsT=actT[:, k, t * 128:(t + 1) * 128], rhs=dw[:, k, g2 * 512:(g2 + 1) * 512],
                                                start=(k == 0), stop=(k == 7)), reads=b_actT + [db], writes=[b_pmm[pb]])
                                        S.op("dve", lambda e, pv=pv, a=a, g2=g2, t=t: e.scalar_tensor_tensor(
                                            out=accp[:, a, g2 * 512:(g2 + 1) * 512], in0=pv, scalar=Wt[:, t, ex:ex + 1],
                                            in1=accp[:, a, g2 * 512:(g2 + 1) * 512], op0=ALU.mult, op1=ALU.add),
                                             reads=[b_pmm[pb], b_Wt, b_accp[a]], writes=[b_accp[a]])
                                    S.dma("sp", dstd[t * 128:(t + 1) * 128, q4 * 1024:(q4 + 1) * 1024], accp[:, a, :],
                                          reads=[b_accp[a]], writes=[b_x3[t][q4]])
                            js.append((ldd, cpd))
                        return js
                    jobs += mke(ex)
                run_jobs(jobs)
                S.barrier()

            deps = []
            for row in b_x3:
                for b in row:
                    if b.writer is not None:
                        deps.append(b.writer)
            S._wait(S.E["sp"], deps)
            with ExitStack() as pf:
                gbc = sb("f_gbc", [128, D], F32, pf); b_g = S.buf()
                xt = sb("f_xt", [128, 2, D], F32, pf); b_xt = S.bufs(2)
                ss = sb("f_ss", [128, 4], F32, pf); b_ss = S.bufs(2)
                junk = sb("f_junk", [128, D], F32, pf); b_junk = S.buf()
                b_out = S.bufs(NT)
                S.dma("sp", gbc[:], fin_g.partition_broadcast(128), writes=[b_g])
                for t in range(NT):
                    r = t % 2
                    S.dma("sp", xt[:, r, :], x2_d[t * 128:(t + 1) * 128, :], writes=[b_xt[r]])
                    S.op("act", lambda e, r=r: e.activation(out=junk[:], in_=xt[:, r, :], func=AF.Square), reads=[b_xt[r]], writes=[b_junk])
                    S.op("dve", lambda e, r=r: e.reduce_sum(out=ss[:, 2 * r:2 * r + 1], in_=junk[:], axis=AX.X), reads=[b_junk], writes=[b_ss[r]])
                    rms_rstd(ss[:, 2 * r:2 * r + 1], float(D), 1e-6, ss[:, 2 * r + 1:2 * r + 2], b_ss[r])
                    S.op("dve", lambda e, r=r: e.scalar_tensor_tensor(out=xt[:, r, :], in0=xt[:, r, :], scalar=ss[:, 2 * r + 1:2 * r + 2], in1=gbc[:],
                                                                      op0=ALU.mult, op1=ALU.mult), reads=[b_xt[r], b_ss[r], b_g], writes=[b_xt[r]])
                    S.dma("sp", out_d[t * 128:(t + 1) * 128, :], xt[:, r, :], reads=[b_xt[r]], writes=[b_out[t]])
                S.finish(b_out)
    return nc


_CACHE = {}


def _prep_inputs(inp):
    f32 = np.float32
    x = np.asarray(inp["x"], f32)[0]
    pos = np.asarray(inp["positions"])[0].astype(np.int32)
    cst = make_consts()
    lru_cw = np.asarray(inp["lru_conv_w"], f32)[0]
    pp = np.zeros((128, NPP), f32)
    for ch in range(16):
        sl = slice(ch * 128, (ch + 1) * 128)
        for jj in range(4):
            pp[:, P_CW + ch * 4 + jj] = lru_cw[jj, sl]
        pp[:, P_CB + ch] = np.asarray(inp["lru_conv_b"], f32)[0, sl]
        pp[:, P_BA + ch] = np.asarray(inp["lru_b_a"], f32)[0, sl]
        pp[:, P_BI + ch] = np.asarray(inp["lru_b_i"], f32)[0, sl]
        pp[:, P_LAM + ch] = np.asarray(inp["lru_lambda"], f32)[0, sl]
        pp[:, P_LG + ch] = np.asarray(inp["lru_norm_g"], f32)[0, sl]
    wr = np.concatenate([np.asarray(inp["router_group_w"], f32)[0],
                         np.asarray(inp["router_expert_w"], f32)[0]], axis=1)
    wr = np.ascontiguousarray(wr.reshape(KC, 128, 36).transpose(1, 0, 2).reshape(128, KC * 36))
    rb = np.ascontiguousarray(np.concatenate([np.asarray(inp["router_group_b"], f32)[0],
                                              np.asarray(inp["router_expert_b"], f32)[0]])[None, :])
    shared = {
        "mem": np.ascontiguousarray(np.asarray(inp["mem"], f32)[0]),
        "cst": cst, "pp": pp,
        "mix_g": np.asarray(inp["mix_norm_g"], f32).reshape(1, D),
        "gn_g": np.asarray(inp["ret_norm_g"], f32).reshape(1, 2048),
        "xat_g": np.asarray(inp["xattn_norm_g"], f32).reshape(1, D),
        "mem_g": np.asarray(inp["mem_norm_g"], f32).reshape(1, D),
        "moe_g": np.asarray(inp["moe_norm_g"], f32).reshape(1, D),
        "fin_g": np.asarray(inp["final_norm_g"], f32).reshape(1, D),
        "rb": rb, "wr": wr,
        "w_in": np.asarray(inp["w_in"], f32)[0], "w_out": np.asarray(inp["w_out"], f32)[0],
        "wq": np.asarray(inp["xattn_wq"], f32)[0], "wk": np.asarray(inp["xattn_wk"], f32)[0],
        "wv": np.asarray(inp["xattn_wv"], f32)[0], "wo": np.asarray(inp["xattn_wo"], f32)[0],
        "w_a": np.asarray(inp["lru_w_a"], f32)[0], "w_i": np.asarray(inp["lru_w_i"], f32)[0],
        "wg": np.asarray(inp["expert_w_gate"], f32)[0], "wu": np.asarray(inp["expert_w_up"], f32)[0],
        "wd": np.asarray(inp["expert_w_down"], f32)[0],
    }
    in_maps = []
    for c in range(NCORES):
        xs = np.zeros((NSLAB * SLAB, D), f32)
        ps_ = np.zeros((NSLAB * SLAB,), np.int32)
        vm = np.zeros((128, 8), f32)
        for j in range(NSLAB):
            g = c - (NSLAB - 1) + j
            if g >= 0:
                xs[j * SLAB:(j + 1) * SLAB] = x[g * SLAB:(g + 1) * SLAB]
                ps_[j * SLAB:(j + 1) * SLAB] = pos[g * SLAB:(g + 1) * SLAB]
                vm[:, j] = 1.0
        pos_pm = np.ascontiguousarray(ps_.reshape(64, 128).T)
        m = dict(shared)
        m.update(xs=xs, pos_pm=pos_pm, vmask=vm)
        in_maps.append(m)
    return in_maps


def kernel(**inputs):
    if "nc" not in _CACHE:
        _CACHE["nc"] = build_program()
    nc = _CACHE["nc"]
    in_maps = _prep_inputs(inputs)
    res = run_bass_kernel_spmd(nc, in_maps, core_ids=list(range(NCORES)))
    out = np.concatenate([np.asarray(r["out"], np.float32) for r in res.results], axis=0)
    return out.reshape(1, NCORES * SLAB, D)
```

```python
import contextlib
from contextlib import ExitStack
import numpy as np
import concourse.bass as bass
import concourse.mybir as mybir
from concourse.bass_utils import run_bass_kernel_spmd

F32 = mybir.dt.float32
BF16 = mybir.dt.bfloat16
I32 = mybir.dt.int32
ALU = mybir.AluOpType
AF = mybir.ActivationFunctionType
AX = mybir.AxisListType

NCORES = 8
D = 4096
KC = 32
SLAB = 1024
NT = 8
NSLAB = 8
NEXP = 32
DE = 1024

C_INVF = 0
C_DEC = 128
C_XI = C_DEC + 8 * 128
C_ZETA = C_XI + 8
C_CD = C_ZETA + 8
C_WGT = C_CD + 8
C_ID = C_WGT + 8 * 56
NCST = C_ID + 128
P_CW = 0
P_CB = 64
P_BA = 80
P_BI = 96
P_LAM = 112
P_LG = 128
NPP = 144


class Buf:
    __slots__ = ("name", "writer", "readers")

    def __init__(self, name=""):
        self.name = name
        self.writer = None
        self.readers = []


class _Eng:
    def __init__(self, name, eng, sem):
        self.name = name
        self.eng = eng
        self.sem = sem
        self.cnt = 0
        self.seen = {}


class Sched:
    def __init__(self, nc, stack, n_dma_sems=32):
        self.nc = nc
        self.E = {}
        for name, eng in (("pe", nc.tensor), ("act", nc.scalar), ("dve", nc.vector),
                          ("pool", nc.gpsimd), ("sp", nc.sync)):
            sem = stack.enter_context(nc.semaphore("s_" + name))
            self.E[name] = _Eng(name, eng, sem)
        self.dsems = []
        for i in range(n_dma_sems):
            sem = stack.enter_context(nc.semaphore("s_dma%d" % i))
            self.dsems.append([sem, 0])
        self.drr = 0

    def buf(self, name=""):
        return Buf(name)

    def bufs(self, n, name=""):
        return [Buf(name + str(i)) for i in range(n)]

    def _wait(self, E, deps):
        need = {}
        for (sem, val, ename) in deps:
            if ename == E.name and ename == "pe":
                continue
            k = id(sem)
            if E.seen.get(k, 0) >= val:
                continue
            if k not in need or need[k][1] < val:
                need[k] = (sem, val)
        for k, (sem, val) in need.items():
            E.eng.wait_ge(sem, val)
            E.seen[k] = val

    @staticmethod
    def _deps(reads, writes):
        deps = []
        for b in reads:
            if b.writer is not None:
                deps.append(b.writer)
        for b in writes:
            if b.writer is not None:
                deps.append(b.writer)
            deps.extend(b.readers)
        return deps

    @staticmethod
    def _commit(tok, reads, writes):
        for b in writes:
            b.writer = tok
            b.readers = []
        for b in reads:
            b.readers.append(tok)

    def op(self, engname, fn, reads=(), writes=()):
        E = self.E[engname]
        self._wait(E, self._deps(reads, writes))
        ins = fn(E.eng)
        E.cnt += 1
        ins.then_inc(E.sem, 1)
        self._commit((E.sem, E.cnt, engname), reads, writes)
        return ins

    def dma(self, qname, out, in_, reads=(), writes=(), **kw):
        E = self.E[qname]
        slot = self.dsems[self.drr]
        self.drr = (self.drr + 1) % len(self.dsems)
        deps = self._deps(reads, writes)
        if slot[1] > 0:
            deps.append((slot[0], slot[1], "dma"))
        self._wait(E, deps)
        ins = E.eng.dma_start(out=out, in_=in_, **kw)
        slot[1] += 16
        ins.then_inc(slot[0], 16)
        self._commit((slot[0], slot[1], "dma"), reads, writes)
        return ins

    def barrier(self):
        toks = [(E.sem, E.cnt, n) for n, E in self.E.items() if E.cnt > 0]
        toks += [(d[0], d[1], "dma") for d in self.dsems if d[1] > 0]
        for E in self.E.values():
            self._wait(E, [t for t in toks if t[2] != E.name])

    def finish(self, bufs):
        deps = []
        for b in bufs:
            if b.writer is not None:
                deps.append(b.writer)
            deps.extend(b.readers)
        self._wait(self.E["sp"], deps)


def make_consts():
    lg = np.log1p(-np.exp2(-5.0 - np.arange(8, dtype=np.float64)))
    cst = np.zeros((128, NCST), np.float64)
    invf = np.float32(10000.0) ** (-(np.arange(0, 256, 2, dtype=np.float32)) / np.float32(256.0))
    cst[:, C_INVF:C_INVF + 128] = invf[None, :].astype(np.float64)
    idx = np.arange(128, dtype=np.float64)
    for h in range(8):
        diff = idx[None, :] - idx[:, None]
        dec = np.where(diff >= 0, np.exp(lg[h] * np.maximum(diff, 0.0)), 0.0) / 16.0
        cst[:, C_DEC + h * 128:C_DEC + (h + 1) * 128] = dec
        cst[:, C_XI + h] = np.exp(lg[h] * (idx + 1.0))
        cst[:, C_ZETA + h] = np.exp(lg[h] * (127.0 - idx)) / 16.0
        cst[:, C_CD + h] = np.exp(lg[h] * 128.0)
        for j in range(56):
            cst[:, C_WGT + h * 56 + j] = np.exp(lg[h] * (7167.0 - (128.0 * j + idx))) / 16.0
    cst[:, C_ID:C_ID + 128] = np.eye(128)
    return cst.astype(np.float32)


def build_program(stop_after=None, skipA=False):
    nc = bass.Bass("TRN2", target_bir_lowering=False)

    def din(name, shape, dt=F32):
        return nc.dram_tensor(name, list(shape), dt, kind="ExternalInput").ap()

    xs = din("xs", [NSLAB * SLAB, D])
    w_outs = None
    pos_pm = din("pos_pm", [128, 64], I32)
    vmask_d = din("vmask", [128, 8])
    mem_d = din("mem", [256, D])
    cst_d = din("cst", [128, NCST])
    pp_d = din("pp", [128, NPP])
    mix_g = din("mix_g", [1, D]); gn_g = din("gn_g", [1, 2048]); xat_g = din("xat_g", [1, D])
    mem_g = din("mem_g", [1, D]); moe_g = din("moe_g", [1, D]); fin_g = din("fin_g", [1, D])
    rb_d = din("rb", [1, 36])
    wr_d = din("wr", [128, KC * 36])
    w_in = din("w_in", [D, 12288] if not skipA else [1, 1])
    w_out = din("w_out", [D, D]); wq = din("wq", [D, D]); wk = din("wk", [D, D])
    wv = din("wv", [D, D]); wo = din("wo", [D, D])
    w_a = din("w_a", [8, 256, 256]); w_i = din("w_i", [8, 256, 256])
    big = stop_after is None
    nexp = NEXP if big else 2
    wg = din("wg", [nexp, D, DE]); wu = din("wu", [nexp, D, DE]); wd = din("wd", [nexp, DE, D])
    out_d = nc.dram_tensor("out", [SLAB, D], F32, kind="ExternalOutput").ap()
    dbg = stop_after is not None
    kind_s = "ExternalOutput" if dbg else "Internal"
    x1_d = nc.dram_tensor("x1_d", [SLAB, D], F32, kind=kind_s).ap()
    x2_d = nc.dram_tensor("x2_d", [SLAB, D], F32, kind=kind_s).ap()
    x3_d = nc.dram_tensor("x3_d", [SLAB, D], F32, kind="Internal").ap()
    mixT_d = nc.dram_tensor("mixT_d", [D, SLAB], BF16, kind="Internal").ap()
    yT_d = nc.dram_tensor("yT_d", [2048, SLAB], F32, kind="Internal").ap()

    with ExitStack() as st:
        S = Sched(nc, st)

        def sb(name, shape, dt, stack=st):
            return stack.enter_context(nc.sbuf_tensor("sb_" + name, list(shape), dt))

        def ps(name, shape, dt):
            return st.enter_context(nc.psum_tensor("ps_" + name, list(shape), dt))

        cst = sb("cst", [128, NCST], F32); b_cst = S.buf()
        pp = sb("pp", [128, NPP], F32); b_pp = S.buf()
        vmask = sb("vmaskt", [128, 8], F32); b_vm = S.buf()
        posi = sb("posi", [128, 64], I32); posf = sb("posf", [128, 64], F32); b_pos = S.buf()
        identb = sb("identb", [128, 128], BF16); b_idb = S.buf()
        onesf = sb("onesf", [128, 128], F32); b_ones = S.buf()
        AT = sb("AT", [128, KC, SLAB], BF16); b_AT = S.buf()
        NQ = 3
        ring = sb("ring", [128, NQ * 8192], BF16); b_ring = S.bufs(NQ)
        ring_i = [0]
        pmm = ps("pmm", [128, 4, 512], F32); b_pmm = S.bufs(4); pmm_i = [0]
        ptr = ps("ptr", [128, 1024], BF16); b_ptr = S.buf()
        ptf = ps("ptf", [128, 512], F32); b_ptf = S.buf()
        pms = ps("pms", [128, 2, 512], F32); b_pms = S.bufs(4)
        sdesc = sb("sdesc", [128, 16], F32); b_sd = S.buf()

        S.dma("sp", cst[:], cst_d, writes=[b_cst])
        S.dma("sp", pp[:], pp_d, writes=[b_pp])
        S.dma("sp", vmask[:], vmask_d, writes=[b_vm])
        S.dma("sp", posi[:], pos_pm, writes=[b_pos])
        S.op("dve", lambda e: e.tensor_copy(out=posf[:], in_=posi[:]), reads=[b_pos], writes=[b_pos])
        S.op("dve", lambda e: e.tensor_copy(out=identb[:], in_=cst[:, C_ID:C_ID + 128]), reads=[b_cst], writes=[b_idb])
        S.op("dve", lambda e: e.memset(onesf[:], 1.0), writes=[b_ones])
        identf = cst[:, C_ID:C_ID + 128]

        def next_pmm():
            i = pmm_i[0]
            pmm_i[0] = (i + 1) % 4
            return i

        def load_wslab(W2d, kc, ncols):
            assert kc * ncols == 8192
            q = ring_i[0]
            ring_i[0] = (q + 1) % NQ
            view = ring[:, q * 8192:(q + 1) * 8192].rearrange("p (k n) -> p k n", k=kc)
            S.dma("pool", view, W2d.rearrange("(k p) n -> p k n", p=128), writes=[b_ring[q]])
            return view, b_ring[q]

        def run_jobs(jobs, depth=2):
            handles = {}
            n = len(jobs)
            for i in range(min(depth, n)):
                handles[i] = jobs[i][0]()
            for i in range(n):
                jobs[i][1](handles.pop(i))
                if i + depth < n:
                    handles[i + depth] = jobs[i + depth][0]()

        def mm_tok(w, bw, A, bA, ntiles, kc, evac, ncols=256):
            for t in range(ntiles):
                pb = next_pmm()
                pv = pmm[:, pb, 0:ncols]
                for k in range(kc):
                    S.op("pe", lambda e, k=k, t=t, pv=pv: e.matmul(pv, lhsT=A[:, k, t * 128:(t + 1) * 128], rhs=w[:, k, 0:ncols],
                                                                  start=(k == 0), stop=(k == kc - 1)),
                         reads=[bA, bw], writes=[b_pmm[pb]])
                evac(t, pv, b_pmm[pb])

        def mm_feat(w, bw, A, bA, ntok, kc, evac, ncols=256):
            for cc in range(ncols // 128):
                for hf in range(max(1, ntok // 512)):
                    n = min(512, ntok)
                    pb = next_pmm()
                    pv = pmm[:, pb, 0:n]
                    for k in range(kc):
                        S.op("pe", lambda e, k=k, cc=cc, hf=hf, pv=pv, n=n: e.matmul(
                            pv, lhsT=w[:, k, cc * 128:(cc + 1) * 128], rhs=A[:, k, hf * 512:hf * 512 + n],
                            start=(k == 0), stop=(k == kc - 1)),
                             reads=[bA, bw], writes=[b_pmm[pb]])
                    evac(cc, hf, pv, b_pmm[pb])

        def rms_rstd(ss_ap, n, eps, out_ap, b):
            S.op("dve", lambda e: e.tensor_scalar(out=out_ap, in0=ss_ap, scalar1=1.0 / n, scalar2=eps,
                                                  op0=ALU.mult, op1=ALU.add), reads=[b], writes=[b])
            S.op("act", lambda e: e.activation(out=out_ap, in_=out_ap, func=AF.Sqrt), reads=[b], writes=[b])
            S.op("dve", lambda e: e.reciprocal(out=out_ap, in_=out_ap), reads=[b], writes=[b])

        tr_cnt = [0]

        def norm_transpose(ph, src_rows, ntiles, gvec, AT_dst, b_dst, col0, f32_out=None):
            with ExitStack() as ls:
                gbc = sb(ph + "gbc", [128, D], F32, ls); b_g = S.buf()
                xt = sb(ph + "xt", [128, D], F32, ls); b_xt = S.buf()
                hb = sb(ph + "hb", [128, D // 2], BF16, ls); b_hb = S.buf()
                ss = sb(ph + "ss", [128, 4], F32, ls); b_ss = S.buf()
                S.dma("sp", gbc[:], gvec.partition_broadcast(128), writes=[b_g])
                HD = D // 2
                for t in range(ntiles):
                    S.dma("sp", xt[:], src_rows[t * 128:(t + 1) * 128, :], writes=[b_xt])
                    for hh in range(2):
                        S.op("act", lambda e, hh=hh: e.activation(out=hb[:], in_=xt[:, hh * HD:(hh + 1) * HD], func=AF.Square), reads=[b_xt], writes=[b_hb])
                        S.op("dve", lambda e, hh=hh: e.reduce_sum(out=ss[:, 2 + hh:3 + hh], in_=hb[:], axis=AX.X), reads=[b_hb], writes=[b_ss])
                    S.op("dve", lambda e: e.tensor_tensor(out=ss[:, 0:1], in0=ss[:, 2:3], in1=ss[:, 3:4], op=ALU.add), reads=[b_ss], writes=[b_ss])
                    rms_rstd(ss[:, 0:1], float(D), 1e-6, ss[:, 1:2], b_ss)
                    for hh in range(2):
                        S.op("dve", lambda e, hh=hh: e.scalar_tensor_tensor(out=hb[:], in0=xt[:, hh * HD:(hh + 1) * HD], scalar=ss[:, 1:2],
                                                                            in1=gbc[:, hh * HD:(hh + 1) * HD], op0=ALU.mult, op1=ALU.mult),
                             reads=[b_xt, b_ss, b_g], writes=[b_hb])
                        for g8 in range(2):
                            for j in range(8):
                                k = g8 * 8 + j
                                S.op("pe", lambda e, k=k, j=j: e.transpose(out=ptr[:, j * 128:(j + 1) * 128],
                                                                           in_=hb[:, k * 128:(k + 1) * 128], identity=identb[:]),
                                     reads=[b_hb, b_idb], writes=[b_ptr])
                            eng = "act" if (tr_cnt[0] % 2 == 0) else "dve"
                            tr_cnt[0] += 1
                            k0 = hh * 16 + g8 * 8
                            dst = AT_dst[:, k0:k0 + 8, col0 + t * 128:col0 + (t + 1) * 128]
                            src = ptr[:, :].rearrange("p (k n) -> p k n", k=8)
                            if eng == "act":
                                S.op("act", lambda e, dst=dst, src=src: e.activation(out=dst, in_=src, func=AF.Copy),
                                     reads=[b_ptr], writes=[b_dst])
                            else:
                                S.op("dve", lambda e, dst=dst, src=src: e.tensor_copy(out=dst, in_=src),
                                     reads=[b_ptr], writes=[b_dst])
                S.barrier()

        with ExitStack() as pa:
          if skipA:
            b_mixT = S.bufs(32, "mixT")
          else:
              Racc = sb("Racc", [128, 16, 256], F32, pa); b_R = S.bufs(16)
              for i in range(16):
                  S.op("dve", lambda e, i=i: e.memset(Racc[:, i, :], 0.0), writes=[b_R[i]])
              lstate = sb("lstate", [128, 16], F32, pa); b_ls = S.bufs(16)
              halo = sb("halo", [128, 16, 4], F32, pa); b_halo = S.bufs(16)
              S.op("dve", lambda e: e.memset(lstate[:], 0.0), writes=b_ls)
              S.op("dve", lambda e: e.memset(halo[:], 0.0), writes=b_halo)
              wab = wib = b_wab = None
              lsc = sb("lsc", [128, 48], F32, pa); b_lsc = S.buf()
              S.op("act", lambda e: e.activation(out=lsc[:, 0:16], in_=pp[:, P_LAM:P_LAM + 16], func=AF.Exp, scale=-1.0),
                   reads=[b_pp], writes=[b_lsc])
              S.op("act", lambda e: e.activation(out=lsc[:, 0:16], in_=lsc[:, 0:16], func=AF.Ln, bias=1.0),
                   reads=[b_lsc], writes=[b_lsc])
              S.op("dve", lambda e: e.tensor_scalar(out=lsc[:, 16:32], in0=lsc[:, 0:16], scalar1=-8.0, scalar2=None, op0=ALU.mult),
                   reads=[b_lsc], writes=[b_lsc])
              S.op("dve", lambda e: e.tensor_scalar(out=lsc[:, 32:48], in0=lsc[:, 0:16], scalar1=-16.0, scalar2=None, op0=ALU.mult),
                   reads=[b_lsc], writes=[b_lsc])
              cosT = sinT = b_cs = rtmp = b_rt = rki = kt = b_kt = vt = b_vt = rpt = b_rpt = None
              ret_n = [0]

              def alloc_ret(stack):
                  nonlocal cosT, sinT, b_cs, rtmp, b_rt, rki, kt, b_kt, vt, b_vt, rpt, b_rpt
                  n = "%d" % ret_n[0]; ret_n[0] += 1
                  cosT = sb("cosT" + n, [128, NT, 128], F32, stack); sinT = sb("sinT" + n, [128, NT, 128], F32, stack); b_cs = S.buf()
                  rtmp = sb("rtmp" + n, [128, 3, 128], F32, stack); b_rt = S.buf()
                  rki = sb("rki" + n, [128, 128], I32, stack)
                  kt = sb("kt" + n, [128, NT, 256], BF16, stack); b_kt = S.buf()
                  vt = sb("vt" + n, [128, NT, 256], BF16, stack); b_vt = S.buf()
                  rpt = sb("rpt" + n, [128, 2, 256], F32, stack); b_rpt = S.buf()
              LB = {}
              lru_n = [0]

              def alloc_lru(stack, nset=1):
                  nonlocal wab, wib, b_wab
                  LB["nset"] = nset
                  n = lru_n[0]; lru_n[0] += 1
                  wab = sb("wab%d" % n, [128, 8, 2, 256], BF16, stack); wib = sb("wib%d" % n, [128, 8, 2, 256], BF16, stack); b_wab = S.buf()
                  S.dma("pool", wab[:], w_a.rearrange("b (c p) d -> p b c d", p=128), writes=[b_wab])
                  S.dma("pool", wib[:], w_i.rearrange("b (c p) d -> p b c d", p=128), writes=[b_wab])
                  LB["xb"] = sb("xb%d" % n, [128, 2 * nset, 1028], F32, stack); LB["b_xb"] = S.bufs(2 * nset)
                  LB["xc"] = sb("xc%d" % n, [128, 2, 1024], F32, stack); LB["b_xc"] = S.bufs(2)
                  LB["xcb"] = sb("xcb%d" % n, [128, 2, 1024], BF16, stack); LB["b_xcb"] = S.bufs(2)
                  for nm in ("lr", "li", "l2", "lh"):
                      LB[nm] = sb(nm + "%d" % n, [128, 1024], F32, stack); LB["b_" + nm] = S.buf()

              def rope_tables(j):
                  for t in range(NT):
                      pcol = posf[:, j * NT + t:j * NT + t + 1]
                      ang = rtmp[:, 0, :]; y = rtmp[:, 1, :]; kf = rtmp[:, 2, :]
                      S.op("dve", lambda e, pcol=pcol: e.tensor_scalar(out=ang, in0=cst[:, C_INVF:C_INVF + 128], scalar1=pcol,
                                                                       scalar2=None, op0=ALU.mult), reads=[b_cst, b_pos], writes=[b_rt])
                      S.op("dve", lambda e: e.tensor_scalar(out=y, in0=ang, scalar1=float(1.0 / (2 * np.pi)), scalar2=0.5,
                                                            op0=ALU.mult, op1=ALU.add), reads=[b_rt], writes=[b_rt])
                      S.op("dve", lambda e: e.tensor_copy(out=rki[:], in_=y), reads=[b_rt], writes=[b_rt])
                      S.op("dve", lambda e: e.tensor_copy(out=kf, in_=rki[:]), reads=[b_rt], writes=[b_rt])
                      S.op("dve", lambda e: e.scalar_tensor_tensor(out=y, in0=kf, scalar=-6.28125, in1=ang, op0=ALU.mult, op1=ALU.add),
                           reads=[b_rt], writes=[b_rt])
                      S.op("dve", lambda e: e.scalar_tensor_tensor(out=y, in0=kf, scalar=-0.0019353071795864769, in1=y,
                                                                   op0=ALU.mult, op1=ALU.add), reads=[b_rt], writes=[b_rt])
                      S.op("dve", lambda e: e.tensor_scalar(out=kf, in0=y, scalar1=-float(np.pi), scalar2=float(2 * np.pi),
                                                            op0=ALU.is_lt, op1=ALU.mult), reads=[b_rt], writes=[b_rt])
                      S.op("dve", lambda e: e.tensor_tensor(out=y, in0=y, in1=kf, op=ALU.add), reads=[b_rt], writes=[b_rt])
                      S.op("dve", lambda e: e.tensor_scalar(out=kf, in0=y, scalar1=float(np.pi), scalar2=-float(2 * np.pi),
                                                            op0=ALU.is_gt, op1=ALU.mult), reads=[b_rt], writes=[b_rt])
                      S.op("dve", lambda e: e.tensor_tensor(out=y, in0=y, in1=kf, op=ALU.add), reads=[b_rt], writes=[b_rt])
                      S.op("act", lambda e, t=t: e.activation(out=sinT[:, t, :], in_=y, func=AF.Sin), reads=[b_rt], writes=[b_cs])
                      S.op("act", lambda e: e.activation(out=kf, in_=y, func=AF.Abs), reads=[b_rt], writes=[b_rt])
                      S.op("act", lambda e, t=t: e.activation(out=cosT[:, t, :], in_=kf, func=AF.Sin, scale=-1.0, bias=float(np.pi / 2)),
                           reads=[b_rt], writes=[b_cs])

              def rope_evac(dst, bdst):
                  def f(t, pv, bp):
                      t1 = pv[:, 0:128]; t2 = pv[:, 128:256]
                      a = rpt[:, 0, 0:128]; b = rpt[:, 0, 128:256]; c = rpt[:, 1, 0:128]; d = rpt[:, 1, 128:256]
                      S.op("dve", lambda e: e.tensor_tensor(out=a, in0=t1, in1=cosT[:, t, :], op=ALU.mult), reads=[bp, b_cs], writes=[b_rpt])
                      S.op("dve", lambda e: e.tensor_tensor(out=b, in0=t2, in1=sinT[:, t, :], op=ALU.mult), reads=[bp, b_cs], writes=[b_rpt])
                      S.op("dve", lambda e: e.tensor_tensor(out=c, in0=t1, in1=sinT[:, t, :], op=ALU.mult), reads=[bp, b_cs], writes=[b_rpt])
                      S.op("dve", lambda e: e.tensor_tensor(out=d, in0=t2, in1=cosT[:, t, :], op=ALU.mult), reads=[bp, b_cs], writes=[b_rpt])
                      S.op("dve", lambda e: e.tensor_tensor(out=dst[:, t, 0:128], in0=a, in1=b, op=ALU.subtract), reads=[b_rpt], writes=[bdst])
                      S.op("dve", lambda e: e.tensor_tensor(out=dst[:, t, 128:256], in0=c, in1=d, op=ALU.add), reads=[b_rpt], writes=[bdst])
                  return f

              def copy_evac(dst, bdst, eng="act"):
                  def f(t, pv, bp):
                      if eng == "act":
                          S.op("act", lambda e: e.activation(out=dst[:, t, :], in_=pv, func=AF.Copy), reads=[bp], writes=[bdst])
                      else:
                          S.op("dve", lambda e: e.tensor_copy(out=dst[:, t, :], in_=pv), reads=[bp], writes=[bdst])
                  return f

              def lru_block(j, blk, own, gate_handles=None):
                  xb, xc, xcb, lr, li, l2, lh = (LB[n_] for n_ in ("xb", "xc", "xcb", "lr", "li", "l2", "lh"))
                  b_xb, b_xc, b_xcb, b_lr, b_li, b_l2, b_lh = (LB["b_" + n_] for n_ in ("xb", "xc", "xcb", "lr", "li", "l2", "lh"))
                  xbase = 2 * (blk % LB["nset"])
                  for cc in range(2):
                      ch = blk * 2 + cc
                      S.op("dve", lambda e, cc=cc, ch=ch: e.tensor_copy(out=xb[:, xbase + cc, 0:4], in_=halo[:, ch, :]),
                           reads=[b_halo[ch]], writes=[b_xb[xbase + cc]])
                      cw = lambda jj, ch=ch: pp[:, P_CW + ch * 4 + jj:P_CW + ch * 4 + jj + 1]
                      S.op("dve", lambda e, cc=cc, ch=ch: e.tensor_scalar(out=xc[:, cc, :], in0=xb[:, xbase + cc, 4:1028], scalar1=cw(3),
                                                                          scalar2=pp[:, P_CB + ch:P_CB + ch + 1], op0=ALU.mult, op1=ALU.add),
                           reads=[b_xb[xbase + cc], b_pp], writes=[b_xc[cc]])
                      for jj in range(3):
                          S.op("dve", lambda e, cc=cc, jj=jj: e.scalar_tensor_tensor(out=xc[:, cc, :], in0=xb[:, xbase + cc, 1 + jj:1025 + jj],
                                                                                     scalar=cw(jj), in1=xc[:, cc, :], op0=ALU.mult, op1=ALU.add),
                               reads=[b_xb[xbase + cc], b_pp, b_xc[cc]], writes=[b_xc[cc]])
                      S.op("dve", lambda e, cc=cc, ch=ch: e.tensor_copy(out=halo[:, ch, :], in_=xb[:, xbase + cc, 1024:1028]),
                           reads=[b_xb[xbase + cc]], writes=[b_halo[ch]])
                      S.op("act", lambda e, cc=cc: e.activation(out=xcb[:, cc, :], in_=xc[:, cc, :], func=AF.Copy),
                           reads=[b_xc[cc]], writes=[b_xcb[cc]])
                  for dc in range(2):
                      ch = blk * 2 + dc
                      for (wmat, bias_off, dst, bd) in ((wab, P_BA, lr, b_lr), (wib, P_BI, li, b_li)):
                          for hf in range(2):
                              pb = next_pmm(); pv = pmm[:, pb, :]
                              for c2 in range(2):
                                  S.op("pe", lambda e, c2=c2, hf=hf, pv=pv, wmat=wmat, dc=dc: e.matmul(
                                      pv, lhsT=wmat[:, blk, c2, dc * 128:(dc + 1) * 128], rhs=xcb[:, c2, hf * 512:(hf + 1) * 512],
                                      start=(c2 == 0), stop=(c2 == 1)), reads=[b_wab, b_xcb[0], b_xcb[1]], writes=[b_pmm[pb]])
                              S.op("act", lambda e, hf=hf, pv=pv, dst=dst, bias_off=bias_off, ch=ch: e.activation(
                                  out=dst[:, hf * 512:(hf + 1) * 512], in_=pv, func=AF.Sigmoid,
                                  bias=pp[:, bias_off + ch:bias_off + ch + 1]), reads=[b_pmm[pb], b_pp], writes=[bd])
                      S.op("act", lambda e, ch=ch: e.activation(out=l2[:], in_=lr[:], func=AF.Exp, scale=lsc[:, 32 + ch:33 + ch]),
                           reads=[b_lr, b_lsc], writes=[b_l2])
                      S.op("act", lambda e, ch=ch: e.activation(out=lr[:], in_=lr[:], func=AF.Exp, scale=lsc[:, 16 + ch:17 + ch]),
                           reads=[b_lr, b_lsc], writes=[b_lr])
                      S.op("dve", lambda e: e.tensor_scalar(out=l2[:], in0=l2[:], scalar1=-1.0, scalar2=1.0, op0=ALU.mult, op1=ALU.add),
                           reads=[b_l2], writes=[b_l2])
                      S.op("dve", lambda e: e.tensor_scalar(out=l2[:], in0=l2[:], scalar1=0.0, scalar2=None, op0=ALU.max),
                           reads=[b_l2], writes=[b_l2])
                      S.op("act", lambda e: e.activation(out=l2[:], in_=l2[:], func=AF.Sqrt), reads=[b_l2], writes=[b_l2])
                      S.op("dve", lambda e: e.scalar_tensor_tensor(out=li[:], in0=l2[:], scalar=vmask[:, j:j + 1], in1=li[:],
                                                                   op0=ALU.mult, op1=ALU.mult), reads=[b_l2, b_li, b_vm], writes=[b_li])
                      S.op("dve", lambda e, dc=dc: e.tensor_tensor(out=l2[:], in0=li[:], in1=xc[:, dc, :], op=ALU.mult),
                           reads=[b_li, b_xc[dc]], writes=[b_l2])
                      S.op("dve", lambda e, ch=ch: e.tensor_tensor_scan(out=lh[:], data0=lr[:], data1=l2[:], initial=lstate[:, ch:ch + 1],
                                                                        op0=ALU.mult, op1=ALU.add),
                           reads=[b_lr, b_l2, b_ls[ch]], writes=[b_lh])
                      S.op("dve", lambda e, ch=ch: e.tensor_copy(out=lstate[:, ch:ch + 1], in_=lh[:, 1023:1024]),
                           reads=[b_lh], writes=[b_ls[ch]])
                      if own:
                          own_lru_out(ch, dc)

              own_ctx = {}

              def own_lru_out(ch, dc):
                  xb, xc, xcb, lr, li, l2, lh = (LB[n_] for n_ in ("xb", "xc", "xcb", "lr", "li", "l2", "lh"))
                  b_xb, b_xc, b_xcb, b_lr, b_li, b_l2, b_lh = (LB["b_" + n_] for n_ in ("xb", "xc", "xcb", "lr", "li", "l2", "lh"))
                  gt = own_ctx["gt"]; b_gt = own_ctx["b_gt"]; ssum = own_ctx["ssum"]; b_ssum = own_ctx["b_ssum"]
                  g = gt[:, dc, :]
                  u = li[:]
                  S.op("dve", lambda e: e.tensor_tensor(out=u, in0=g, in1=g, op=ALU.mult), reads=[b_gt[dc]], writes=[b_li])
                  S.op("dve", lambda e: e.tensor_scalar(out=u, in0=u, scalar1=0.044715, scalar2=1.0, op0=ALU.mult, op1=ALU.add),
                       reads=[b_li], writes=[b_li])
                  S.op("dve", lambda e: e.tensor_tensor(out=u, in0=u, in1=g, op=ALU.mult), reads=[b_li, b_gt[dc]], writes=[b_li])
                  S.op("act", lambda e: e.activation(out=u, in_=u, func=AF.Sigmoid, scale=1.5957691216057308), reads=[b_li], writes=[b_li])
                  S.op("dve", lambda e: e.tensor_tensor(out=u, in0=u, in1=g, op=ALU.mult), reads=[b_li, b_gt[dc]], writes=[b_li])
                  S.op("dve", lambda e: e.tensor_tensor(out=lh[:], in0=lh[:], in1=u, op=ALU.mult), reads=[b_li, b_lh], writes=[b_lh])
                  S.dma("sp", yT_d[ch * 128:(ch + 1) * 128, :], lh[:], reads=[b_lh], writes=[own_ctx["b_yT"][ch]])
                  S.op("act", lambda e: e.activation(out=l2[:], in_=lh[:], func=AF.Square), reads=[b_lh], writes=[b_l2])
                  for hf in range(2):
                      S.op("pe", lambda e, hf=hf: e.matmul(ptf[:, :], lhsT=onesf[:], rhs=l2[:, hf * 512:(hf + 1) * 512], start=True, stop=True),
                           reads=[b_ones, b_l2], writes=[b_ptf])
                      S.op("dve", lambda e, hf=hf: e.tensor_tensor(out=ssum[:, hf * 512:(hf + 1) * 512], in0=ssum[:, hf * 512:(hf + 1) * 512],
                                                                   in1=ptf[:, :], op=ALU.add), reads=[b_ptf, b_ssum], writes=[b_ssum])

              def xb_evac(cc_base):
                  def f(cc, hf, pv, bp):
                      xb = LB["xb"]; b_xb = LB["b_xb"]
                      S.op("act", lambda e: e.activation(out=xb[:, cc_base + cc, 4 + hf * 512:4 + (hf + 1) * 512], in_=pv, func=AF.Copy),
                           reads=[bp], writes=[b_xb[cc_base + cc]])
                  return f

              for j in range(NSLAB - 1):
                  norm_transpose("pa%d" % j, xs[j * SLAB:(j + 1) * SLAB, :], NT, mix_g, AT, b_AT, 0)
                  rsc_ = ExitStack()
                  alloc_ret(rsc_)
                  rope_tables(j)
                  jobs = []
                  for h in range(8):
                      def mk(h):
                          def ld_k():
                              return load_wslab(w_in[:, 2048 + h * 256:2048 + (h + 1) * 256], KC, 256)

                          def cp_k(hd):
                              mm_tok(hd[0], hd[1], AT, b_AT, NT, KC, rope_evac(kt, b_kt))
                              for t in range(NT):
                                  S.op("dve", lambda e, t=t: e.tensor_scalar(out=kt[:, t, :], in0=kt[:, t, :],
                                                                             scalar1=cst[:, C_WGT + h * 56 + j * NT + t:C_WGT + h * 56 + j * NT + t + 1],
                                                                             scalar2=None, op0=ALU.mult), reads=[b_kt, b_cst], writes=[b_kt])

                          def ld_v():
                              return load_wslab(w_in[:, 4096 + h * 256:4096 + (h + 1) * 256], KC, 256)

                          def cp_v(hd):
                              mm_tok(hd[0], hd[1], AT, b_AT, NT, KC, copy_evac(vt, b_vt))
                              for dc in range(2):
                                  r = dc
                                  pv = pms[:, r, 0:256]
                                  for t in range(NT):
                                      S.op("pe", lambda e, t=t, dc=dc, pv=pv: e.matmul(pv, lhsT=kt[:, t, dc * 128:(dc + 1) * 128], rhs=vt[:, t, :],
                                                                                      start=(t == 0), stop=(t == NT - 1)),
                                           reads=[b_kt, b_vt], writes=[b_pms[r]])
                                  S.op("dve", lambda e, dc=dc, pv=pv: e.tensor_tensor(out=Racc[:, h * 2 + dc, :], in0=Racc[:, h * 2 + dc, :], in1=pv, op=ALU.add),
                                       reads=[b_pms[r]], writes=[b_R[h * 2 + dc]])
                          return [(ld_k, cp_k), (ld_v, cp_v)]
                      jobs += mk(h)
                  run_jobs(jobs)
                  S.barrier()
                  rsc_.close()
                  jobs = []
                  lsc_ = ExitStack()
                  alloc_lru(lsc_, 2)
                  for blk in range(8):
                      def mkl(blk):
                          def ld():
                              return load_wslab(w_in[:, 8192 + blk * 256:8192 + (blk + 1) * 256], KC, 256)

                          def cp(hd):
                              mm_feat(hd[0], hd[1], AT, b_AT, SLAB, KC, xb_evac(2 * (blk % 2)))
                              lru_block(j, blk, False)
                          return [(ld, cp)]
                      jobs += mkl(blk)
                  run_jobs(jobs)
                  S.barrier()
                  lsc_.close()

              j = NSLAB - 1
              norm_transpose("pa7", xs[j * SLAB:(j + 1) * SLAB, :], NT, mix_g, AT, b_AT, 0)
              with ExitStack() as po:
                  alloc_ret(po)
                  rope_tables(j)
                  qt = sb("qt", [128, NT, 256], BF16, po); b_qt = S.buf()
                  sg = sb("sg", [128, NT, 256], F32, po); b_sg = S.buf()
                  qT = sb("qT", [128, 2, SLAB], BF16, po); b_qT = S.buf()
                  kT = sb("kT", [128, 2, SLAB], BF16, po); b_kT = S.buf()
                  Rb = sb("Rb", [128, 2, 256], BF16, po); b_Rb = S.buf()
                  PT = sb("PT", [128, 128], BF16, po); b_PT = S.buf()
                  oc = sb("oc", [128, 256], F32, po); b_oc = S.buf()
                  osb = sb("osb", [128, 256], F32, po); b_osb = S.buf()
                  rtb = sb("rtb", [128, 256], BF16, po); b_rtb = S.buf()
                  rTs = sb("rTs", [128, 2, 128], BF16, po); b_rTs = S.buf()
                  kz = sb("kz", [128, 256], BF16, po); b_kz = S.buf()
                  gng = sb("gng", [128, 2048], F32, po); b_gng = S.buf()
                  st6 = sb("st6", [128, 16], F32, po); b_st6 = S.buf()
                  b_mixT = S.bufs(32, "mixT")
                  S.dma("sp", gng[:], gn_g.partition_broadcast(128), writes=[b_gng])

                  def retention_head(h):
                      for (src, bs, dstT, bdT) in ((qt, b_qt, qT, b_qT), (kt, b_kt, kT, b_kT)):
                          for t in range(NT):
                              for dc in range(2):
                                  S.op("pe", lambda e, t=t, dc=dc, src=src: e.transpose(out=ptr[:, dc * 128:(dc + 1) * 128],
                                                                                      in_=src[:, t, dc * 128:(dc + 1) * 128], identity=identb[:]),
                                       reads=[bs, b_idb], writes=[b_ptr])
                              S.op("act", lambda e, t=t, dstT=dstT: e.activation(out=dstT[:, :, t * 128:(t + 1) * 128],
                                                                                 in_=ptr[:, 0:256].rearrange("p (k n) -> p k n", k=2), func=AF.Copy),
                                   reads=[b_ptr], writes=[bdT])
                      for dc in range(2):
                          S.op("act", lambda e, dc=dc: e.activation(out=Rb[:, dc, :], in_=Racc[:, h * 2 + dc, :], func=AF.Copy),
                               reads=[b_R[h * 2 + dc]], writes=[b_Rb])
                      for i in range(NT):
                          cs = slice(i * 128, (i + 1) * 128)
                          pST = pms[:, 0, 0:128]
                          for dc in range(2):
                              S.op("pe", lambda e, dc=dc: e.matmul(pST, lhsT=kT[:, dc, cs], rhs=qT[:, dc, cs], start=(dc == 0), stop=(dc == 1)),
                                   reads=[b_kT, b_qT], writes=[b_pms[0]])
                          S.op("dve", lambda e: e.tensor_tensor(out=PT[:], in0=pST, in1=cst[:, C_DEC + h * 128:C_DEC + (h + 1) * 128], op=ALU.mult),
                               reads=[b_pms[0], b_cst], writes=[b_PT])
                          pOI = pms[:, 0, 256:512]
                          S.op("pe", lambda e: e.matmul(pOI, lhsT=PT[:], rhs=vt[:, i, :], start=True, stop=True),
                               reads=[b_PT, b_vt], writes=[b_pms[1]])
                          pOC = pms[:, 1, 0:256]
                          for dc in range(2):
                              S.op("pe", lambda e, dc=dc: e.matmul(pOC, lhsT=qT[:, dc, cs], rhs=Rb[:, dc, :], start=(dc == 0), stop=(dc == 1)),
                                   reads=[b_qT, b_Rb], writes=[b_pms[2]])
                          S.op("act", lambda e: e.activation(out=oc[:], in_=pOC, func=AF.Copy, scale=cst[:, C_XI + h:C_XI + h + 1]),
                               reads=[b_pms[2], b_cst], writes=[b_oc])
                          S.op("dve", lambda e: e.tensor_tensor(out=osb[:], in0=pOI, in1=oc[:], op=ALU.add),
                               reads=[b_pms[1], b_oc], writes=[b_osb])
                          S.op("dve", lambda e: e.bn_stats(out=st6[:, 0:6], in_=osb[:]), reads=[b_osb], writes=[b_st6])
                          S.op("dve", lambda e: e.bn_aggr(out=st6[:, 8:10], in_=st6[:, 0:6]), reads=[b_st6], writes=[b_st6])
                          S.op("dve", lambda e: e.tensor_scalar(out=st6[:, 10:11], in0=st6[:, 9:10], scalar1=1e-5, scalar2=None, op0=ALU.add),
                               reads=[b_st6], writes=[b_st6])
                          S.op("act", lambda e: e.activation(out=st6[:, 10:11], in_=st6[:, 10:11], func=AF.Sqrt), reads=[b_st6], writes=[b_st6])
                          S.op("dve", lambda e: e.reciprocal(out=st6[:, 11:12], in_=st6[:, 10:11]), reads=[b_st6], writes=[b_st6])
                          S.op("dve", lambda e: e.tensor_scalar(out=osb[:], in0=osb[:], scalar1=st6[:, 8:9], scalar2=st6[:, 11:12],
                                                                op0=ALU.subtract, op1=ALU.mult), reads=[b_osb, b_st6], writes=[b_osb])
                          S.op("dve", lambda e: e.tensor_tensor(out=osb[:], in0=osb[:], in1=gng[:, h * 256:(h + 1) * 256], op=ALU.mult),
                               reads=[b_osb, b_gng], writes=[b_osb])
                          S.op("dve", lambda e: e.tensor_tensor(out=rtb[:], in0=osb[:], in1=sg[:, i, :], op=ALU.mult),
                               reads=[b_osb, b_sg], writes=[b_rtb])
                          for dc in range(2):
                              S.op("pe", lambda e, dc=dc: e.transpose(out=ptr[:, 512 + dc * 128:512 + (dc + 1) * 128], in_=rtb[:, dc * 128:(dc + 1) * 128],
                                                                      identity=identb[:]), reads=[b_rtb, b_idb], writes=[b_ptr])
                          S.op("act", lambda e: e.activation(out=rTs[:], in_=ptr[:, 512:768].rearrange("p (k n) -> p k n", k=2), func=AF.Copy),
                               reads=[b_ptr], writes=[b_rTs])
                          for dc in range(2):
                              r0 = h * 256 + dc * 128
                              S.dma("sp", mixT_d[r0:r0 + 128, cs], rTs[:, dc, :], reads=[b_rTs], writes=[b_mixT[h * 2 + dc]])
                          if i < NT - 1:
                              S.op("dve", lambda e: e.tensor_scalar(out=kz[:], in0=kt[:, i, :], scalar1=cst[:, C_ZETA + h:C_ZETA + h + 1],
                                                                    scalar2=None, op0=ALU.mult), reads=[b_kt, b_cst], writes=[b_kz])
                              for dc in range(2):
                                  pRU = pms[:, 1, 256:512]
                                  S.op("pe", lambda e, dc=dc: e.matmul(pRU, lhsT=kz[:, dc * 128:(dc + 1) * 128], rhs=vt[:, i, :], start=True, stop=True),
                                       reads=[b_kz, b_vt], writes=[b_pms[3]])
                                  S.op("dve", lambda e, dc=dc: e.scalar_tensor_tensor(out=Racc[:, h * 2 + dc, :], in0=Racc[:, h * 2 + dc, :],
                                                                                      scalar=cst[:, C_CD + h:C_CD + h + 1], in1=pRU,
                                                                                      op0=ALU.mult, op1=ALU.add),
                                       reads=[b_pms[3], b_cst], writes=[b_R[h * 2 + dc]])
                                  S.op("act", lambda e, dc=dc: e.activation(out=Rb[:, dc, :], in_=Racc[:, h * 2 + dc, :], func=AF.Copy),
                                       reads=[b_R[h * 2 + dc]], writes=[b_Rb])

                  jobs = []
                  for h in range(8):
                      def mko(h):
                          def ld(c0):
                              return lambda: load_wslab(w_in[:, c0 + h * 256:c0 + (h + 1) * 256], KC, 256)

                          def cp_q(hd):
                              mm_tok(hd[0], hd[1], AT, b_AT, NT, KC, rope_evac(qt, b_qt))

                          def cp_k(hd):
                              mm_tok(hd[0], hd[1], AT, b_AT, NT, KC, rope_evac(kt, b_kt))

                          def cp_v(hd):
                              mm_tok(hd[0], hd[1], AT, b_AT, NT, KC, copy_evac(vt, b_vt))

                          def cp_g(hd):
                              def ev(t, pv, bp):
                                  S.op("act", lambda e: e.activation(out=sg[:, t, :], in_=pv, func=AF.Silu), reads=[bp], writes=[b_sg])
                              mm_tok(hd[0], hd[1], AT, b_AT, NT, KC, ev)
                              retention_head(h)
                          return [(ld(0), cp_q), (ld(2048), cp_k), (ld(4096), cp_v), (ld(6144), cp_g)]
                      jobs += mko(h)
                  run_jobs(jobs)
                  S.barrier()

              with ExitStack() as pl:
                  alloc_lru(pl)
                  gt = sb("gt", [128, 2, 1024], F32, pl); b_gt = S.bufs(2)
                  ssum = sb("ssum", [128, 1024], F32, pl); b_ssum = S.buf()
                  b_yT = S.bufs(16, "yT")
                  S.op("dve", lambda e: e.memset(ssum[:], 0.0), writes=[b_ssum])
                  own_ctx.update(gt=gt, b_gt=b_gt, ssum=ssum, b_ssum=b_ssum, b_yT=b_yT)
                  jobs = []
                  for blk in range(8):
                      def mkl2(blk):
                          def ld_x():
                              return load_wslab(w_in[:, 8192 + blk * 256:8192 + (blk + 1) * 256], KC, 256)

                          def cp_x(hd):
                              mm_feat(hd[0], hd[1], AT, b_AT, SLAB, KC, xb_evac(0))

                          def ld_g():
                              return load_wslab(w_in[:, 10240 + blk * 256:10240 + (blk + 1) * 256], KC, 256)

                          def cp_g(hd):
                              def ev(cc, hf, pv, bp):
                                  S.op("act", lambda e: e.activation(out=gt[:, cc, hf * 512:(hf + 1) * 512], in_=pv, func=AF.Copy),
                                       reads=[bp], writes=[b_gt[cc]])
                              mm_feat(hd[0], hd[1], AT, b_AT, SLAB, KC, ev)
                              lru_block(NSLAB - 1, blk, True)
                          return [(ld_x, cp_x), (ld_g, cp_g)]
                      jobs += mkl2(blk)
                  run_jobs(jobs)
                  rms_rstd(ssum[:], 2048.0, 1e-6, ssum[:], b_ssum)
                  lh = LB["lh"]; b_lh = LB["b_lh"]; xcb = LB["xcb"]; b_xcb = LB["b_xcb"]
                  for ch in range(16):
                      S.dma("sp", lh[:], yT_d[ch * 128:(ch + 1) * 128, :], reads=[b_yT[ch]], writes=[b_lh])
                      S.op("dve", lambda e, ch=ch: e.scalar_tensor_tensor(out=xcb[:, 0, :], in0=lh[:], scalar=pp[:, P_LG + ch:P_LG + ch + 1],
                                                                          in1=ssum[:], op0=ALU.mult, op1=ALU.mult),
                           reads=[b_lh, b_pp, b_ssum], writes=[b_xcb[0]])
                      S.dma("sp", mixT_d[2048 + ch * 128:2048 + (ch + 1) * 128, :], xcb[:, 0, :], reads=[b_xcb[0]], writes=[b_mixT[16 + ch]])
                  S.barrier()
              S.barrier()

        b_x1 = [[S.buf() for _ in range(16)] for _ in range(NT)]
        b_x2 = [[S.buf() for _ in range(16)] for _ in range(NT)]

        def load_AT_from(dram_T, deps):
            for k in range(KC):
                S.dma("sp", AT[:, k, :], dram_T[k * 128:(k + 1) * 128, :], reads=[deps[k]], writes=[b_AT])

        def resid_linear(ph, W, src_rows, src_bufs, dst_rows=None, dst_bufs=None):
            dst_rows = x1_d if dst_rows is None else dst_rows
            dst_bufs = b_x1 if dst_bufs is None else dst_bufs
            with ExitStack() as ls:
                rs = sb(ph + "rs", [128, 4, 256], F32, ls); b_rs = S.bufs(4)
                ri = [0]
                jobs = []
                for s in range(16):
                    def mk(s):
                        def ld():
                            return load_wslab(W[:, s * 256:(s + 1) * 256], KC, 256)

                        def cp(hd):
                            pend = {}

                            def issue(t):
                                r = ri[0]; ri[0] = (r + 1) % 4
                                S.dma("sp", rs[:, r, :], src_rows[t * 128:(t + 1) * 128, s * 256:(s + 1) * 256],
                                      reads=[src_bufs[t][s]] if src_bufs else [], writes=[b_rs[r]])
                                pend[t] = r
                            issue(0); issue(1)

                            def ev(t, pv, bp):
                                r = pend.pop(t)
                                if t + 2 < NT:
                                    issue(t + 2)
                                S.op("dve", lambda e: e.tensor_tensor(out=rs[:, r, :], in0=pv, in1=rs[:, r, :], op=ALU.add),
                                     reads=[bp, b_rs[r]], writes=[b_rs[r]])
                                S.dma("sp", dst_rows[t * 128:(t + 1) * 128, s * 256:(s + 1) * 256], rs[:, r, :], reads=[b_rs[r]], writes=[dst_bufs[t][s]])
                            mm_tok(hd[0], hd[1], AT, b_AT, NT, KC, ev)
                        return (ld, cp)
                    jobs.append(mk(s))
                run_jobs(jobs)
                S.barrier()

        load_AT_from(mixT_d, b_mixT)
        resid_linear("wo1", w_out, xs[(NSLAB - 1) * SLAB:NSLAB * SLAB, :], None)

        if stop_after == "A":
            S.finish([b for row in b_x1 for b in row])
            return nc

        with ExitStack() as pb_:
            memT = sb("memT", [128, KC, 256], BF16, pb_); b_memT = S.buf()
            norm_transpose("pm", mem_d, 2, mem_g, memT, b_memT, 0)
            KTm = sb("KTm", [128, KC, 256], BF16, pb_); b_KT = S.buf()
            Vm = sb("Vm", [128, 2, D], BF16, pb_); b_Vm = S.buf()
            jobs = []
            for s in range(16):
                def mkk(s):
                    def ld():
                        return load_wslab(wk[:, s * 256:(s + 1) * 256], KC, 256)

                    def cp(hd):
                        def ev(cc, hf, pv, bp):
                            S.op("act", lambda e: e.activation(out=KTm[:, s * 2 + cc, :], in_=pv, func=AF.Copy), reads=[bp], writes=[b_KT])
                        mm_feat(hd[0], hd[1], memT, b_memT, 256, KC, ev)
                    return (ld, cp)

                def mkv(s):
                    def ld():
                        return load_wslab(wv[:, s * 256:(s + 1) * 256], KC, 256)

                    def cp(hd):
                        def ev(t, pv, bp):
                            S.op("act", lambda e: e.activation(out=Vm[:, t, s * 256:(s + 1) * 256], in_=pv, func=AF.Copy), reads=[bp], writes=[b_Vm])
                        mm_tok(hd[0], hd[1], memT, b_memT, 2, KC, ev)
                    return (ld, cp)
                jobs += [mkk(s), mkv(s)]
            run_jobs(jobs)

            x1_all = [b for row in b_x1 for b in row]
            b_x1tile = S.buf()
            for b in x1_all:
                pass
            sync_tok = S.buf()
            S.op("dve", lambda e: e.memset(sdesc[:, 0:1], 0.0), reads=[], writes=[b_sd])

            def norm_transpose_x1(ph, gvec):
                deps = []
                for b in x1_all:
                    if b.writer is not None:
                        deps.append(b.writer)
                S._wait(S.E["sp"], deps)
                norm_transpose(ph, x1_d, NT, gvec, AT, b_AT, 0)

            if stop_after == "B1":
                S.barrier(); S.finish([]); return nc
            norm_transpose_x1("pb", xat_g)
            if stop_after == "B2":
                S.barrier(); S.finish([]); return nc
            b_oT = S.bufs(32, "oT")
            with ExitStack() as px:
                qx = sb("qx", [128, 8, SLAB], BF16, px); b_qx = S.buf()
                prob = sb("prob", [128, 256], F32, px); b_prob = S.buf()
                probb = sb("probb", [128, 256], BF16, px); b_probb = S.buf()
                pT = sb("pT", [128, 2, SLAB], BF16, px); b_pT = S.buf()
                sm = sb("sm", [128, 8], F32, px); b_sm = S.buf()
                oTs = sb("oTs", [128, 2, 512], BF16, px); b_oTs = S.bufs(2)
                for hx in range(4):
                    jobs = []
                    for s4 in range(4):
                        def mkq(s4):
                            def ld():
                                c0 = hx * 1024 + s4 * 256
                                return load_wslab(wq[:, c0:c0 + 256], KC, 256)

                            def cp(hd):
                                def ev(cc, hf, pv, bp):
                                    S.op("act", lambda e: e.activation(out=qx[:, s4 * 2 + cc, hf * 512:(hf + 1) * 512], in_=pv, func=AF.Copy),
                                         reads=[bp], writes=[b_qx])
                                mm_feat(hd[0], hd[1], AT, b_AT, SLAB, KC, ev)
                            return (ld, cp)
                        jobs.append(mkq(s4))
                    run_jobs(jobs)
                    for t in range(NT):
                        pb = next_pmm(); pv = pmm[:, pb, 0:256]
                        for dc in range(8):
                            S.op("pe", lambda e, dc=dc, pv=pv: e.matmul(pv, lhsT=qx[:, dc, t * 128:(t + 1) * 128], rhs=KTm[:, hx * 8 + dc, :],
                                                                      start=(dc == 0), stop=(dc == 7)), reads=[b_qx, b_KT], writes=[b_pmm[pb]])
                        S.op("dve", lambda e, pv=pv: e.reduce_max(out=sm[:, 0:1], in_=pv, axis=AX.X), reads=[b_pmm[pb]], writes=[b_sm])
                        S.op("dve", lambda e: e.tensor_scalar(out=sm[:, 1:2], in0=sm[:, 0:1], scalar1=-1.0 / 32.0, scalar2=None, op0=ALU.mult),
                             reads=[b_sm], writes=[b_sm])
                        S.op("act", lambda e, pv=pv: e.activation(out=prob[:], in_=pv, func=AF.Exp, scale=1.0 / 32.0, bias=sm[:, 1:2]),
                             reads=[b_pmm[pb], b_sm], writes=[b_prob])
                        S.op("dve", lambda e: e.reduce_sum(out=sm[:, 2:3], in_=prob[:], axis=AX.X), reads=[b_prob], writes=[b_sm])
                        S.op("dve", lambda e: e.reciprocal(out=sm[:, 3:4], in_=sm[:, 2:3]), reads=[b_sm], writes=[b_sm])
                        S.op("dve", lambda e: e.tensor_scalar(out=probb[:], in0=prob[:], scalar1=sm[:, 3:4], scalar2=None, op0=ALU.mult),
                             reads=[b_prob, b_sm], writes=[b_probb])
                        for mc in range(2):
                            S.op("pe", lambda e, mc=mc: e.transpose(out=ptr[:, mc * 128:(mc + 1) * 128], in_=probb[:, mc * 128:(mc + 1) * 128],
                                                                    identity=identb[:]), reads=[b_probb, b_idb], writes=[b_ptr])
                        S.op("act", lambda e, t=t: e.activation(out=pT[:, :, t * 128:(t + 1) * 128],
                                                                in_=ptr[:, 0:256].rearrange("p (k n) -> p k n", k=2), func=AF.Copy),
                             reads=[b_ptr], writes=[b_pT])
                    for dvc in range(8):
                        for hf in range(2):
                            pb = next_pmm(); pv = pmm[:, pb, :]
                            for mc in range(2):
                                S.op("pe", lambda e, mc=mc, pv=pv, hf=hf, dvc=dvc: e.matmul(
                                    pv, lhsT=Vm[:, mc, hx * 1024 + dvc * 128:hx * 1024 + (dvc + 1) * 128], rhs=pT[:, mc, hf * 512:(hf + 1) * 512],
                                    start=(mc == 0), stop=(mc == 1)), reads=[b_Vm, b_pT], writes=[b_pmm[pb]])
                            S.op("act", lambda e, pv=pv, hf=hf: e.activation(out=oTs[:, hf, :], in_=pv, func=AF.Copy), reads=[b_pmm[pb]], writes=[b_oTs[hf]])
                            r0 = hx * 1024 + dvc * 128
                            S.dma("sp", mixT_d[r0:r0 + 128, hf * 512:(hf + 1) * 512], oTs[:, hf, :], reads=[b_oTs[hf], b_AT],
                                  writes=[b_oT[hx * 8 + dvc]])
                S.barrier()
            if stop_after == "B3":
                S.barrier(); S.finish([]); return nc
            load_AT_from(mixT_d, b_oT)
            if stop_after == "B4":
                S.barrier(); S.finish([]); return nc
            resid_linear("wo2", wo, x1_d, b_x1, x2_d, b_x2)
            S.barrier()

        if stop_after == "B":
            S.finish([b for row in b_x2 for b in row])
            return nc

        with ExitStack() as pc:
            Wt = sb("Wt", [128, NT, 32], F32, pc); b_Wt = S.buf()
            wrs = sb("wrs", [128, KC * 36], F32, pc); b_wrs = S.buf()
            whi = sb("whi", [128, KC, 36], BF16, pc); wlo = sb("wlo", [128, KC, 36], BF16, pc); b_whl = S.buf()
            rbb = sb("rbb", [128, 36], F32, pc); b_rbb = S.buf()
            S.dma("sp", wrs[:], wr_d, writes=[b_wrs])
            S.dma("sp", rbb[:], rb_d.partition_broadcast(128), writes=[b_rbb])
            whi2 = whi[:, :, :].rearrange("p k n -> p (k n)"); wlo2 = wlo[:, :, :].rearrange("p k n -> p (k n)")
            S.op("act", lambda e: e.activation(out=whi2, in_=wrs[:], func=AF.Copy), reads=[b_wrs], writes=[b_whl])
            S.op("dve", lambda e: e.tensor_tensor(out=wlo2, in0=wrs[:], in1=whi2, op=ALU.subtract), reads=[b_wrs, b_whl], writes=[b_whl])
            deps = []
            for b in [b for row in b_x2 for b in row]:
                if b.writer is not None:
                    deps.append(b.writer)
            S._wait(S.E["sp"], deps)
            with ExitStack() as pr:
                gbc = sb("c_gbc", [128, D], F32, pr); b_g = S.buf()
                xt = sb("c_xt", [128, D], F32, pr); b_xt = S.buf()
                hit = sb("c_hit", [128, D], BF16, pr); b_hit = S.buf()
                lot = sb("c_lot", [128, D], BF16, pr); b_lot = S.buf()
                loT = sb("c_loT", [128, KC, 128], BF16, pr); b_loT = S.buf()
                ss = sb("c_ss", [128, 2], F32, pr); b_ss = S.buf()
                lg = sb("c_lg", [128, 40], F32, pr); b_lg = S.buf()
                rw = sb("c_rw", [128, 8, 32], F32, pr); b_rw = S.buf()
                S.dma("sp", gbc[:], moe_g.partition_broadcast(128), writes=[b_g])
                for t in range(NT):
                    S.dma("sp", xt[:], x2_d[t * 128:(t + 1) * 128, :], writes=[b_xt])
                    S.op("act", lambda e: e.activation(out=hit[:], in_=xt[:], func=AF.Square), reads=[b_xt], writes=[b_hit])
                    S.op("dve", lambda e: e.reduce_sum(out=ss[:, 0:1], in_=hit[:], axis=AX.X), reads=[b_hit], writes=[b_ss])
                    rms_rstd(ss[:, 0:1], float(D), 1e-6, ss[:, 1:2], b_ss)
                    S.op("dve", lambda e: e.scalar_tensor_tensor(out=xt[:], in0=xt[:], scalar=ss[:, 1:2], in1=gbc[:],
                                                                 op0=ALU.mult, op1=ALU.mult), reads=[b_xt, b_ss, b_g], writes=[b_xt])
                    S.op("act", lambda e: e.activation(out=hit[:], in_=xt[:], func=AF.Copy), reads=[b_xt], writes=[b_hit])
                    S.op("dve", lambda e: e.tensor_tensor(out=lot[:], in0=xt[:], in1=hit[:], op=ALU.subtract), reads=[b_xt, b_hit], writes=[b_lot])
                    for (srcb, bsrc, dstT, bdst, c0) in ((hit, b_hit, AT, b_AT, t * 128), (lot, b_lot, loT, b_loT, 0)):
                        for g8 in range(4):
                            for jx in range(8):
                                k = g8 * 8 + jx
                                S.op("pe", lambda e, k=k, jx=jx, srcb=srcb: e.transpose(out=ptr[:, jx * 128:(jx + 1) * 128],
                                                                                      in_=srcb[:, k * 128:(k + 1) * 128], identity=identb[:]),
                                     reads=[bsrc, b_idb], writes=[b_ptr])
                            src = ptr[:, :].rearrange("p (k n) -> p k n", k=8)
                            S.op("act", lambda e, g8=g8, src=src, dstT=dstT, c0=c0: e.activation(
                                out=dstT[:, g8 * 8:(g8 + 1) * 8, c0:c0 + 128], in_=src, func=AF.Copy), reads=[b_ptr], writes=[bdst])
                    pl_ = pms[:, 0, 0:36]
                    nmm = 0
                    for k in range(KC):
                        for (lh_, bl_, wv_) in ((AT[:, k, t * 128:(t + 1) * 128], b_AT, whi), (AT[:, k, t * 128:(t + 1) * 128], b_AT, wlo),
                                                (loT[:, k, :], b_loT, whi)):
                            S.op("pe", lambda e, k=k, lh_=lh_, wv_=wv_, nmm=nmm: e.matmul(pl_, lhsT=lh_, rhs=wv_[:, k, :],
                                                                                       start=(nmm == 0), stop=(nmm == 3 * KC - 1)),
                                 reads=[bl_, b_whl], writes=[b_pms[0]])
                            nmm += 1
                    S.op("dve", lambda e: e.tensor_tensor(out=lg[:, 0:36], in0=pl_, in1=rbb[:], op=ALU.add), reads=[b_pms[0], b_rbb], writes=[b_lg])
                    S.op("dve", lambda e: e.reduce_max(out=lg[:, 36:37], in_=lg[:, 0:4], axis=AX.X), reads=[b_lg], writes=[b_lg])
                    S.op("dve", lambda e: e.tensor_scalar(out=rw[:, 0, 0:4], in0=lg[:, 0:4], scalar1=lg[:, 36:37], scalar2=None, op0=ALU.subtract),
                         reads=[b_lg], writes=[b_rw])
                    S.op("act", lambda e: e.activation(out=rw[:, 1, 0:4], in_=rw[:, 0, 0:4], func=AF.Exp), reads=[b_rw], writes=[b_rw])
                    S.op("dve", lambda e: e.reduce_sum(out=lg[:, 37:38], in_=rw[:, 1, 0:4], axis=AX.X), reads=[b_rw], writes=[b_lg])
                    S.op("dve", lambda e: e.reciprocal(out=lg[:, 37:38], in_=lg[:, 37:38]), reads=[b_lg], writes=[b_lg])
                    S.op("dve", lambda e: e.tensor_scalar(out=rw[:, 2, 0:4], in0=lg[:, 0:4], scalar1=lg[:, 36:37], scalar2=None, op0=ALU.is_ge),
                         reads=[b_lg], writes=[b_rw])
                    S.op("dve", lambda e: e.tensor_scalar(out=rw[:, 2, 0:4], in0=rw[:, 2, 0:4], scalar1=-1.0, scalar2=1e30, op0=ALU.add, op1=ALU.mult),
                         reads=[b_rw], writes=[b_rw])
                    for g in range(4):
                        S.op("dve", lambda e, g=g: e.tensor_scalar(out=rw[:, 3, g * 8:(g + 1) * 8], in0=lg[:, 4 + g * 8:4 + (g + 1) * 8],
                                                                   scalar1=rw[:, 2, g:g + 1], scalar2=None, op0=ALU.add), reads=[b_lg, b_rw], writes=[b_rw])
                    S.op("dve", lambda e: e.reduce_max(out=lg[:, 38:39], in_=rw[:, 3, :], axis=AX.X), reads=[b_rw], writes=[b_lg])
                    S.op("dve", lambda e: e.tensor_scalar(out=rw[:, 4, :], in0=rw[:, 3, :], scalar1=lg[:, 38:39], scalar2=None, op0=ALU.is_ge),
                         reads=[b_rw, b_lg], writes=[b_rw])
                    S.op("dve", lambda e: e.scalar_tensor_tensor(out=rw[:, 5, :], in0=rw[:, 4, :], scalar=-1e30, in1=rw[:, 3, :], op0=ALU.mult, op1=ALU.add),
                         reads=[b_rw], writes=[b_rw])
                    S.op("dve", lambda e: e.reduce_max(out=lg[:, 39:40], in_=rw[:, 5, :], axis=AX.X), reads=[b_rw], writes=[b_lg])
                    S.op("dve", lambda e: e.tensor_scalar(out=rw[:, 6, :], in0=rw[:, 5, :], scalar1=lg[:, 39:40], scalar2=None, op0=ALU.is_ge),
                         reads=[b_rw, b_lg], writes=[b_rw])
                    S.op("dve", lambda e: e.tensor_tensor(out=ss[:, 0:1], in0=lg[:, 38:39], in1=lg[:, 39:40], op=ALU.subtract), reads=[b_lg], writes=[b_ss])
                    S.op("act", lambda e: e.activation(out=ss[:, 0:1], in_=ss[:, 0:1], func=AF.Sigmoid), reads=[b_ss], writes=[b_ss])
                    S.op("dve", lambda e: e.tensor_tensor(out=ss[:, 0:1], in0=ss[:, 0:1], in1=lg[:, 37:38], op=ALU.mult), reads=[b_ss, b_lg], writes=[b_ss])
                    S.op("dve", lambda e: e.tensor_tensor(out=ss[:, 1:2], in0=lg[:, 37:38], in1=ss[:, 0:1], op=ALU.subtract), reads=[b_ss, b_lg], writes=[b_ss])
                    S.op("dve", lambda e: e.tensor_scalar(out=rw[:, 7, :], in0=rw[:, 6, :], scalar1=ss[:, 1:2], scalar2=None, op0=ALU.mult),
                         reads=[b_rw, b_ss], writes=[b_rw])
                    S.op("dve", lambda e, t=t: e.scalar_tensor_tensor(out=Wt[:, t, :], in0=rw[:, 4, :], scalar=ss[:, 0:1], in1=rw[:, 7, :],
                                                                      op0=ALU.mult, op1=ALU.add), reads=[b_rw, b_ss], writes=[b_Wt])

                S.barrier()
            if stop_after == "C1":
                S.barrier(); S.finish([]); return nc
            with ExitStack() as pe_:
                actT = sb("actT", [128, 8, SLAB], BF16, pe_); b_actT = S.bufs(8)
                sil = sb("sil", [128, 2, 512], F32, pe_); b_sil = S.bufs(2)
                accp = sb("accp", [128, 4, 1024], F32, pe_); b_accp = S.bufs(4)
                pg_hold = {}
                ai = [0]; si = [0]
                b_x3 = [[S.buf() for _ in range(4)] for _ in range(NT)]
                for t in range(NT):
                    for q4 in range(4):
                        b_x3[t][q4].writer = None
                jobs = []
                for ex in range(nexp):
                    def mke(ex):
                        js = []
                        for s4 in range(4):
                            def ldg(s4=s4):
                                return load_wslab(wg[ex, :, s4 * 256:(s4 + 1) * 256], KC, 256)

                            def cpg(hd, s4=s4):
                                pg_hold[s4] = hd
                            def ldu(s4=s4):
                                return load_wslab(wu[ex, :, s4 * 256:(s4 + 1) * 256], KC, 256)

                            def cpu(hd, s4=s4):
                                gw, gb = pg_hold.pop(s4)
                                uw, ub = hd
                                for cc in range(2):
                                    dch = s4 * 2 + cc
                                    for hf in range(2):
                                        pbg = next_pmm(); pvg = pmm[:, pbg, :]
                                        for k in range(KC):
                                            S.op("pe", lambda e, k=k, pvg=pvg, cc=cc, hf=hf: e.matmul(
                                                pvg, lhsT=gw[:, k, cc * 128:(cc + 1) * 128], rhs=AT[:, k, hf * 512:(hf + 1) * 512],
                                                start=(k == 0), stop=(k == KC - 1)), reads=[b_AT, gb], writes=[b_pmm[pbg]])
                                        pbu = next_pmm(); pvu = pmm[:, pbu, :]
                                        for k in range(KC):
                                            S.op("pe", lambda e, k=k, pvu=pvu, cc=cc, hf=hf: e.matmul(
                                                pvu, lhsT=uw[:, k, cc * 128:(cc + 1) * 128], rhs=AT[:, k, hf * 512:(hf + 1) * 512],
                                                start=(k == 0), stop=(k == KC - 1)), reads=[b_AT, ub], writes=[b_pmm[pbu]])
                                        r = si[0]; si[0] = (r + 1) % 2
                                        S.op("act", lambda e, r=r, pvg=pvg: e.activation(out=sil[:, r, :], in_=pvg, func=AF.Silu),
                                             reads=[b_pmm[pbg]], writes=[b_sil[r]])
                                        S.op("dve", lambda e, r=r, pvu=pvu, dch=dch, hf=hf: e.tensor_tensor(
                                            out=actT[:, dch, hf * 512:(hf + 1) * 512], in0=pvu, in1=sil[:, r, :], op=ALU.mult),
                                             reads=[b_pmm[pbu], b_sil[r]], writes=[b_actT[dch]])
                            js += [(ldg, cpg), (ldu, cpu)]
                        for q4 in range(4):
                            def ldd(q4=q4):
                                return load_wslab(wd[ex, :, q4 * 1024:(q4 + 1) * 1024], 8, 1024)

                            def cpd(hd, q4=q4):
                                dw, db = hd
                                srcd = x2_d if ex % 2 == 0 else x3_d
                                dstd = x3_d if ex % 2 == 0 else x2_d
                                pend = {}

                                def issue_load(t):
                                    a = ai[0]; ai[0] = (a + 1) % 4
                                    S.dma("sp", accp[:, a, :], srcd[t * 128:(t + 1) * 128, q4 * 1024:(q4 + 1) * 1024],
                                          reads=[b_x3[t][q4]], writes=[b_accp[a]])
                                    pend[t] = a
                                issue_load(0); issue_load(1)
                                for t in range(NT):
                                    a = pend.pop(t)
                                    if t + 2 < NT:
                                        issue_load(t + 2)
                                    for g2 in range(2):
                                        pb = next_pmm(); pv = pmm[:, pb, :]
                                        for k in range(8):
                                            S.op("pe", lambda e, k=k, pv=pv, t=t, g2=g2: e.matmul(
                                                pv, lhsT=actT[:, k, t * 128:(t + 1) * 128], rhs=dw[:, k, g2 * 512:(g2 + 1) * 512],
                                                start=(k == 0), stop=(k == 7)), reads=b_actT + [db], writes=[b_pmm[pb]])
                                        S.op("dve", lambda e, pv=pv, a=a, g2=g2, t=t: e.scalar_tensor_tensor(
                                            out=accp[:, a, g2 * 512:(g2 + 1) * 512], in0=pv, scalar=Wt[:, t, ex:ex + 1],
                                            in1=accp[:, a, g2 * 512:(g2 + 1) * 512], op0=ALU.mult, op1=ALU.add),
                                             reads=[b_pmm[pb], b_Wt, b_accp[a]], writes=[b_accp[a]])
                                    S.dma("sp", dstd[t * 128:(t + 1) * 128, q4 * 1024:(q4 + 1) * 1024], accp[:, a, :],
                                          reads=[b_accp[a]], writes=[b_x3[t][q4]])
                            js.append((ldd, cpd))
                        return js
                    jobs += mke(ex)
                run_jobs(jobs)
                S.barrier()

            deps = []
            for row in b_x3:
                for b in row:
                    if b.writer is not None:
                        deps.append(b.writer)
            S._wait(S.E["sp"], deps)
            with ExitStack() as pf:
                gbc = sb("f_gbc", [128, D], F32, pf); b_g = S.buf()
                xt = sb("f_xt", [128, 2, D], F32, pf); b_xt = S.bufs(2)
                ss = sb("f_ss", [128, 4], F32, pf); b_ss = S.bufs(2)
                junk = sb("f_junk", [128, D], F32, pf); b_junk = S.buf()
                b_out = S.bufs(NT)
                S.dma("sp", gbc[:], fin_g.partition_broadcast(128), writes=[b_g])
                for t in range(NT):
                    r = t % 2
                    S.dma("sp", xt[:, r, :], x2_d[t * 128:(t + 1) * 128, :], writes=[b_xt[r]])
                    S.op("act", lambda e, r=r: e.activation(out=junk[:], in_=xt[:, r, :], func=AF.Square), reads=[b_xt[r]], writes=[b_junk])
                    S.op("dve", lambda e, r=r: e.reduce_sum(out=ss[:, 2 * r:2 * r + 1], in_=junk[:], axis=AX.X), reads=[b_junk], writes=[b_ss[r]])
                    rms_rstd(ss[:, 2 * r:2 * r + 1], float(D), 1e-6, ss[:, 2 * r + 1:2 * r + 2], b_ss[r])
                    S.op("dve", lambda e, r=r: e.scalar_tensor_tensor(out=xt[:, r, :], in0=xt[:, r, :], scalar=ss[:, 2 * r + 1:2 * r + 2], in1=gbc[:],
                                                                      op0=ALU.mult, op1=ALU.mult), reads=[b_xt[r], b_ss[r], b_g], writes=[b_xt[r]])
                    S.dma("sp", out_d[t * 128:(t + 1) * 128, :], xt[:, r, :], reads=[b_xt[r]], writes=[b_out[t]])
                S.finish(b_out)
    return nc


_CACHE = {}


def _prep_inputs(inp):
    f32 = np.float32
    x = np.asarray(inp["x"], f32)[0]
    pos = np.asarray(inp["positions"])[0].astype(np.int32)
    cst = make_consts()
    lru_cw = np.asarray(inp["lru_conv_w"], f32)[0]
    pp = np.zeros((128, NPP), f32)
    for ch in range(16):
        sl = slice(ch * 128, (ch + 1) * 128)
        for jj in range(4):
            pp[:, P_CW + ch * 4 + jj] = lru_cw[jj, sl]
        pp[:, P_CB + ch] = np.asarray(inp["lru_conv_b"], f32)[0, sl]
        pp[:, P_BA + ch] = np.asarray(inp["lru_b_a"], f32)[0, sl]
        pp[:, P_BI + ch] = np.asarray(inp["lru_b_i"], f32)[0, sl]
        pp[:, P_LAM + ch] = np.asarray(inp["lru_lambda"], f32)[0, sl]
        pp[:, P_LG + ch] = np.asarray(inp["lru_norm_g"], f32)[0, sl]
    wr = np.concatenate([np.asarray(inp["router_group_w"], f32)[0],
                         np.asarray(inp["router_expert_w"], f32)[0]], axis=1)
    wr = np.ascontiguousarray(wr.reshape(KC, 128, 36).transpose(1, 0, 2).reshape(128, KC * 36))
    rb = np.ascontiguousarray(np.concatenate([np.asarray(inp["router_group_b"], f32)[0],
                                              np.asarray(inp["router_expert_b"], f32)[0]])[None, :])
    shared = {
        "mem": np.ascontiguousarray(np.asarray(inp["mem"], f32)[0]),
        "cst": cst, "pp": pp,
        "mix_g": np.asarray(inp["mix_norm_g"], f32).reshape(1, D),
        "gn_g": np.asarray(inp["ret_norm_g"], f32).reshape(1, 2048),
        "xat_g": np.asarray(inp["xattn_norm_g"], f32).reshape(1, D),
        "mem_g": np.asarray(inp["mem_norm_g"], f32).reshape(1, D),
        "moe_g": np.asarray(inp["moe_norm_g"], f32).reshape(1, D),
        "fin_g": np.asarray(inp["final_norm_g"], f32).reshape(1, D),
        "rb": rb, "wr": wr,
        "w_in": np.asarray(inp["w_in"], f32)[0], "w_out": np.asarray(inp["w_out"], f32)[0],
        "wq": np.asarray(inp["xattn_wq"], f32)[0], "wk": np.asarray(inp["xattn_wk"], f32)[0],
        "wv": np.asarray(inp["xattn_wv"], f32)[0], "wo": np.asarray(inp["xattn_wo"], f32)[0],
        "w_a": np.asarray(inp["lru_w_a"], f32)[0], "w_i": np.asarray(inp["lru_w_i"], f32)[0],
        "wg": np.asarray(inp["expert_w_gate"], f32)[0], "wu": np.asarray(inp["expert_w_up"], f32)[0],
        "wd": np.asarray(inp["expert_w_down"], f32)[0],
    }
    in_maps = []
    for c in range(NCORES):
        xs = np.zeros((NSLAB * SLAB, D), f32)
        ps_ = np.zeros((NSLAB * SLAB,), np.int32)
        vm = np.zeros((128, 8), f32)
        for j in range(NSLAB):
            g = c - (NSLAB - 1) + j
            if g >= 0:
                xs[j * SLAB:(j + 1) * SLAB] = x[g * SLAB:(g + 1) * SLAB]
                ps_[j * SLAB:(j + 1) * SLAB] = pos[g * SLAB:(g + 1) * SLAB]
                vm[:, j] = 1.0
        pos_pm = np.ascontiguousarray(ps_.reshape(64, 128).T)
        m = dict(shared)
        m.update(xs=xs, pos_pm=pos_pm, vmask=vm)
        in_maps.append(m)
    return in_maps


def kernel(**inputs):
    if "nc" not in _CACHE:
        _CACHE["nc"] = build_program()
    nc = _CACHE["nc"]
    in_maps = _prep_inputs(inputs)
    res = run_bass_kernel_spmd(nc, in_maps, core_ids=list(range(NCORES)))
    out = np.concatenate([np.asarray(r["out"], np.float32) for r in res.results], axis=0)
    return out.reshape(1, NCORES * SLAB, D)
```
